# Optimizing a Trainium2 kernel written in Bass

```python
import jax, jax.numpy as jnp
from jax import lax
import numpy as np

D_MODEL = 2048
BATCH = 4
SEQ = 2048
DEPTH = 1

HEAD_DIM = 128
NSA_HEADS = 8
NSA_KV_HEADS = 2
SB_HEADS = 8
ROT_DIM = HEAD_DIM // 4
ROPE_THETA = 500000.0
CMP_LEN = 32
CMP_STRIDE = 16
SEL_LEN = 64
SEL_TOPN = 16
WINDOW = 512
FORCE_BONUS = 1000.0
QBLOCK = 128
SEL_QBLOCK = 32
MEM_LEN = 256
X_HEADS = 4
N_GROUPS = 4
EXPERTS_PER_GROUP = 8
N_EXPERTS = N_GROUPS * EXPERTS_PER_GROUP
D_EXPERT = 512
TOPK_IN_GROUP = 2
EPS = 1e-6

NSA_Q_W = NSA_HEADS * HEAD_DIM
NSA_KV_W = NSA_KV_HEADS * HEAD_DIM
SB_W = SB_HEADS * HEAD_DIM
X_W = X_HEADS * HEAD_DIM
IN_SIZES = (NSA_Q_W,) + (NSA_KV_W,) * 6 + (3 * NSA_HEADS, SB_W, SB_W, SB_W, D_MODEL, D_MODEL)
IN_WIDTH = sum(IN_SIZES)

kernel_name = "hybrid_nsa_stickbreak_hmoe_block"


def rms_norm(x, g):
    xf = x.astype(jnp.float32)
    y = xf * lax.rsqrt(jnp.mean(xf * xf, axis=-1, keepdims=True) + EPS)
    return (y * g.astype(jnp.float32)).astype(x.dtype)


def to_heads(x, n):
    b, t, _ = x.shape
    return x.reshape(b, t, n, HEAD_DIM).transpose(0, 2, 1, 3)


def from_heads(x):
    b, h, t, d = x.shape
    return x.transpose(0, 2, 1, 3).reshape(b, t, h * d)


def partial_rope(x, positions):
    half = ROT_DIM // 2
    inv = ROPE_THETA ** (-jnp.arange(half, dtype=jnp.float32) * (2.0 / ROT_DIM))
    ang = positions.astype(jnp.float32)[:, None, :, None] * inv
    cos, sin = jnp.cos(ang), jnp.sin(ang)
    xf = x.astype(jnp.float32)
    x1, x2 = xf[..., :half], xf[..., half:ROT_DIM]
    out = jnp.concatenate([x1 * cos - x2 * sin, x2 * cos + x1 * sin, xf[..., ROT_DIM:]], axis=-1)
    return out.astype(x.dtype)


def masked_softmax(s, mask):
    s = jnp.where(mask, s, -jnp.inf)
    m = jnp.max(s, axis=-1, keepdims=True)
    m = jnp.where(jnp.isfinite(m), m, 0.0)
    e = jnp.where(mask, jnp.exp(s - m), 0.0)
    den = jnp.sum(e, axis=-1, keepdims=True)
    return e / jnp.where(den > 0, den, 1.0)


def compress_blocks(tok_blocks, pe, w1, w2):
    flat = (tok_blocks + pe).reshape(*tok_blocks.shape[:-2], CMP_LEN * HEAD_DIM)
    return jax.nn.silu(flat @ w1) @ w2


def nsa_attention(q, kc_tok, vc_tok, ks, vs, kw, vw, gates,
                  pe_k, w1_k, w2_k, pe_v, w1_v, w2_v):
    b, h, t_len, d = q.shape
    hkv = kc_tok.shape[1]
    grp = h // hkv
    scale = d ** -0.5
    qg = q.reshape(b, hkv, grp, t_len, d)
    t_all = jnp.arange(t_len)

    n_cmp = (t_len - CMP_LEN) // CMP_STRIDE + 1
    cmp_idx = np.arange(n_cmp)[:, None] * CMP_STRIDE + np.arange(CMP_LEN)[None, :]
    kc = compress_blocks(kc_tok[:, :, cmp_idx], pe_k, w1_k, w2_k)
    vc = compress_blocks(vc_tok[:, :, cmp_idx], pe_v, w1_v, w2_v)
    cmp_vis = jnp.asarray(cmp_idx[:, -1])[None, :] <= t_all[:, None]
    s_c = jnp.einsum("bkgtd,bkcd->bkgtc", qg, kc).astype(jnp.float32) * scale
    p_c = masked_softmax(s_c, cmp_vis)
    o_c = jnp.einsum("bkgtc,bkcd->bkgtd", p_c.astype(vc.dtype), vc)

    n_sel = t_len // SEL_LEN
    cmp_start = np.arange(n_cmp) * CMP_STRIDE
    sel_start = np.arange(n_sel) * SEL_LEN
    overlap = ((cmp_start[:, None] < sel_start[None, :] + SEL_LEN)
               & (cmp_start[:, None] + CMP_LEN > sel_start[None, :])).astype(np.float32)
    imp = jnp.einsum("bkgtc,cj->bktj", p_c, jnp.asarray(overlap))
    cur = t_all // SEL_LEN
    jb = jnp.arange(n_sel)
    forced = (jb[None, :] == 0) | (jb[None, :] == cur[:, None]) | (jb[None, :] == cur[:, None] - 1)
    valid = jnp.asarray(sel_start)[None, :] <= t_all[:, None]
    score = jnp.where(valid, imp + jnp.where(forced, FORCE_BONUS, 0.0), -jnp.inf)
    top_n = min(SEL_TOPN, n_sel)
    top_val, top_idx = lax.top_k(score, top_n)
    top_ok = jnp.isfinite(top_val)

    k_blk = ks.reshape(b, hkv, n_sel, SEL_LEN, d)
    v_blk = vs.reshape(b, hkv, n_sel, SEL_LEN, d)
    kw_pad = jnp.pad(kw, ((0, 0), (0, 0), (WINDOW, 0), (0, 0)))
    vw_pad = jnp.pad(vw, ((0, 0), (0, 0), (WINDOW, 0), (0, 0)))
    nq = t_len // SEL_QBLOCK
    q_blocks = jnp.moveaxis(qg.reshape(b, hkv, grp, nq, SEL_QBLOCK, d), 3, 0)
    i_blocks = jnp.moveaxis(top_idx.reshape(b, hkv, nq, SEL_QBLOCK, top_n), 2, 0)
    ok_blocks = jnp.moveaxis(top_ok.reshape(b, hkv, nq, SEL_QBLOCK, top_n), 2, 0)
    bi = jnp.arange(b)[:, None, None, None]
    hi = jnp.arange(hkv)[None, :, None, None]

    def one_block(args):
        qb, ib, okb, c = args
        tq = c * SEL_QBLOCK + jnp.arange(SEL_QBLOCK)
        kg = k_blk[bi, hi, ib]
        vg = v_blk[bi, hi, ib]
        s_pos = ib[..., None] * SEL_LEN + jnp.arange(SEL_LEN)
        allow = okb[..., None] & (s_pos <= tq[None, None, :, None, None])
        s = jnp.einsum("bkgtd,bktnld->bkgtnl", qb, kg).astype(jnp.float32) * scale
        p = masked_softmax(s.reshape(*s.shape[:4], -1),
                           allow.reshape(b, hkv, 1, SEL_QBLOCK, -1)).reshape(s.shape)
        o_s = jnp.einsum("bkgtnl,bktnld->bkgtd", p.astype(vg.dtype), vg)
        kwb = lax.dynamic_slice_in_dim(kw_pad, c * SEL_QBLOCK, WINDOW + SEL_QBLOCK, axis=2)
        vwb = lax.dynamic_slice_in_dim(vw_pad, c * SEL_QBLOCK, WINDOW + SEL_QBLOCK, axis=2)
        w_pos = c * SEL_QBLOCK - WINDOW + jnp.arange(WINDOW + SEL_QBLOCK)
        diff = tq[:, None] - w_pos[None, :]
        allow_w = (diff >= 0) & (diff < WINDOW) & (w_pos[None, :] >= 0)
        s_w = jnp.einsum("bkgtd,bksd->bkgts", qb, kwb).astype(jnp.float32) * scale
        p_w = masked_softmax(s_w, allow_w)
        o_w = jnp.einsum("bkgts,bksd->bkgtd", p_w.astype(vwb.dtype), vwb)
        return o_s, o_w

    o_s, o_w = lax.map(one_block, (q_blocks, i_blocks, ok_blocks, jnp.arange(nq)))
    o_s = jnp.moveaxis(o_s, 0, 3).reshape(b, hkv, grp, t_len, d)
    o_w = jnp.moveaxis(o_w, 0, 3).reshape(b, hkv, grp, t_len, d)
    g = gates.transpose(0, 2, 1, 3).reshape(b, hkv, grp, t_len, 3).astype(q.dtype)
    o = g[..., 0:1] * o_c + g[..., 1:2] * o_s + g[..., 2:3] * o_w
    return o.reshape(b, h, t_len, d)


def stick_breaking_attention(q, k, v):
    b, h, t_len, d = q.shape
    scale = d ** -0.5
    outs = []
    for c in range(t_len // QBLOCK):
        end = (c + 1) * QBLOCK
        qb = q[:, :, c * QBLOCK:end]
        z = jnp.einsum("bhtd,bhsd->bhts", qb, k[:, :, :end]).astype(jnp.float32) * scale
        tq = c * QBLOCK + jnp.arange(QBLOCK)
        strict = jnp.arange(end)[None, :] < tq[:, None]
        log_1m = jnp.where(strict, jax.nn.log_sigmoid(-z), 0.0)
        log_w = jax.nn.log_sigmoid(z) + lax.cumsum(log_1m, axis=3, reverse=True) - log_1m
        a = jnp.where(strict, jnp.exp(log_w), 0.0)
        outs.append(jnp.einsum("bhts,bhsd->bhtd", a.astype(v.dtype), v[:, :, :end]))
    return jnp.concatenate(outs, axis=2)


def cross_attention(h, m, w_q, w_k, w_v, w_out):
    q = to_heads(h @ w_q, X_HEADS)
    k = to_heads(m @ w_k, X_HEADS)
    v = to_heads(m @ w_v, X_HEADS)
    s = jnp.einsum("bhtd,bhmd->bhtm", q, k).astype(jnp.float32) * (HEAD_DIM ** -0.5)
    p = jax.nn.softmax(s, axis=-1).astype(v.dtype)
    return from_heads(jnp.einsum("bhtm,bhmd->bhtd", p, v)) @ w_out


def hier_moe(h, w_rg, b_rg, w_re, b_re, w_eg, w_eu, w_ed):
    b, t_len, d = h.shape
    hn = h.reshape(b * t_len, d)
    p_grp = jax.nn.softmax((hn @ w_rg + b_rg).astype(jnp.float32), axis=-1)
    g_top = jnp.argmax(p_grp, axis=-1)
    p_gtop = jnp.max(p_grp, axis=-1)
    e_logits = (hn @ w_re + b_re).astype(jnp.float32).reshape(-1, N_GROUPS, EXPERTS_PER_GROUP)
    e_in = jnp.take_along_axis(e_logits, g_top[:, None, None], axis=1)[:, 0]
    p_in = jax.nn.softmax(e_in, axis=-1)
    top_p, top_e = lax.top_k(p_in, TOPK_IN_GROUP)
    w_sel = top_p / jnp.sum(top_p, axis=-1, keepdims=True) * p_gtop[:, None]
    w_grp = jnp.sum(jax.nn.one_hot(top_e, EXPERTS_PER_GROUP, dtype=jnp.float32) * w_sel[..., None], axis=1)
    combine = (jax.nn.one_hot(g_top, N_GROUPS, dtype=jnp.float32)[:, :, None] * w_grp[:, None, :]).astype(h.dtype)
    y = jnp.zeros_like(hn)
    for gi in range(N_GROUPS):
        a = jnp.einsum("nd,edf->nef", hn, w_eg[gi])
        u = jnp.einsum("nd,edf->nef", hn, w_eu[gi])
        y = y + jnp.einsum("nef,efd->nd", jax.nn.silu(a) * u * combine[:, gi, :, None], w_ed[gi])
    return y.reshape(b, t_len, d)


def setup_inputs(seed: int = 0) -> dict:
    key = jax.random.key(seed)
    ks = iter(jax.random.split(key, 40))
    f32 = jnp.float32

    def dense(shape, fan_in):
        return jax.random.normal(next(ks), shape, f32) * (fan_in ** -0.5)

    def gain(shape):
        return 1.0 + 0.02 * jax.random.normal(next(ks), shape, f32)

    def small(shape, s):
        return s * jax.random.normal(next(ks), shape, f32)

    L = DEPTH
    return {
        "x": jax.random.normal(next(ks), (BATCH, SEQ, D_MODEL), f32),
        "mem": jax.random.normal(next(ks), (BATCH, MEM_LEN, D_MODEL), f32),
        "positions": jnp.broadcast_to(jnp.arange(SEQ, dtype=jnp.int32), (BATCH, SEQ)),
        "g_mix": gain((L, D_MODEL)),
        "w_in": dense((L, D_MODEL, IN_WIDTH), D_MODEL),
        "cmp_pe_k": small((L, CMP_LEN, HEAD_DIM), 0.1),
        "cmp_w1_k": dense((L, CMP_LEN * HEAD_DIM, HEAD_DIM), CMP_LEN * HEAD_DIM),
        "cmp_w2_k": dense((L, HEAD_DIM, HEAD_DIM), HEAD_DIM),
        "cmp_pe_v": small((L, CMP_LEN, HEAD_DIM), 0.1),
        "cmp_w1_v": dense((L, CMP_LEN * HEAD_DIM, HEAD_DIM), CMP_LEN * HEAD_DIM),
        "cmp_w2_v": dense((L, HEAD_DIM, HEAD_DIM), HEAD_DIM),
        "w_br_nsa": dense((L, NSA_Q_W, D_MODEL), NSA_Q_W),
        "w_br_sb": dense((L, SB_W, D_MODEL), SB_W),
        "w_o": dense((L, D_MODEL, D_MODEL), D_MODEL),
        "g_cross": gain((L, D_MODEL)),
        "g_mem": gain((L, D_MODEL)),
        "w_cq": dense((L, D_MODEL, X_W), D_MODEL),
        "w_ck": dense((L, D_MODEL, X_W), D_MODEL),
        "w_cv": dense((L, D_MODEL, X_W), D_MODEL),
        "w_co": dense((L, X_W, D_MODEL), X_W),
        "g_moe": gain((L, D_MODEL)),
        "w_rg": dense((L, D_MODEL, N_GROUPS), D_MODEL),
        "b_rg": small((L, N_GROUPS), 0.01),
        "w_re": dense((L, D_MODEL, N_EXPERTS), D_MODEL),
        "b_re": small((L, N_EXPERTS), 0.01),
        "w_eg": dense((L, N_GROUPS, EXPERTS_PER_GROUP, D_MODEL, D_EXPERT), D_MODEL),
        "w_eu": dense((L, N_GROUPS, EXPERTS_PER_GROUP, D_MODEL, D_EXPERT), D_MODEL),
        "w_ed": dense((L, N_GROUPS, EXPERTS_PER_GROUP, D_EXPERT, D_MODEL), D_EXPERT),
        "g_final": gain((D_MODEL,)),
    }


def reference(x, mem, positions, g_mix, w_in, cmp_pe_k, cmp_w1_k, cmp_w2_k,
              cmp_pe_v, cmp_w1_v, cmp_w2_v, w_br_nsa, w_br_sb, w_o,
              g_cross, g_mem, w_cq, w_ck, w_cv, w_co,
              g_moe, w_rg, b_rg, w_re, b_re, w_eg, w_eu, w_ed, g_final):
    b, t_len, _ = x.shape
    split_at = [int(v) for v in np.cumsum(IN_SIZES)[:-1]]
    for l in range(DEPTH):
        h = rms_norm(x, g_mix[l])
        (q_n, kc_t, vc_t, ks_t, vs_t, kw_t, vw_t, nsa_g,
         q_s, k_s, v_s, gate_a, gate_b) = jnp.split(h @ w_in[l], split_at, axis=-1)
        q_n = partial_rope(to_heads(q_n, NSA_HEADS), positions)
        kc_t = partial_rope(to_heads(kc_t, NSA_KV_HEADS), positions)
        ks_t = partial_rope(to_heads(ks_t, NSA_KV_HEADS), positions)
        kw_t = partial_rope(to_heads(kw_t, NSA_KV_HEADS), positions)
        nsa_gates = jax.nn.sigmoid(nsa_g.astype(jnp.float32)).reshape(b, t_len, NSA_HEADS, 3)
        o_nsa = nsa_attention(q_n, kc_t, to_heads(vc_t, NSA_KV_HEADS), ks_t,
                              to_heads(vs_t, NSA_KV_HEADS), kw_t, to_heads(vw_t, NSA_KV_HEADS),
                              nsa_gates, cmp_pe_k[l], cmp_w1_k[l], cmp_w2_k[l],
                              cmp_pe_v[l], cmp_w1_v[l], cmp_w2_v[l])
        o_sb = stick_breaking_attention(to_heads(q_s, SB_HEADS), to_heads(k_s, SB_HEADS),
                                        to_heads(v_s, SB_HEADS))
        y_a = from_heads(o_nsa) @ w_br_nsa[l]
        y_b = from_heads(o_sb) @ w_br_sb[l]
        ga = jax.nn.sigmoid(gate_a.astype(jnp.float32)).astype(x.dtype)
        gb = jax.nn.sigmoid(gate_b.astype(jnp.float32)).astype(x.dtype)
        x = x + (ga * y_a + gb * y_b) @ w_o[l]
        x = x + cross_attention(rms_norm(x, g_cross[l]), rms_norm(mem, g_mem[l]),
                                w_cq[l], w_ck[l], w_cv[l], w_co[l])
        x = x + hier_moe(rms_norm(x, g_moe[l]), w_rg[l], b_rg[l], w_re[l], b_re[l],
                         w_eg[l], w_eu[l], w_ed[l])
    return rms_norm(x, g_final)
```

```python
import types
import numpy as np
from contextlib import ExitStack
import concourse.bass as bass
import concourse.mybir as mybir
from concourse.bass_utils import run_bass_kernel_spmd

F32 = mybir.dt.float32
BF16 = mybir.dt.bfloat16
I32 = mybir.dt.int32
U8 = mybir.dt.uint8
AF = mybir.ActivationFunctionType
ALU = mybir.AluOpType

D = 2048
T = 2048
NOWN = 1024
HD = 128
NEG = -30000.0
SCALE = HD ** -0.5
C_QN, C_KC, C_VC, C_KS, C_VS, C_KW, C_VW, C_G, C_QS, C_KSB, C_VSB, C_GA, C_GB = (
    0, 1024, 1280, 1536, 1792, 2048, 2304, 2560, 2584, 3608, 4632, 5656, 7704)
IN_W = 9752


def _freeze(fn):
    if fn.__closure__ is None:
        return fn
    cells = []
    for c in fn.__closure__:
        try:
            cells.append(types.CellType(c.cell_contents))
        except ValueError:
            cells.append(c)
    return types.FunctionType(fn.__code__, fn.__globals__, fn.__name__, fn.__defaults__, tuple(cells))


class Buf:
    __slots__ = ("name", "w", "r", "dsem", "dcnt", "excl")

    def __init__(self, name):
        self.name = name
        self.excl = False
        self.w = None
        self.r = []
        self.dsem = None
        self.dcnt = 0


class Sched:
    ENGS = ("pe", "act", "dve", "pool", "sp")

    def __init__(self, nc, stack):
        self.nc = nc
        self.stack = stack
        self.sem = {e: stack.enter_context(nc.semaphore("s_" + e)) for e in self.ENGS}
        self.cnt = {e: 0 for e in self.ENGS}
        self.q = {e: [] for e in self.ENGS}
        self.waited = {e: {} for e in self.ENGS}
        self.nsem = 5
        self.ninst = 0
        self.tail = []
        self.bufs = {}
        self.dma_bufs = []

    def B(self, *key):
        b = self.bufs.get(key)
        if b is None:
            b = Buf(str(key))
            self.bufs[key] = b
        return b

    def _wait(self, eng, ev):
        if ev is None:
            return
        sem, val = ev
        if sem is self.sem[eng]:
            if eng in ("pe", "sp") or val > self.cnt[eng]:
                return
        w = self.waited[eng]
        if w.get(id(sem), 0) >= val:
            return
        w[id(sem)] = val
        self.q[eng].append(("wait", sem, val))

    def _deps(self, eng, reads, writes):
        for b in reads:
            self._wait(eng, b.w)
            if b.excl:
                for ev in b.r:
                    if ev[0] is not self.sem[eng]:
                        self._wait(eng, ev)
        for b in writes:
            self._wait(eng, b.w)
            for ev in b.r:
                self._wait(eng, ev)

    def _commit(self, ev, reads, writes):
        for b in reads:
            b.r = [x for x in b.r if x[0] is not ev[0]] + [ev]
        for b in writes:
            b.w = ev
            b.r = []

    def op(self, eng, fn, reads=(), writes=(), signal=True):
        self._deps(eng, reads, writes)
        if signal:
            self.cnt[eng] += 1
            ev = (self.sem[eng], self.cnt[eng])
        else:
            ev = (self.sem[eng], self.cnt[eng] + 1)
        self.q[eng].append(("op", _freeze(fn), signal))
        self._commit(ev, reads, writes)
        self.ninst += 1
        return ev

    def _dsem(self, b):
        if b.dsem is None:
            b.dsem = self.stack.enter_context(self.nc.semaphore("d%d" % self.nsem))
            self.nsem += 1
            self.dma_bufs.append(b)
        return b.dsem

    def dma(self, eng, out, in_, reads=(), writes=(), owner=None):
        self._deps(eng, reads, writes)
        owner = owner or (writes[0] if writes else reads[0])
        sem = self._dsem(owner)
        owner.dcnt += 16
        ev = (sem, owner.dcnt)
        self.q[eng].append(("dma", out, in_, sem))
        self._commit(ev, reads, writes)
        self.ninst += 1
        return ev

    def barrier(self, exclude=()):
        self.marks = getattr(self, "marks", [])
        self.marks.append({e: sum(1 for it in self.q[e] if it[0] != "wait") for e in self.ENGS})
        evs = [(self.sem[e], self.cnt[e]) for e in self.ENGS if self.cnt[e] > 0]
        evs += [(b.dsem, b.dcnt) for b in self.dma_bufs if b.dcnt > 0 and b not in exclude]
        for e in self.ENGS:
            for ev in evs:
                self._wait(e, ev)

    def check(self):
        val = {}
        pc = {e: 0 for e in self.ENGS}
        prog = True
        while prog:
            prog = False
            for e in self.ENGS:
                q = self.q[e]
                while pc[e] < len(q):
                    it = q[pc[e]]
                    if it[0] == "wait":
                        if val.get(id(it[1]), 0) < it[2]:
                            break
                    elif it[0] == "op":
                        if it[2]:
                            val[id(self.sem[e])] = val.get(id(self.sem[e]), 0) + 1
                    else:
                        val[id(it[3])] = val.get(id(it[3]), 0) + 16
                    pc[e] += 1
                    prog = True
        stuck = {e: (pc[e], len(self.q[e])) for e in self.ENGS if pc[e] < len(self.q[e])}
        if stuck:
            for e, (p, n) in stuck.items():
                it = self.q[e][p]
                print("DEADLOCK", e, p, n, it[0], it[2], "have", val.get(id(it[1]), 0))
            raise RuntimeError("deadlock in schedule: %s" % stuck)
        print("schedule ok: insts", {e: len(self.q[e]) for e in self.ENGS}, "nsem", self.nsem)

    def emit(self):
        nc = self.nc
        for ev in self.tail:
            self._wait("sp", ev)
        self.check()
        with nc.Block() as block:
            def run(eng):
                def body(e):
                    sem_e = self.sem[eng]
                    for item in self.q[eng]:
                        k = item[0]
                        if k == "wait":
                            e.wait_ge(item[1], item[2])
                        elif k == "op":
                            ins = item[1](e)
                            if item[2]:
                                ins.then_inc(sem_e, 1)
                        else:
                            _, out, in_, sem = item
                            e.dma_start(out=out, in_=in_).then_inc(sem, 16)
                return body
            block.tensor(run("pe"))
            block.scalar(run("act"))
            block.vector(run("dve"))
            block.gpsimd(run("pool"))
            block.sync(run("sp"))


class Arena:
    def __init__(self, nc, nbytes):
        self.t = nc.alloc_sbuf_tensor("arena", [128, nbytes], U8)
        self.n = nbytes
        self.off = 0
        self.top = nbytes

    def alloc(self, shape, dt, top=False):
        esz = {F32: 4, BF16: 2, I32: 4}[dt]
        used = 1
        for s in shape[1:]:
            used *= s
        n = (used * esz + 63) // 64 * 64
        assert self.off + n <= self.top, ("SBUF arena overflow", self.off, n, self.top)
        if top:
            self.top -= n
            o = self.top
        else:
            o = self.off
            self.off += n
        v = self.t[0:shape[0], o:o + n].bitcast(dt)[:, 0:used]
        if len(shape) == 3:
            v = v.rearrange("p (a b) -> p a b", a=shape[1])
        elif len(shape) == 4:
            v = v.rearrange("p (a b c) -> p a b c", a=shape[1], b=shape[2])
        return v

    def mark(self):
        return self.off

    def release(self, m):
        self.off = m

    def release_top(self):
        self.top = self.n


def build(stop_after=99, taps=()):
    nc = bass.Bass("TRN2", target_bir_lowering=False)
    din = {}

    def dram_in(name, shape, dt=F32):
        din[name] = nc.dram_tensor(name, list(shape), dt, kind="ExternalInput").ap()
        return din[name]

    x_nat = dram_in("x_nat", [T, D])
    x_own = dram_in("x_own", [NOWN, D])
    pos_nat = dram_in("pos_nat", [1, T], I32)
    pos_own = dram_in("pos_own", [1, NOWN], I32)
    mem = dram_in("mem", [256, D])
    g_mix = dram_in("g_mix", [1, D]); g_cross = dram_in("g_cross", [1, D]); g_mem = dram_in("g_mem", [1, D])
    g_moe = dram_in("g_moe", [1, D]); g_final = dram_in("g_final", [1, D])
    w_in = dram_in("w_in", [D, IN_W])
    cmp_pe_k = dram_in("cmp_pe_k", [32, 128]); cmp_w1_k = dram_in("cmp_w1_k", [4096, 128]); cmp_w2_k = dram_in("cmp_w2_k", [128, 128])
    cmp_pe_v = dram_in("cmp_pe_v", [32, 128]); cmp_w1_v = dram_in("cmp_w1_v", [4096, 128]); cmp_w2_v = dram_in("cmp_w2_v", [128, 128])
    w_br_nsa = dram_in("w_br_nsa", [1024, D]); w_br_sb = dram_in("w_br_sb", [1024, D]); w_o = dram_in("w_o", [D, D])
    w_cq = dram_in("w_cq", [D, 512]); w_ck = dram_in("w_ck", [D, 512]); w_cv = dram_in("w_cv", [D, 512]); w_co = dram_in("w_co", [512, D])
    w_r = dram_in("w_r", [D, 36])
    b_r = dram_in("b_r", [1, 36])
    w_eg = dram_in("w_eg", [32, D, 512]); w_eu = dram_in("w_eu", [32, D, 512]); w_ed = dram_in("w_ed", [32, 512, D])
    c_ident = dram_in("c_ident", [128, 128]); c_negtri = dram_in("c_negtri", [128, 128]); c_negones = dram_in("c_negones", [128, 128])
    c_r32 = dram_in("c_r32", [32, 32]); c_invf = dram_in("c_invf", [32, 2])
    c_cbi = dram_in("c_cbi", [128, 4 * 256]); c_cbs = dram_in("c_cbs", [128, 4 * 256]); c_wb = dram_in("c_wb", [128, 8 * 256])
    c_cmpb = dram_in("c_cmpb", [128, NOWN]); c_fb = dram_in("c_fb", [128, 8 * 32]); c_vm = dram_in("c_vm", [128, 8 * 32])
    c_E = dram_in("c_E", [32, 16 * 128]); c_ovl = dram_in("c_ovl", [128, 33])
    out_d = nc.dram_tensor("out", [NOWN, D], F32, kind="ExternalOutput").ap()
    tap_d = {}
    for nm, shp, dt in taps:
        tap_d[nm] = nc.dram_tensor("tap_" + nm, list(shp), dt, kind="ExternalOutput").ap()
    scr_hTown = nc.dram_tensor("scr_hTown", [128, 16, NOWN], BF16, kind="Internal").ap()
    scr_qsb = nc.dram_tensor("scr_qsb", [8, 128, NOWN], BF16, kind="Internal").ap()
    scr_ksb = nc.dram_tensor("scr_ksb", [8, 128, T], BF16, kind="Internal").ap()
    scr_vsb = nc.dram_tensor("scr_vsb", [128, 16, 1024], BF16, kind="Internal").ap()

    with ExitStack() as st:
        S = Sched(nc, st)
        B = S.B
        AR = Arena(nc, 206 * 1024)
        ps = [nc.alloc_psum_tensor("ps%d" % i, [128, 512], F32) for i in range(8)]
        PB = [B("ps", i) for i in range(8)]
        for pb_ in PB:
            pb_.excl = True

        def tap(name, src_ap, src_bufs):
            if name in tap_d:
                S.tail.append(S.dma("sp", tap_d[name], src_ap, reads=src_bufs))

        identf = AR.alloc([128, 128], F32)
        ident = AR.alloc([128, 128], BF16)
        r32 = AR.alloc([32, 32], F32)
        invf = AR.alloc([32, 2], F32)
        epsc = AR.alloc([128, 2], F32)
        stats = AR.alloc([128, 256], F32)
        stage_c = AR.alloc([128, 2048], F32)
        m_ess = AR.mark()
        negtri = AR.alloc([128, 128], BF16)
        negones = AR.alloc([128, 128], BF16)
        bC = B("const")

        def load_const_bf16(dst, src, ncols):
            S.dma("sp", stage_c[0:dst.shape[0], 0:ncols], src, writes=[B("stage_c")])
            S.op("dve", lambda e: e.tensor_copy(out=dst, in_=stage_c[0:dst.shape[0], 0:ncols]),
                 reads=[B("stage_c")], writes=[bC])

        S.dma("sp", identf, c_ident[:, :], writes=[bC])
        S.dma("sp", r32, c_r32[:, :], writes=[bC])
        S.dma("sp", invf, c_invf[:, :], writes=[bC])
        load_const_bf16(ident, c_ident[:, :], 128)
        load_const_bf16(negtri, c_negtri[:, :], 128)
        load_const_bf16(negones, c_negones[:, :], 128)
        cbi = AR.alloc([128, 4, 256], BF16); cbs = AR.alloc([128, 4, 256], BF16); wbm = AR.alloc([128, 8, 256], BF16)
        cmpb = AR.alloc([128, NOWN], BF16)
        Emat = AR.alloc([32, 16, 128], BF16)
        ovl = AR.alloc([128, 33], BF16)
        fb = AR.alloc([128, 8, 32], F32); vm = AR.alloc([128, 8, 32], F32)
        load_const_bf16(cbi.rearrange("p a b -> p (a b)"), c_cbi[:, :], 1024)
        load_const_bf16(cbs.rearrange("p a b -> p (a b)"), c_cbs[:, :], 1024)
        load_const_bf16(wbm.rearrange("p a b -> p (a b)"), c_wb[:, :], 2048)
        load_const_bf16(cmpb, c_cmpb[:, :], 1024)
        load_const_bf16(Emat.rearrange("p a b -> p (a b)"), c_E[:, :], 2048)
        load_const_bf16(ovl, c_ovl[:, :], 33)
        S.dma("sp", fb.rearrange("p a b -> p (a b)"), c_fb[:, :], writes=[bC])
        S.dma("sp", vm.rearrange("p a b -> p (a b)"), c_vm[:, :], writes=[bC])
        S.op("pool", lambda e: e.memset(stats, 0.0), writes=[B("stats")])
        S.op("pool", lambda e: e.memset(epsc, 1e-6), writes=[bC])
        gb = stage_c
        KT_slc = AR.alloc([128, 2, T], BF16)
        KT_win = AR.alloc([128, 2, T], BF16)
        Vx_slc = AR.alloc([128, 16, 2, 130], BF16)
        Vx_win = AR.alloc([128, 16, 2, 130], BF16)
        kcT = AR.alloc([128, 2, 128], BF16)
        vcx = AR.alloc([128, 2, 162], BF16)
        gates_sb = AR.alloc([128, 8, 24], F32)
        m_nsa = AR.mark()

        stat_col = [0]

        norm_cur = {}

        def norm_tiles(x_dram, ntiles, g_dram, sink, xin=None, tag="n"):
            mid_fn, end_fn = sink if isinstance(sink, tuple) else (sink, None)
            S.dma("sp", gb, g_dram[0:1, :].to_broadcast([128, D]), writes=[B("gb"), B("stage_c")])
            info = {}

            def s1(tt):
                sl = tt % 2
                if xin is None:
                    xs = tt % len(n_xt)
                    xt = n_xt[xs]; bx = B("n_xt", xs)
                    S.dma("sp", xt, x_dram[tt * 128:(tt + 1) * 128, :], writes=[bx])
                else:
                    xt, bx = xin(tt)
                col = stat_col[0] % 64
                stat_col[0] += 1
                c4 = col * 4
                bs = B("stats", col)
                info[tt] = (xt, bx, c4, bs, col)
                S.op("act", lambda e: e.activation(out=n_junk[sl], in_=xt, func=AF.Square, scale=float(D) ** -0.5, accum_out=stats[:, c4:c4 + 1]),
                     reads=[bx, B("stats")], writes=[B("n_junk", sl), bs])
                S.op("act", lambda e: e.activation(out=stats[:, c4 + 2:c4 + 3], in_=stats[:, c4:c4 + 1], func=AF.Sqrt, bias=epsc[:, 0:1]),
                     reads=[bs, bC], writes=[bs])

            def s2(tt):
                xt, bx, c4, bs, col = info[tt]
                sl = tt % 2
                S.op("dve", lambda e: e.reciprocal(out=stats[:, c4 + 3:c4 + 4], in_=stats[:, c4 + 2:c4 + 3]), reads=[bs], writes=[bs])
                hb = n_hb[sl]; bh = B("n_hb", sl)
                S.op("dve", lambda e: e.scalar_tensor_tensor(out=hb, in0=xt, scalar=stats[:, c4 + 3:c4 + 4], in1=gb, op0=ALU.mult, op1=ALU.mult),
                     reads=[bx, bs, B("gb")], writes=[bh])
                S.op("pool", lambda e: e.memset(stats[:, c4:c4 + 1], 0.0), reads=[bs], writes=[bs])
                norm_cur["c4"] = c4; norm_cur["col"] = col
                mid_fn(tt, hb, bh)

            s1(0)
            if ntiles > 1:
                s1(1)
            s2(0)
            for tt in range(ntiles):
                if tt + 2 < ntiles:
                    s1(tt + 2)
                if tt + 1 < ntiles:
                    s2(tt + 1)
                if end_fn is not None:
                    end_fn(tt)

        def transpose_pe(hb, bh, pbank):
            for half in range(2):
                pst = ps[pbank + half].bitcast(BF16)
                bp = PB[pbank + half]
                for k8 in range(8):
                    kc = half * 8 + k8
                    S.op("pe", lambda e: e.transpose(out=pst[:, k8 * 128:(k8 + 1) * 128], in_=hb[:, kc * 128:(kc + 1) * 128], identity=ident),
                         reads=[bh, bC], writes=[bp], signal=(k8 == 7))

        def transpose_evac(dstT, dst_bufs, tt, pbank):
            for half in range(2):
                pst = ps[pbank + half].bitcast(BF16)
                bp = PB[pbank + half]
                dst = dstT[:, half * 8:(half + 1) * 8, tt * 128:(tt + 1) * 128]
                src = pst[:, 0:1024].rearrange("p (k t) -> p k t", k=8)
                if half == 0:
                    S.op("act", lambda e: e.activation(out=dst, in_=src, func=AF.Copy), reads=[bp], writes=dst_bufs)
                else:
                    S.op("dve", lambda e: e.tensor_copy(out=dst, in_=src), reads=[bp], writes=dst_bufs)

        def transpose_to(hb, bh, dstT, dst_bufs, tt, pbank):
            transpose_pe(hb, bh, pbank)
            transpose_evac(dstT, dst_bufs, tt, pbank)

        def mm_transpose_pe(hb, bh, pbank):
            for q in range(4):
                bp = PB[pbank + q]
                for k4 in range(4):
                    kc = q * 4 + k4
                    S.op("pe", lambda e: e.matmul(out=ps[pbank + q][:, k4 * 128:(k4 + 1) * 128], lhsT=hb[:, kc * 128:(kc + 1) * 128], rhs=ident,
                                                  start=True, stop=True),
                         reads=[bh, bC], writes=[bp], signal=(k4 == 3))

        def mm_transpose_evac(dstT, dst_bufs, tt, pbank):
            for q in range(4):
                bp = PB[pbank + q]
                dst = dstT[:, q * 4:(q + 1) * 4, tt * 128:(tt + 1) * 128]
                src = ps[pbank + q][:, :].rearrange("p (k t) -> p k t", k=4)
                if q % 2 == 0:
                    S.op("act", lambda e: e.activation(out=dst, in_=src, func=AF.Copy), reads=[bp], writes=dst_bufs)
                else:
                    S.op("dve", lambda e: e.tensor_copy(out=dst, in_=src), reads=[bp], writes=dst_bufs)

        def tsink(dstT, dbuf):
            return (lambda tt, hb, bh: mm_transpose_pe(hb, bh, (tt % 2) * 4),
                    lambda tt: mm_transpose_evac(dstT, [dbuf], tt, (tt % 2) * 4))

        class Ring:
            def __init__(self, name, nslots, shape, top=False):
                self.t = [AR.alloc(shape, BF16, top=top) for _ in range(nslots)]
                self.b = [B(name, i) for i in range(nslots)]
                self.i = 0
                self.n = nslots

            def load(self, src_aps):
                t = self.t[self.i]; b = self.b[self.i]
                self.i = (self.i + 1) % self.n
                for dv, sa in src_aps:
                    S.dma("pool", dv(t), sa, writes=[b])
                return t, b

        def wsrc(w, r0, nrows, c0, ncols):
            return w[r0:r0 + nrows, c0:c0 + ncols].rearrange("(k p) c -> p k c", p=128)

        hT_nat = AR.alloc([128, 16, T], BF16)
        m_A = AR.mark()
        hT_own = AR.alloc([128, 16, NOWN], BF16)
        m_A2 = AR.mark()
        n_xt = [AR.alloc([128, D], F32) for _ in range(3)]
        n_junk = [AR.alloc([128, D], BF16) for _ in range(2)]
        n_hb = [AR.alloc([128, D], BF16) for _ in range(2)]
        bhTn = B("hT_nat"); bhTo = B("hT_own")

        norm_tiles(x_own, 8, g_mix, tsink(hT_own, bhTo))
        norm_tiles(x_nat, 16, g_mix, tsink(hT_nat, bhTn))
        S.dma("sp", scr_hTown[:, :, :], hT_own, reads=[bhTo], writes=[B("scr_hTown")], owner=B("scr_hTown"))
        tap("hT_own", hT_own, [bhTo])
        if stop_after <= 0:
            S.emit(); return nc
        S.barrier()
        AR.release(m_A2)

        scr_qn = nc.dram_tensor("scr_qn", [8, 128, NOWN], BF16, kind="Internal").ap()
        RC = 256
        state = {}

        def alloc_proj_tmps():
            state["posi"] = AR.alloc([32, RC], I32); state["posf"] = AR.alloc([32, RC], F32)
            state["tmpa"] = AR.alloc([32, RC], F32); state["tmpi"] = AR.alloc([32, RC], I32); state["tmpm"] = AR.alloc([32, RC], F32)
            state["wring"] = Ring("wring", 2, [128, 16, 256])
            state["raw32"] = [AR.alloc([32, 512], F32) for _ in range(2)]
            state["ropt1"] = [AR.alloc([32, 512], F32) for _ in range(2)]
            state["ropt2"] = [AR.alloc([32, 512], F32) for _ in range(2)]
            state["stg"] = [AR.alloc([128, 512], BF16) for _ in range(3)]

        def rope_tables(pos_dram, n, cs):
            bp = B("ropetmp")
            posi, posf, tmpa, tmpi, tmpm = (state[k] for k in ("posi", "posf", "tmpa", "tmpi", "tmpm"))
            for c0 in range(0, n, RC):
                S.dma("sp", posi, pos_dram[0:1, c0:c0 + RC].to_broadcast([32, RC]), writes=[bp])
                S.op("dve", lambda e: e.tensor_copy(out=posf, in_=posi), reads=[bp], writes=[bp])
                for which in range(2):
                    S.op("dve", lambda e, which=which: e.tensor_scalar(out=tmpa, in0=posf, scalar1=invf[:, 0:1],
                                                                      scalar2=(0.25 if which == 0 else 0.0), op0=ALU.mult, op1=ALU.add),
                         reads=[bp, bC], writes=[bp])
                    S.op("dve", lambda e: e.tensor_copy(out=tmpi, in_=tmpa), reads=[bp], writes=[bp])
                    S.op("dve", lambda e: e.tensor_copy(out=tmpm, in_=tmpi), reads=[bp], writes=[bp])
                    S.op("dve", lambda e: e.tensor_sub(out=tmpa, in0=tmpa, in1=tmpm), reads=[bp], writes=[bp])
                    S.op("dve", lambda e: e.tensor_scalar(out=tmpm, in0=tmpa, scalar1=0.5, scalar2=None, op0=ALU.is_gt), reads=[bp], writes=[bp])
                    S.op("dve", lambda e: e.tensor_sub(out=tmpa, in0=tmpa, in1=tmpm), reads=[bp], writes=[bp])
                    S.op("dve", lambda e: e.tensor_scalar(out=tmpm, in0=tmpa, scalar1=-0.5, scalar2=None, op0=ALU.is_lt), reads=[bp], writes=[bp])
                    S.op("dve", lambda e: e.tensor_add(out=tmpa, in0=tmpa, in1=tmpm), reads=[bp], writes=[bp])
                    S.op("act", lambda e, which=which, c0=c0: e.activation(out=cs[:, which, c0:c0 + RC], in_=tmpa, func=AF.Sin, scale=2.0 * np.pi),
                         reads=[bp], writes=[B("cs")])

        pbi = [0]

        def next_bank(lo, n):
            i = lo + (pbi[0] % n)
            pbi[0] += 1
            return i

        rope_i = [0]
        stg_i = [0]

        def evac_rope(pp, bp, dst, dbufs, cs, t0):
            raw32, ropt1, ropt2 = state["raw32"], state["ropt1"], state["ropt2"]
            sl = rope_i[0] % 2
            rope_i[0] += 1
            S.op("act", lambda e: e.activation(out=dst, in_=pp, func=AF.Copy), reads=[bp], writes=dbufs)
            S.op("dve", lambda e: e.tensor_copy(out=raw32[sl], in_=pp[0:32, :]), reads=[bp], writes=[B("raw32", sl)])
            rb = 6 + sl
            S.op("pe", lambda e: e.matmul(out=ps[rb][0:32, :], lhsT=r32, rhs=raw32[sl], start=True, stop=True),
                 reads=[B("raw32", sl), bC], writes=[PB[rb]])
            S.op("dve", lambda e: e.tensor_tensor(out=ropt1[sl], in0=raw32[sl], in1=cs[:, 0, t0:t0 + 512], op=ALU.mult),
                 reads=[B("raw32", sl), B("cs")], writes=[B("ropt1", sl)])
            S.op("dve", lambda e: e.tensor_tensor(out=ropt2[sl], in0=ps[rb][0:32, :], in1=cs[:, 1, t0:t0 + 512], op=ALU.mult),
                 reads=[PB[rb], B("cs")], writes=[B("ropt2", sl)])
            S.op("dve", lambda e: e.tensor_tensor(out=dst[0:32, :], in0=ropt1[sl], in1=ropt2[sl], op=ALU.add),
                 reads=[B("ropt1", sl), B("ropt2", sl)], writes=dbufs)

        def proj_fm(hT, bhT, ntok, col0, ncols, evac):
            wring = state["wring"]
            pending = None
            for g0 in range(0, ncols, 256):
                gw = min(256, ncols - g0)
                wt, wbuf = wring.load([(lambda t, gw=gw: t[:, :, 0:gw], wsrc(w_in, 0, D, col0 + g0, gw))])
                for cc in range(gw // 128):
                    for tc in range(ntok // 512):
                        bk = next_bank(0, 4)
                        for kc in range(16):
                            S.op("pe", lambda e, bk=bk, wt=wt, cc=cc, kc=kc, tc=tc: e.matmul(
                                out=ps[bk][:, :], lhsT=wt[:, kc, cc * 128:(cc + 1) * 128], rhs=hT[:, kc, tc * 512:(tc + 1) * 512],
                                start=(kc == 0), stop=(kc == 15)),
                                reads=[wbuf, bhT], writes=[PB[bk]], signal=(kc == 15))
                        if pending is not None:
                            evac(*pending)
                        pending = (g0 // 128 + cc, tc, ps[bk][:, :], PB[bk])
            if pending is not None:
                evac(*pending)

        def proj_tm(hT, bhT, ntiles, col0, ncols, evac):
            wring = state["wring"]
            for g0 in range(0, ncols, 256):
                gw = min(256, ncols - g0)
                wt, wbuf = wring.load([(lambda t, gw=gw: t[:, :, 0:gw], wsrc(w_in, 0, D, col0 + g0, gw))])
                for tt in range(ntiles):
                    bk = next_bank(0, 4)
                    for kc in range(16):
                        S.op("pe", lambda e, bk=bk, wt=wt, kc=kc, tt=tt, gw=gw: e.matmul(
                            out=ps[bk][:, 0:gw], lhsT=hT[:, kc, tt * 128:(tt + 1) * 128], rhs=wt[:, kc, 0:gw],
                            start=(kc == 0), stop=(kc == 15)),
                            reads=[wbuf, bhT], writes=[PB[bk]], signal=(kc == 15))
                    evac(tt, g0, gw, ps[bk][:, :], PB[bk])

        def stage_slot():
            sl = stg_i[0] % 3
            stg_i[0] += 1
            return sl

        def spill_evac(dram_fn):
            def f(c, tc, pp, bp):
                stg = state["stg"]
                sl = stage_slot()
                if sl % 2:
                    S.op("act", lambda e: e.activation(out=stg[sl], in_=pp, func=AF.Copy), reads=[bp], writes=[B("stg", sl)])
                else:
                    S.op("dve", lambda e: e.tensor_copy(out=stg[sl], in_=pp), reads=[bp], writes=[B("stg", sl)])
                dst, dbuf = dram_fn(c, tc)
                S.dma("sp", dst, stg[sl], reads=[B("stg", sl)], writes=[dbuf], owner=B("stg", sl))
            return f

        def rope_spill_evac(dram_fn, cs):
            def f(c, tc, pp, bp):
                stg = state["stg"]
                sl = stage_slot()
                evac_rope(pp, bp, stg[sl], [B("stg", sl)], cs, tc * 512)
                dst, dbuf = dram_fn(c, tc)
                S.dma("sp", dst, stg[sl], reads=[B("stg", sl)], writes=[dbuf], owner=B("stg", sl))
            return f

        cs_own = AR.alloc([32, 2, NOWN], F32)
        alloc_proj_tmps()
        rope_tables(pos_own, NOWN, cs_own)
        proj_fm(hT_own, bhTo, NOWN, C_QN, 1024,
                rope_spill_evac(lambda c, tc: (scr_qn[c, :, tc * 512:(tc + 1) * 512], B("scr_qn")), cs_own))
        proj_fm(hT_own, bhTo, NOWN, C_QS, 1024,
                spill_evac(lambda c, tc: (scr_qsb[c, :, tc * 512:(tc + 1) * 512], B("scr_qsb"))))
        proj_tm(hT_own, bhTo, 8, C_G, 24,
                lambda tt, g0, gw, pp, bp: S.op("act", lambda e: e.activation(out=gates_sb[:, tt, :], in_=pp[:, 0:24], func=AF.Sigmoid),
                                               reads=[bp], writes=[B("gates")]))
        tap("gates", gates_sb, [B("gates")])
        if stop_after <= 1:
            S.emit(); return nc
        S.barrier()
        AR.release(m_A)

        cs_nat = AR.alloc([32, 2, T], F32)
        alloc_proj_tmps()
        rope_tables(pos_nat, T, cs_nat)
        proj_fm(hT_nat, bhTn, T, C_KSB, 1024,
                spill_evac(lambda c, tc: (scr_ksb[c, :, tc * 512:(tc + 1) * 512], B("scr_ksb"))))

        def vsb_evac(tt, g0, gw, pp, bp):
            stg = state["stg"]
            sl = stage_slot()
            S.op("dve", lambda e: e.tensor_copy(out=stg[sl][:, 0:gw], in_=pp[:, 0:gw]), reads=[bp], writes=[B("stg", sl)])
            S.dma("sp", scr_vsb[:, tt, g0:g0 + gw], stg[sl][:, 0:gw], reads=[B("stg", sl)], writes=[B("scr_vsb")], owner=B("stg", sl))
        proj_tm(hT_nat, bhTn, 16, C_VSB, 1024, vsb_evac)
        if stop_after <= 1.5:
            S.emit(); return nc

        tokT = AR.alloc([128, 2, T], BF16)
        w1c = AR.alloc([128, 32, 128], BF16)
        w2c = AR.alloc([128, 128], BF16)
        peT = AR.alloc([128, 32], BF16)
        pe_tm = AR.alloc([32, 128], BF16)
        cbias = AR.alloc([128, 2], F32)
        hidT = AR.alloc([128, 128], BF16)

        S.op("pool", lambda e: e.memset(Vx_slc.rearrange("p a b c -> p (a b c)"), 1.0), writes=[B("Vx_slc")])
        S.op("pool", lambda e: e.memset(Vx_win.rearrange("p a b c -> p (a b c)"), 1.0), writes=[B("Vx_win")])
        S.op("pool", lambda e: e.memset(kcT.rearrange("p a b -> p (a b)"), 0.0), writes=[B("kcT")])
        S.op("pool", lambda e: e.memset(vcx.rearrange("p a b -> p (a b)"), 0.0), writes=[B("vcx")])
        proj_fm(hT_nat, bhTn, T, C_KS, 256,
                lambda c, tc, pp, bp: evac_rope(pp, bp, KT_slc[:, c, tc * 512:(tc + 1) * 512], [B("KT_slc")], cs_nat, tc * 512))
        proj_fm(hT_nat, bhTn, T, C_KW, 256,
                lambda c, tc, pp, bp: evac_rope(pp, bp, KT_win[:, c, tc * 512:(tc + 1) * 512], [B("KT_win")], cs_nat, tc * 512))

        def v_evac(Vx, vb):
            def f(tt, g0, gw, pp, bp):
                S.op("dve", lambda e: e.tensor_copy(out=Vx[:, tt, :, 0:128], in_=pp[:, 0:256].rearrange("p (h d) -> p h d", h=2)),
                     reads=[bp], writes=[vb])
            return f
        proj_tm(hT_nat, bhTn, 16, C_VS, 256, v_evac(Vx_slc, B("Vx_slc")))
        proj_tm(hT_nat, bhTn, 16, C_VW, 256, v_evac(Vx_win, B("Vx_win")))

        bcw = B("cmpw")
        btok = B("tokT")
        for kv in range(2):
            if kv == 0:
                proj_fm(hT_nat, bhTn, T, C_KC, 256,
                        lambda c, tc, pp, bp: evac_rope(pp, bp, tokT[:, c, tc * 512:(tc + 1) * 512], [btok], cs_nat, tc * 512))
            else:
                proj_fm(hT_nat, bhTn, T, C_VC, 256,
                        lambda c, tc, pp, bp: S.op("act", lambda e: e.activation(out=tokT[:, c, tc * 512:(tc + 1) * 512], in_=pp, func=AF.Copy),
                                                   reads=[bp], writes=[btok]))
            w1d, w2d, ped = ((cmp_w1_k, cmp_w2_k, cmp_pe_k), (cmp_w1_v, cmp_w2_v, cmp_pe_v))[kv]
            S.dma("pool", w1c, w1d.rearrange("(l p) f -> p l f", p=128), writes=[bcw])
            S.dma("pool", w2c, w2d[:, :], writes=[bcw])
            S.dma("pool", pe_tm, ped[:, :], writes=[bcw])
            pst = ps[4].bitcast(BF16)
            S.op("pe", lambda e, pst=pst: e.transpose(out=pst[:, 0:32], in_=pe_tm, identity=ident[0:32, 0:32]),
                 reads=[bcw, bC], writes=[PB[4]])
            S.op("dve", lambda e, pst=pst: e.tensor_copy(out=peT, in_=pst[:, 0:32]), reads=[PB[4]], writes=[B("peT")])
            for l in range(32):
                S.op("pe", lambda e, l=l: e.matmul(out=ps[5][:, 0:1], lhsT=w1c[:, l, :], rhs=peT[:, l:l + 1], start=(l == 0), stop=(l == 31)),
                     reads=[bcw, B("peT")], writes=[PB[5]], signal=(l == 31))
            S.op("dve", lambda e, kv=kv: e.tensor_copy(out=cbias[:, kv:kv + 1], in_=ps[5][:, 0:1]), reads=[PB[5]], writes=[B("cbias")])
            for hh in range(2):
                bk = next_bank(0, 4)
                for l in range(32):
                    S.op("pe", lambda e, bk=bk, l=l, hh=hh: e.matmul(
                        out=ps[bk][:, 0:127], lhsT=w1c[:, l, :], rhs=tokT[:, hh, l:l + 16 * 126 + 1:16], start=(l == 0), stop=(l == 31)),
                        reads=[bcw, btok], writes=[PB[bk]], signal=(l == 31))
                S.op("act", lambda e, bk=bk, kv=kv: e.activation(out=hidT[:, 0:127], in_=ps[bk][:, 0:127], func=AF.Silu, bias=cbias[:, kv:kv + 1]),
                     reads=[PB[bk], B("cbias")], writes=[B("hidT")])
                bk2 = next_bank(0, 4)
                if kv == 0:
                    S.op("pe", lambda e, bk2=bk2: e.matmul(out=ps[bk2][:, 0:127], lhsT=w2c, rhs=hidT[:, 0:127], start=True, stop=True),
                         reads=[bcw, B("hidT")], writes=[PB[bk2]])
                    S.op("dve", lambda e, bk2=bk2, hh=hh: e.tensor_copy(out=kcT[:, hh, 0:127], in_=ps[bk2][:, 0:127]), reads=[PB[bk2]], writes=[B("kcT")])
                else:
                    S.op("pe", lambda e, bk2=bk2: e.matmul(out=ps[bk2][0:127, 0:128], lhsT=hidT[:, 0:127], rhs=w2c, start=True, stop=True),
                         reads=[bcw, B("hidT")], writes=[PB[bk2]])
                    S.op("dve", lambda e, bk2=bk2, hh=hh: e.tensor_copy(out=vcx[0:127, hh, 0:128], in_=ps[bk2][0:127, 0:128]),
                         reads=[PB[bk2]], writes=[B("vcx")])
        for hh in range(2):
            S.op("dve", lambda e, hh=hh: e.tensor_copy(out=vcx[:, hh, 128:161], in_=ovl), reads=[bC], writes=[B("vcx")])
        tap("kcT", kcT, [B("kcT")]); tap("vcx", vcx, [B("vcx")]); tap("KT_slc", KT_slc, [B("KT_slc")]); tap("Vx_slc", Vx_slc, [B("Vx_slc")])
        if stop_after <= 2:
            S.emit(); return nc
        S.barrier()
        AR.release(m_nsa)

        oT_nsa = AR.alloc([128, 8, NOWN], BF16, top=True)
        oT_sb = AR.alloc([128, 8, NOWN], BF16, top=True)
        QT_nsa = AR.alloc([128, 8, NOWN], BF16)
        for hq in range(8):
            S.dma("sp", QT_nsa[:, hq, :], scr_qn[hq, :, :], reads=[B("scr_qn")], writes=[B("QT_nsa", hq)])
        tap("QT_nsa", QT_nsa, [B("QT_nsa", c) for c in range(8)])
        o_acc = AR.alloc([128, 8, 4, 128], F32)
        o_bf = AR.alloc([128, 8, 4, 128], BF16)
        imp = AR.alloc([128, 8, 32], F32)
        imp_tmp = AR.alloc([128, 4, 32], F32)
        sc = AR.alloc([128, 8, 64], F32)
        m8 = AR.alloc([128, 8, 16], F32)
        sel = AR.alloc([128, 8, 32], F32)
        mbT = AR.alloc([32, NOWN], BF16)
        coef = AR.alloc([128, 512], F32)
        eT = [AR.alloc([128, 512], BF16) for _ in range(2)]
        PT = [AR.alloc([128, 2, 256], BF16) for _ in range(3)]
        coef_i = [0]

        def coef_slot():
            i = coef_i[0] % 128
            coef_i[0] += 1
            return i * 4, B("coef", i)

        pt_i = [0]
        for kvh in range(2):
            for g in range(4):
                head = kvh * 4 + g
                for tc in range(2):
                    bk = next_bank(0, 3)
                    S.op("pe", lambda e: e.matmul(out=ps[bk][:, :], lhsT=kcT[:, kvh, :], rhs=QT_nsa[:, head, tc * 512:(tc + 1) * 512], start=True, stop=False),
                         reads=[B("kcT"), B("QT_nsa", head)], writes=[PB[bk]], signal=False)
                    S.op("pe", lambda e: e.matmul(out=ps[bk][:, :], lhsT=ident, rhs=cmpb[:, tc * 512:(tc + 1) * 512], start=False, stop=True),
                         reads=[bC], writes=[PB[bk]])
                    sl = pt_i[0] % 2
                    pt_i[0] += 1
                    S.op("act", lambda e: e.activation(out=eT[sl], in_=ps[bk][:, :], func=AF.Exp, scale=SCALE),
                         reads=[PB[bk]], writes=[B("eT", sl)])
                    par = (g * 2 + tc) % 2
                    bo_o = 3 + par
                    bo_i = 5 + par
                    for j in range(4):
                        S.op("pe", lambda e: e.matmul(out=ps[bo_o][:, j * 128:(j + 1) * 128], lhsT=eT[sl][:, j * 128:(j + 1) * 128], rhs=vcx[:, kvh, 0:128],
                                                      start=True, stop=True),
                             reads=[B("eT", sl), B("vcx")], writes=[PB[bo_o]], signal=(j == 3))
                    for j in range(4):
                        S.op("pe", lambda e: e.matmul(out=ps[bo_i][:, j * 33:(j + 1) * 33], lhsT=eT[sl][:, j * 128:(j + 1) * 128], rhs=vcx[:, kvh, 128:161],
                                                      start=True, stop=True),
                             reads=[B("eT", sl), B("vcx")], writes=[PB[bo_i]], signal=(j == 3))
                    ca, bca = coef_slot(); cb_, bcb = coef_slot(); cg_, bcg = coef_slot()
                    pi3 = ps[bo_i][:, 0:132].rearrange("p (j c) -> p j c", j=4)
                    po3 = ps[bo_o][:, :].rearrange("p (j c) -> p j c", j=4)
                    tis = list(range(tc * 4, tc * 4 + 4))
                    S.op("dve", lambda e: e.tensor_scalar(out=coef[:, ca:ca + 4], in0=pi3[:, :, 32], scalar1=1e-30, scalar2=None, op0=ALU.max),
                         reads=[PB[bo_i]], writes=[bca])
                    S.op("dve", lambda e: e.reciprocal(out=coef[:, cb_:cb_ + 4], in_=coef[:, ca:ca + 4]), reads=[bca], writes=[bcb])
                    S.op("dve", lambda e: e.tensor_tensor(out=coef[:, cg_:cg_ + 4], in0=coef[:, cb_:cb_ + 4], in1=gates_sb[:, tc * 4:(tc + 1) * 4, head * 3],
                                                          op=ALU.mult), reads=[bcb, B("gates")], writes=[bcg])
                    bimps = [B("imp", ti) for ti in tis]
                    rden_b = coef[:, cb_:cb_ + 4].unsqueeze(2).to_broadcast([128, 4, 32])
                    if g == 0:
                        S.op("dve", lambda e: e.tensor_tensor(out=imp[:, tc * 4:(tc + 1) * 4, :], in0=pi3[:, :, 0:32], in1=rden_b, op=ALU.mult),
                             reads=[PB[bo_i], bcb], writes=bimps)
                    else:
                        S.op("dve", lambda e: e.tensor_tensor(out=imp_tmp, in0=pi3[:, :, 0:32], in1=rden_b, op=ALU.mult),
                             reads=[PB[bo_i], bcb], writes=[B("imp_tmp")])
                        S.op("dve", lambda e: e.tensor_tensor(out=imp[:, tc * 4:(tc + 1) * 4, :], in0=imp[:, tc * 4:(tc + 1) * 4, :], in1=imp_tmp, op=ALU.add),
                             reads=[B("imp_tmp")] + bimps, writes=bimps)
                    cg_b = coef[:, cg_:cg_ + 4].unsqueeze(2).to_broadcast([128, 4, 128])
                    S.op("dve", lambda e: e.tensor_tensor(out=o_acc[:, tc * 4:(tc + 1) * 4, g, :], in0=po3, in1=cg_b, op=ALU.mult),
                         reads=[PB[bo_o], bcg], writes=[B("o_acc", ti, g) for ti in tis])
            for ti in range(8):
                bimp = B("imp", ti)
                S.op("dve", lambda e, ti=ti: e.tensor_tensor(out=sc[:, ti, 0:32], in0=imp[:, ti, :], in1=fb[:, ti, :], op=ALU.add),
                     reads=[bimp, bC], writes=[B("sc", ti)])
                S.op("dve", lambda e, ti=ti: e.max(out=m8[:, ti, 0:8], in_=sc[:, ti, 0:32]), reads=[B("sc", ti)], writes=[B("m8", ti)])
                S.op("dve", lambda e, ti=ti: e.match_replace(out=sc[:, ti, 32:64], in_to_replace=m8[:, ti, 0:8], in_values=sc[:, ti, 0:32], imm_value=-3e9),
                     reads=[B("sc", ti), B("m8", ti)], writes=[B("sc2", ti)])
                S.op("dve", lambda e, ti=ti: e.max(out=m8[:, ti, 8:16], in_=sc[:, ti, 32:64]), reads=[B("sc2", ti)], writes=[B("m8", ti)])
                S.op("dve", lambda e, ti=ti: e.tensor_scalar(out=sel[:, ti, :], in0=sc[:, ti, 0:32], scalar1=m8[:, ti, 15:16], scalar2=None, op0=ALU.is_ge),
                     reads=[B("sc", ti), B("m8", ti)], writes=[B("sel", ti)])
                S.op("dve", lambda e, ti=ti: e.tensor_tensor(out=sel[:, ti, :], in0=sel[:, ti, :], in1=vm[:, ti, :], op=ALU.mult),
                     reads=[B("sel", ti), bC], writes=[B("sel", ti)])
                bk = next_bank(0, 3)
                S.op("pe", lambda e, bk=bk, ti=ti: e.transpose(out=ps[bk][0:32, 0:128], in_=sel[:, ti, :], identity=identf),
                     reads=[B("sel", ti), bC], writes=[PB[bk]])
                S.op("dve", lambda e, bk=bk, ti=ti: e.tensor_scalar(out=mbT[:, ti * 128:(ti + 1) * 128], in0=ps[bk][0:32, 0:128], scalar1=-NEG, scalar2=NEG,
                                                                   op0=ALU.mult, op1=ALU.add),
                     reads=[PB[bk]], writes=[B("mbT")])
            if kvh == 0:
                tap("imp", imp, [B("imp", ti) for ti in range(8)])
                tap("sel", sel, [B("sel", ti) for ti in range(8)])
                if stop_after <= 2.5:
                    S.emit(); return nc
            items = []
            seq = 0
            for g in range(4):
                for p in range(4):
                    for branch in range(2):
                        tiles = list(range(0, 4 * p + 4)) if branch == 0 else list(range(max(0, 4 * p - 4), 4 * p + 4))
                        npairs = len(tiles) // 2
                        for m in range(npairs):
                            items.append(dict(g=g, p=p, branch=branch, tiles=tiles, m=m, npairs=npairs, seq=seq))
                        seq += 1

            def sel_front(it, i):
                g, p, branch, tiles, m = it["g"], it["p"], it["branch"], it["tiles"], it["m"]
                head = kvh * 4 + g
                KT = KT_slc if branch == 0 else KT_win
                bKT = B("KT_slc") if branch == 0 else B("KT_win")
                bk = i % 3
                sl = i % 3
                for s in range(2):
                    G = tiles[2 * m + s]
                    o_ap = ps[bk][:, s * 256:(s + 1) * 256]
                    extra = []
                    if branch == 0:
                        extra.append((Emat[:, G, :], mbT[:, p * 256:(p + 1) * 256], [bC, B("mbT")]))
                        if G >= 4 * p:
                            extra.append((ident, cbi[:, G - 4 * p, :], [bC]))
                    else:
                        extra.append((ident, wbm[:, G - (4 * p - 4), :], [bC]))
                    S.op("pe", lambda e: e.matmul(out=o_ap, lhsT=KT[:, kvh, G * 128:(G + 1) * 128], rhs=QT_nsa[:, head, p * 256:(p + 1) * 256],
                                                  start=True, stop=False),
                         reads=[bKT, B("QT_nsa", head)], writes=[PB[bk]], signal=False)
                    for xi, (l_ap, r_ap, rb) in enumerate(extra):
                        last = xi == len(extra) - 1
                        S.op("pe", lambda e: e.matmul(out=o_ap, lhsT=l_ap, rhs=r_ap, start=False, stop=last),
                             reads=rb, writes=[PB[bk]], signal=(last and s == 1))

            def sel_exp(it, i):
                bk = i % 3
                sl = i % 3
                S.op("act", lambda e: e.activation(out=PT[sl].rearrange("p a b -> p (a b)"), in_=ps[bk][:, :], func=AF.Exp, scale=SCALE),
                     reads=[PB[bk]], writes=[B("PT", sl)])

            def sel_back(it, i):
                g, p, branch, tiles, m, npairs = it["g"], it["p"], it["branch"], it["tiles"], it["m"], it["npairs"]
                head = kvh * 4 + g
                Vx = Vx_slc if branch == 0 else Vx_win
                bVx = B("Vx_slc") if branch == 0 else B("Vx_win")
                sl = i % 3
                bo0 = 3 + 2 * (it["seq"] % 2)
                for s in range(2):
                    G = tiles[2 * m + s]
                    for j in range(2):
                        first = (m == 0 and s == 0)
                        last = (m == npairs - 1 and s == 1)
                        bo = bo0 + j
                        S.op("pe", lambda e: e.matmul(out=ps[bo][:, 0:129], lhsT=PT[sl][:, s, j * 128:(j + 1) * 128], rhs=Vx[:, G, kvh, 0:129],
                                                      start=first, stop=last),
                             reads=[B("PT", sl), bVx], writes=[PB[bo]], signal=last)
                if m != npairs - 1:
                    return
                for j in range(2):
                    ti = 2 * p + j
                    bo = bo0 + j
                    c0, bc = coef_slot()
                    S.op("dve", lambda e: e.tensor_scalar(out=coef[:, c0:c0 + 1], in0=ps[bo][:, 128:129], scalar1=1e-30,
                                                          scalar2=None, op0=ALU.max), reads=[PB[bo]], writes=[bc])
                    S.op("dve", lambda e: e.reciprocal(out=coef[:, c0 + 1:c0 + 2], in_=coef[:, c0:c0 + 1]), reads=[bc], writes=[bc])
                    S.op("dve", lambda e: e.tensor_tensor(out=coef[:, c0 + 2:c0 + 3], in0=coef[:, c0 + 1:c0 + 2],
                                                          in1=gates_sb[:, ti, head * 3 + 1 + branch:head * 3 + 2 + branch], op=ALU.mult),
                         reads=[bc, B("gates")], writes=[bc])
                    dst = o_acc[:, ti, g, :] if branch == 0 else o_bf[:, ti, g, :]
                    dbuf = B("o_acc", ti, g) if branch == 0 else B("o_bf", ti, g)
                    S.op("dve", lambda e: e.scalar_tensor_tensor(out=dst, in0=ps[bo][:, 0:128], scalar=coef[:, c0 + 2:c0 + 3],
                                                                 in1=o_acc[:, ti, g, :], op0=ALU.mult, op1=ALU.add),
                         reads=[PB[bo], bc, B("o_acc", ti, g)], writes=[dbuf])
                if p == 3 and branch == 1:
                    for half in range(2):
                        bkt = 7
                        pst = ps[bkt].bitcast(BF16)
                        for j in range(4):
                            ti = half * 4 + j
                            S.op("pe", lambda e: e.transpose(out=pst[:, j * 128:(j + 1) * 128], in_=o_bf[:, ti, g, :], identity=ident),
                                 reads=[B("o_bf", ti, g), bC], writes=[PB[bkt]], signal=(j == 3))
                        S.op("act", lambda e: e.activation(out=oT_nsa[:, head, half * 512:(half + 1) * 512], in_=pst[:, 0:512], func=AF.Copy),
                             reads=[PB[bkt]], writes=[B("oT_nsa", head)])

            sel_front(items[0], 0)
            sel_front(items[1], 1)
            sel_exp(items[0], 0)
            for i, it in enumerate(items):
                if i + 2 < len(items):
                    sel_front(items[i + 2], i + 2)
                if i + 1 < len(items):
                    sel_exp(items[i + 1], i + 1)
                sel_back(it, i)
        tap("oT_nsa", oT_nsa, [B("oT_nsa", h) for h in range(8)])
        if stop_after <= 3:
            S.emit(); return nc
        S.barrier()
        AR.release(m_nsa)

        sbq = [AR.alloc([128, NOWN], BF16) for _ in range(2)]
        sbk = [AR.alloc([128, T], BF16) for _ in range(2)]
        sbv = [AR.alloc([128, 16, 128], BF16) for _ in range(2)]
        e_sb = [AR.alloc([128, 512], F32) for _ in range(2)]
        sp_sb = [AR.alloc([128, 512], F32) for _ in range(2)]
        spb = [AR.alloc([128, 2, 256], BF16) for _ in range(2)]
        t_sb = [AR.alloc([128, 2, 256], F32) for _ in range(2)]
        AT = [AR.alloc([128, 2, 256], BF16) for _ in range(2)]
        Rsum = AR.alloc([128, 256], F32)
        items = []
        for head in range(8):
            for p in range(4):
                npairs = 2 * p + 2
                for mi, m in enumerate(range(npairs - 1, -1, -1)):
                    items.append(dict(head=head, p=p, mi=mi, m=m, npairs=npairs))
        loaded = set()

        def sb_s1(it, i):
            head, p, mi, m = it["head"], it["p"], it["mi"], it["m"]
            hs = head % 2
            if head not in loaded:
                loaded.add(head)
                S.dma("sp", sbq[hs], scr_qsb[head, :, :], reads=[B("scr_qsb")], writes=[B("sbq", hs)])
                S.dma("sp", sbk[hs], scr_ksb[head, :, :], reads=[B("scr_ksb")], writes=[B("sbk", hs)])
                S.dma("sp", sbv[hs], scr_vsb[:, :, head * 128:(head + 1) * 128], reads=[B("scr_vsb")], writes=[B("sbv", hs)])
            bz = i % 3
            for s in range(2):
                G = 2 * m + s
                o_ap = ps[bz][:, s * 256:(s + 1) * 256]
                diag = G >= 4 * p
                S.op("pe", lambda e: e.matmul(out=o_ap, lhsT=sbk[hs][:, G * 128:(G + 1) * 128], rhs=sbq[hs][:, p * 256:(p + 1) * 256],
                                              start=True, stop=(not diag)),
                     reads=[B("sbk", hs), B("sbq", hs)], writes=[PB[bz]], signal=(s == 1 and not diag))
                if diag:
                    S.op("pe", lambda e: e.matmul(out=o_ap, lhsT=ident, rhs=cbs[:, G - 4 * p, :], start=False, stop=True),
                         reads=[bC], writes=[PB[bz]], signal=(s == 1))

        sb_dve_cast = False
        SB_BF16_SP = True
        sb_cast_pending = []

        def sb_s2(it, i):
            sl = i % 2
            bz = i % 3
            bcn = 3 + i % 2
            btot = 5
            S.op("act", lambda e: e.activation(out=e_sb[sl], in_=ps[bz][:, :], func=AF.Exp, scale=SCALE),
                 reads=[PB[bz]], writes=[B("e_sb", sl)])
            if SB_BF16_SP:
                S.op("act", lambda e: e.activation(out=spb[sl].rearrange("p a b -> p (a b)"), in_=e_sb[sl], func=AF.Ln, bias=1.0),
                     reads=[B("e_sb", sl)], writes=[B("spb", sl)])
                sb_s2b(it, i)
                return
            S.op("act", lambda e: e.activation(out=sp_sb[sl], in_=e_sb[sl], func=AF.Ln, bias=1.0),
                 reads=[B("e_sb", sl)], writes=[B("sp_sb", sl)])
            if not sb_dve_cast:
                S.op("act", lambda e: e.activation(out=spb[sl].rearrange("p a b -> p (a b)"), in_=e_sb[sl], func=AF.Ln, bias=1.0),
                     reads=[B("e_sb", sl)], writes=[B("spb", sl)])
            else:
                sb_cast_pending.append((sl, it, i))
                return
            sb_s2b(it, i)

        def sb_s2b(it, i):
            sl = i % 2
            bcn = 3 + i % 2
            btot = 5
            S.op("pe", lambda e: e.matmul(out=ps[bcn][:, 256:512], lhsT=negtri, rhs=spb[sl][:, 1, :], start=True, stop=True),
                 reads=[bC, B("spb", sl)], writes=[PB[bcn]], signal=False)
            S.op("pe", lambda e: e.matmul(out=ps[bcn][:, 0:256], lhsT=negtri, rhs=spb[sl][:, 0, :], start=True, stop=False),
                 reads=[bC, B("spb", sl)], writes=[PB[bcn]], signal=False)
            S.op("pe", lambda e: e.matmul(out=ps[bcn][:, 0:256], lhsT=negones, rhs=spb[sl][:, 1, :], start=False, stop=True),
                 reads=[bC, B("spb", sl)], writes=[PB[bcn]])
            if it["mi"] < it["npairs"] - 1:
                S.op("pe", lambda e: e.matmul(out=ps[btot][:, 0:256], lhsT=negones, rhs=spb[sl][:, 0, :], start=True, stop=False),
                     reads=[bC, B("spb", sl)], writes=[PB[btot]], signal=False)
                S.op("pe", lambda e: e.matmul(out=ps[btot][:, 0:256], lhsT=negones, rhs=spb[sl][:, 1, :], start=False, stop=True),
                     reads=[bC, B("spb", sl)], writes=[PB[btot]])

        s3_dve_done = set()

        def sb_s3_dve(it, i):
            if i in s3_dve_done:
                return
            s3_dve_done.add(i)
            sl = i % 2
            bz = i % 3
            bcn = 3 + i % 2
            tf = t_sb[sl].rearrange("p a b -> p (a b)")
            if SB_BF16_SP and it["mi"] == 0:
                spo, spbuf = spb[sl].rearrange("p a b -> p (a b)"), B("spb", sl)
            else:
                spo, spbuf = sp_sb[sl], B("sp_sb", sl)
            S.op("dve", lambda e: e.scalar_tensor_tensor(out=tf, in0=ps[bz][:, :], scalar=SCALE, in1=spo, op0=ALU.mult, op1=ALU.subtract),
                 reads=[PB[bz], spbuf], writes=[B("t_sb", sl)])
            S.op("dve", lambda e: e.tensor_tensor(out=tf, in0=tf, in1=ps[bcn][:, :], op=ALU.add),
                 reads=[PB[bcn], B("t_sb", sl)], writes=[B("t_sb", sl)])

        def sb_s3(it, i):
            head, p, mi, m, npairs = it["head"], it["p"], it["mi"], it["m"], it["npairs"]
            hs = head % 2
            sl = i % 2
            bo = 6 + (p % 2)
            tf = t_sb[sl].rearrange("p a b -> p (a b)")
            sb_s3_dve(it, i)
            S.op("act", lambda e: e.activation(out=AT[sl].rearrange("p a b -> p (a b)"), in_=tf, func=AF.Exp),
                 reads=[B("t_sb", sl)], writes=[B("AT", sl)])
            for s in range(2):
                G = 2 * m + s
                first = (mi == 0 and s == 0)
                last = (mi == npairs - 1 and s == 1)
                S.op("pe", lambda e: e.matmul(out=ps[bo][:, 0:256], lhsT=sbv[hs][:, G, :], rhs=AT[sl][:, s, :], start=first, stop=last),
                     reads=[B("sbv", hs), B("AT", sl)], writes=[PB[bo]], signal=last)
            if mi == npairs - 1:
                S.op("act", lambda e: e.activation(out=oT_sb[:, head, p * 256:(p + 1) * 256], in_=ps[bo][:, 0:256], func=AF.Copy),
                     reads=[PB[bo]], writes=[B("oT_sb", head)])

        n_it = len(items)
        sb_s1(items[0], 0)
        sb_s1(items[1], 1)
        sb_s2(items[0], 0)
        if sb_dve_cast:
            slc, itc, ic = sb_cast_pending.pop()
            S.op("dve", lambda e: e.tensor_copy(out=spb[slc].rearrange("p a b -> p (a b)"), in_=sp_sb[slc]),
                 reads=[B("sp_sb", slc)], writes=[B("spb", slc)])
            sb_s2b(itc, ic)
        for i, it in enumerate(items):
            if it["mi"] == 0:
                S.op("dve", lambda e: e.tensor_copy(out=Rsum, in_=ps[5][:, 0:256]), reads=[PB[5]], writes=[B("Rsum")])
            elif it["mi"] < it["npairs"] - 1:
                S.op("dve", lambda e: e.tensor_tensor(out=Rsum, in0=Rsum, in1=ps[5][:, 0:256], op=ALU.add),
                     reads=[PB[5], B("Rsum")], writes=[B("Rsum")])
            if i + 2 < n_it:
                sb_s1(items[i + 2], i + 2)
            if i + 1 < n_it:
                sb_s2(items[i + 1], i + 1)
                if sb_dve_cast:
                    sb_s3_dve(it, i)
                    slc, itc, ic = sb_cast_pending.pop()
                    S.op("dve", lambda e: e.tensor_copy(out=spb[slc].rearrange("p a b -> p (a b)"), in_=sp_sb[slc]),
                         reads=[B("sp_sb", slc)], writes=[B("spb", slc)])
                    sb_s2b(itc, ic)
                if items[i + 1]["mi"] > 0:
                    sn = (i + 1) % 2
                    if SB_BF16_SP:
                        S.op("pool", lambda e: e.tensor_tensor(out=sp_sb[sn].rearrange("p (a b) -> p a b", a=2), in0=spb[sn],
                                                               in1=Rsum.unsqueeze(1).to_broadcast([128, 2, 256]), op=ALU.subtract),
                             reads=[B("spb", sn), B("Rsum")], writes=[B("sp_sb", sn)])
                    else:
                        S.op("pool", lambda e: e.tensor_tensor(out=sp_sb[sn].rearrange("p (a b) -> p a b", a=2), in0=sp_sb[sn].rearrange("p (a b) -> p a b", a=2),
                                                               in1=Rsum.unsqueeze(1).to_broadcast([128, 2, 256]), op=ALU.subtract),
                             reads=[B("sp_sb", sn), B("Rsum")], writes=[B("sp_sb", sn)])
            sb_s3(it, i)
        tap("oT_sb", oT_sb, [B("oT_sb", h) for h in range(8)])
        if stop_after <= 4:
            S.emit(); return nc
        S.barrier()
        AR.release(m_ess)

        mT = AR.alloc([128, 16, NOWN], BF16, top=True)
        hT2 = AR.alloc([128, 16, NOWN], BF16)
        bhT2 = B("hT2")
        S.dma("sp", hT2, scr_hTown[:, :, :], reads=[B("scr_hTown")], writes=[bhT2])
        wg = Ring("wg", 4, [128, 16, 128])
        wb_ = Ring("wb", 4, [128, 8, 128])
        gsb = [AR.alloc([128, 512], F32) for _ in range(2)]
        ysb = [AR.alloc([128, 512], F32) for _ in range(2)]
        gi = [0]
        for fc in range(16):
            wga, bga = wg.load([(lambda t: t, wsrc(w_in, 0, D, C_GA + fc * 128, 128))])
            wgb, bgb = wg.load([(lambda t: t, wsrc(w_in, 0, D, C_GB + fc * 128, 128))])
            wba, bba = wb_.load([(lambda t: t, wsrc(w_br_nsa, 0, 1024, fc * 128, 128))])
            wbb, bbb = wb_.load([(lambda t: t, wsrc(w_br_sb, 0, 1024, fc * 128, 128))])
            for tc in range(2):
                tsl = slice(tc * 512, (tc + 1) * 512)
                res = []
                for (wgt, bwg, wbr, bwb, oT, obn) in ((wga, bga, wba, bba, oT_nsa, "oT_nsa"), (wgb, bgb, wbb, bbb, oT_sb, "oT_sb")):
                    bkg = next_bank(0, 4)
                    for kc in range(16):
                        S.op("pe", lambda e, bkg=bkg, wgt=wgt, kc=kc, tsl=tsl: e.matmul(out=ps[bkg][:, :], lhsT=wgt[:, kc, :], rhs=hT2[:, kc, tsl],
                                                                                    start=(kc == 0), stop=(kc == 15)),
                             reads=[bwg, bhT2], writes=[PB[bkg]], signal=(kc == 15))
                    sl = gi[0] % 2
                    S.op("act", lambda e, bkg=bkg, sl=sl: e.activation(out=gsb[sl], in_=ps[bkg][:, :], func=AF.Sigmoid), reads=[PB[bkg]], writes=[B("gsb", sl)])
                    bky = 4 + (gi[0] % 4)
                    gi[0] += 1
                    for kc in range(8):
                        S.op("pe", lambda e, bky=bky, wbr=wbr, kc=kc, tsl=tsl, oT=oT: e.matmul(out=ps[bky][:, :], lhsT=wbr[:, kc, :], rhs=oT[:, kc, tsl],
                                                                                           start=(kc == 0), stop=(kc == 7)),
                             reads=[bwb] + [B(obn, h) for h in range(8)], writes=[PB[bky]], signal=(kc == 7))
                    res.append((sl, bky))
                (sa, ya), (sb_, yb) = res
                S.op("dve", lambda e, sa=sa, ya=ya: e.tensor_tensor(out=ysb[0], in0=gsb[sa], in1=ps[ya][:, :], op=ALU.mult),
                     reads=[B("gsb", sa), PB[ya]], writes=[B("ysb", 0)])
                S.op("dve", lambda e, sb_=sb_, yb=yb: e.tensor_tensor(out=ysb[1], in0=gsb[sb_], in1=ps[yb][:, :], op=ALU.mult),
                     reads=[B("gsb", sb_), PB[yb]], writes=[B("ysb", 1)])
                S.op("dve", lambda e, fc=fc, tsl=tsl: e.tensor_tensor(out=mT[:, fc, tsl], in0=ysb[0], in1=ysb[1], op=ALU.add),
                     reads=[B("ysb", 0), B("ysb", 1)], writes=[B("mT")])
        tap("mT", mT, [B("mT")])
        wo_start = AR.mark()
        wo_sb = AR.alloc([128, 16, D], BF16)
        wo_end = AR.mark()
        for q4 in range(4):
            S.dma("pool", wo_sb[:, q4 * 4:(q4 + 1) * 4, :], wsrc(w_o, q4 * 512, 512, 0, D), writes=[B("wo", q4)])
        S.barrier(exclude=[B("wo", q4) for q4 in range(4)])
        AR.release(m_ess)
        x1 = AR.alloc([128, 8, D], F32)
        m_res = AR.mark()
        assert AR.mark() <= wo_start, (AR.mark(), wo_start)
        AR.off = wo_end
        S.dma("sp", x1, x_own.rearrange("(a p) d -> p a d", p=128), writes=[B("x1")])
        for tt in range(8):
            for dc in range(4):
                bk = next_bank(0, 4)
                for kc in range(16):
                    S.op("pe", lambda e, bk=bk, kc=kc, tt=tt, dc=dc: e.matmul(out=ps[bk][:, :], lhsT=mT[:, kc, tt * 128:(tt + 1) * 128], rhs=wo_sb[:, kc, dc * 512:(dc + 1) * 512],
                                                                         start=(kc == 0), stop=(kc == 15)),
                         reads=[B("mT"), B("wo", kc // 4)], writes=[PB[bk]], signal=(kc == 15))
                S.op("dve", lambda e, bk=bk, tt=tt, dc=dc: e.tensor_tensor(out=x1[:, tt, dc * 512:(dc + 1) * 512], in0=x1[:, tt, dc * 512:(dc + 1) * 512], in1=ps[bk][:, :], op=ALU.add),
                     reads=[PB[bk], B("x1")], writes=[B("x1", tt)])
        tap("x1", x1, [B("x1", tt) for tt in range(8)])
        if stop_after <= 5:
            S.emit(); return nc
        S.barrier()
        AR.release(m_res)
        AR.release_top()

        hT3 = AR.alloc([128, 16, NOWN], BF16); bhT3 = B("hT3")
        m_I = AR.mark()
        QcT = AR.alloc([128, 4, NOWN], BF16)
        KcT = AR.alloc([128, 4, 256], BF16)
        Vcx = AR.alloc([128, 2, 4, 130], BF16)
        oc_bf = AR.alloc([128, 8, 512], BF16)
        ocT = AR.alloc([128, 4, NOWN], BF16)
        wco = AR.alloc([128, 4, D], BF16)
        PTc = [AR.alloc([128, 2, 512], BF16) for _ in range(2)]
        coefx = AR.alloc([128, 256], F32)
        memT = AR.alloc([128, 16, 256], BF16)
        m_I1 = AR.mark()
        n_xt = [AR.alloc([128, D], F32) for _ in range(2)]
        n_junk = [AR.alloc([128, D], BF16) for _ in range(2)]
        n_hb = [AR.alloc([128, D], BF16) for _ in range(2)]

        def x1_tiles(tt):
            return x1[:, tt, :], B("x1", tt)
        cnt_t = [0]

        def sinkT(dstT, dbuf):
            def f(tt, hb, bh):
                pb = (cnt_t[0] % 2) * 2
                cnt_t[0] += 1
                transpose_to(hb, bh, dstT, [dbuf], tt, pb)
            return f
        norm_tiles(None, 8, g_cross, tsink(hT3, bhT3), xin=x1_tiles)
        norm_tiles(mem, 2, g_mem, tsink(memT, B("memT")))
        S.barrier()
        AR.release(m_I1)
        wc = [AR.alloc([128, 16, 512], BF16) for _ in range(2)]
        S.dma("pool", wc[0], wsrc(w_ck, 0, D, 0, 512), writes=[B("wc", 0)])
        S.dma("pool", wc[1], wsrc(w_cv, 0, D, 0, 512), writes=[B("wc", 1)])
        S.dma("pool", wco, wsrc(w_co, 0, 512, 0, D), writes=[B("wco")])
        for hh in range(4):
            bk = next_bank(4, 4)
            for kc in range(16):
                S.op("pe", lambda e, bk=bk, kc=kc, hh=hh: e.matmul(out=ps[bk][:, 0:256], lhsT=wc[0][:, kc, hh * 128:(hh + 1) * 128], rhs=memT[:, kc, :],
                                                              start=(kc == 0), stop=(kc == 15)),
                     reads=[B("wc", 0), B("memT")], writes=[PB[bk]], signal=(kc == 15))
            S.op("act", lambda e, bk=bk, hh=hh: e.activation(out=KcT[:, hh, :], in_=ps[bk][:, 0:256], func=AF.Copy), reads=[PB[bk]], writes=[B("KcT")])
        S.op("pool", lambda e: e.memset(Vcx.rearrange("p a b c -> p (a b c)"), 1.0), writes=[B("Vcx")])
        for mt in range(2):
            bk = next_bank(4, 4)
            for kc in range(16):
                S.op("pe", lambda e, bk=bk, kc=kc, mt=mt: e.matmul(out=ps[bk][:, :], lhsT=memT[:, kc, mt * 128:(mt + 1) * 128], rhs=wc[1][:, kc, :],
                                                              start=(kc == 0), stop=(kc == 15)),
                     reads=[B("wc", 1), B("memT")], writes=[PB[bk]], signal=(kc == 15))
            S.op("dve", lambda e, bk=bk, mt=mt: e.tensor_copy(out=Vcx[:, mt, :, 0:128], in_=ps[bk][:, :].rearrange("p (h d) -> p h d", h=4)),
                 reads=[PB[bk]], writes=[B("Vcx")])
        S.dma("pool", wc[0], wsrc(w_cq, 0, D, 0, 512), writes=[B("wc", 0)])
        for hh in range(4):
            for tc in range(2):
                bk = next_bank(4, 4)
                for kc in range(16):
                    S.op("pe", lambda e, bk=bk, kc=kc, hh=hh, tc=tc: e.matmul(out=ps[bk][:, :], lhsT=wc[0][:, kc, hh * 128:(hh + 1) * 128], rhs=hT3[:, kc, tc * 512:(tc + 1) * 512],
                                                                         start=(kc == 0), stop=(kc == 15)),
                         reads=[B("wc", 0), bhT3], writes=[PB[bk]], signal=(kc == 15))
                S.op("act", lambda e, bk=bk, hh=hh, tc=tc: e.activation(out=QcT[:, hh, tc * 512:(tc + 1) * 512], in_=ps[bk][:, :], func=AF.Copy),
                     reads=[PB[bk]], writes=[B("QcT", hh)])
        ci = [0]
        for hh in range(4):
            for tc in range(2):
                sl = ci[0] % 2
                ci[0] += 1
                for mt in range(2):
                    bk = next_bank(4, 4)
                    S.op("pe", lambda e, bk=bk, hh=hh, tc=tc, mt=mt: e.matmul(out=ps[bk][:, :], lhsT=KcT[:, hh, mt * 128:(mt + 1) * 128], rhs=QcT[:, hh, tc * 512:(tc + 1) * 512],
                                                                         start=True, stop=True),
                         reads=[B("KcT"), B("QcT", hh)], writes=[PB[bk]])
                    S.op("act", lambda e, bk=bk, sl=sl, mt=mt: e.activation(out=PTc[sl][:, mt, :], in_=ps[bk][:, :], func=AF.Exp, scale=SCALE),
                         reads=[PB[bk]], writes=[B("PTc", sl)])
                for j in range(4):
                    ti = tc * 4 + j
                    bo = next_bank(0, 4)
                    for mt in range(2):
                        S.op("pe", lambda e, bo=bo, sl=sl, mt=mt, j=j, hh=hh: e.matmul(out=ps[bo][:, 0:129], lhsT=PTc[sl][:, mt, j * 128:(j + 1) * 128], rhs=Vcx[:, mt, hh, 0:129],
                                                                                  start=(mt == 0), stop=(mt == 1)),
                             reads=[B("PTc", sl), B("Vcx")], writes=[PB[bo]], signal=(mt == 1))
                    c0, bc = coef_slot_x = ((ci[0] * 8 + j) % 64) * 4, B("coefx", (ci[0] * 8 + j) % 64)
                    S.op("dve", lambda e, bo=bo, c0=c0: e.reciprocal(out=coefx[:, c0 + 1:c0 + 2], in_=ps[bo][:, 128:129]), reads=[PB[bo]], writes=[bc])
                    S.op("act", lambda e, bo=bo, c0=c0, ti=ti, hh=hh: e.activation(out=oc_bf[:, ti, hh * 128:(hh + 1) * 128], in_=ps[bo][:, 0:128], func=AF.Copy,
                                                                              scale=coefx[:, c0 + 1:c0 + 2]),
                         reads=[PB[bo], bc], writes=[B("oc_bf", ti)])
        for hh in range(4):
            for half in range(2):
                bk = next_bank(4, 4)
                pst = ps[bk].bitcast(BF16)
                for j in range(4):
                    ti = half * 4 + j
                    S.op("pe", lambda e, pst=pst, j=j, ti=ti, hh=hh: e.transpose(out=pst[:, j * 128:(j + 1) * 128], in_=oc_bf[:, ti, hh * 128:(hh + 1) * 128], identity=ident),
                         reads=[B("oc_bf", ti), bC], writes=[PB[bk]], signal=(j == 3))
                S.op("act", lambda e, pst=pst, hh=hh, half=half: e.activation(out=ocT[:, hh, half * 512:(half + 1) * 512], in_=pst[:, 0:512], func=AF.Copy),
                     reads=[PB[bk]], writes=[B("ocT")])
        for tt in range(8):
            for dc in range(4):
                bk = next_bank(0, 4)
                for kc in range(4):
                    S.op("pe", lambda e, bk=bk, kc=kc, tt=tt, dc=dc: e.matmul(out=ps[bk][:, :], lhsT=ocT[:, kc, tt * 128:(tt + 1) * 128], rhs=wco[:, kc, dc * 512:(dc + 1) * 512],
                                                                         start=(kc == 0), stop=(kc == 3)),
                         reads=[B("ocT"), B("wco")], writes=[PB[bk]], signal=(kc == 3))
                S.op("dve", lambda e, bk=bk, tt=tt, dc=dc: e.tensor_tensor(out=x1[:, tt, dc * 512:(dc + 1) * 512], in0=x1[:, tt, dc * 512:(dc + 1) * 512], in1=ps[bk][:, :], op=ALU.add),
                     reads=[PB[bk], B("x1", tt)], writes=[B("x1", tt)])
        tap("x2", x1, [B("x1", tt) for tt in range(8)])
        if stop_after <= 6:
            S.emit(); return nc
        S.barrier()
        AR.release(m_I)

        n_xt = None
        comb = AR.alloc([128, 8, 32], F32)
        mJ0 = AR.mark()
        ering = Ring("ering", 4, [128, 16, 512], top=True)

        def load_expert(ex):
            wgt, bwg = ering.load([(lambda t: t[:, 0:8, :], wsrc(w_eg[ex], 0, 1024, 0, 512)), (lambda t: t[:, 8:16, :], wsrc(w_eg[ex], 1024, 1024, 0, 512))])
            wut, bwu = ering.load([(lambda t: t[:, 0:8, :], wsrc(w_eu[ex], 0, 1024, 0, 512)), (lambda t: t[:, 8:16, :], wsrc(w_eu[ex], 1024, 1024, 0, 512))])
            wdt_, bwd = ering.load([(lambda t: t.rearrange("p a b -> p (a b)")[:, 0:4 * D].rearrange("p (a b) -> p a b", a=4), wsrc(w_ed[ex], 0, 512, 0, D))])
            return wgt, bwg, wut, bwu, wdt_, bwd
        pre_ex0 = load_expert(0)
        n_junk = [AR.alloc([128, D], BF16) for _ in range(1)] * 2
        n_hb = [AR.alloc([128, D], BF16) for _ in range(2)]
        hn32 = AR.alloc([128, D], F32)
        hnT32 = AR.alloc([128, 16, 128], F32)
        wr_sb = AR.alloc([128, 16, 36], F32)
        br_sb = AR.alloc([128, 36], F32)
        rt = AR.alloc([128, 8, 96], F32)
        S.dma("sp", wr_sb, w_r.rearrange("(k p) c -> p k c", p=128), writes=[B("wr")])
        S.dma("sp", br_sb, b_r[0:1, :].to_broadcast([128, 36]), writes=[B("wr")])

        def sinkJ(tt, hb, bh):
            pb = (cnt_t[0] % 2) * 2
            cnt_t[0] += 1
            transpose_to(hb, bh, hT3, [bhT3], tt, pb)
            c4 = norm_cur["c4"]
            S.op("dve", lambda e, tt=tt, c4=c4: e.scalar_tensor_tensor(out=hn32, in0=x1[:, tt, :], scalar=stats[:, c4 + 3:c4 + 4], in1=gb, op0=ALU.mult, op1=ALU.mult),
                 reads=[B("x1", tt), B("stats", norm_cur["col"]), B("gb")], writes=[B("hn32")])
            for q in range(4):
                bk = 4 + q % 2
                for k4 in range(4):
                    kc = q * 4 + k4
                    S.op("pe", lambda e, bk=bk, k4=k4, kc=kc: e.transpose(out=ps[bk][:, k4 * 128:(k4 + 1) * 128], in_=hn32[:, kc * 128:(kc + 1) * 128], identity=identf),
                         reads=[B("hn32"), bC], writes=[PB[bk]], signal=(k4 == 3))
                S.op("dve", lambda e, bk=bk, q=q: e.tensor_copy(out=hnT32[:, q * 4:(q + 1) * 4, :], in_=ps[bk][:, :].rearrange("p (k t) -> p k t", k=4)),
                     reads=[PB[bk]], writes=[B("hnT32")])
            bk = 6 + tt % 2
            for kc in range(16):
                S.op("pe", lambda e, bk=bk, kc=kc: e.matmul(out=ps[bk][:, 0:36], lhsT=hnT32[:, kc, :], rhs=wr_sb[:, kc, :], start=(kc == 0), stop=(kc == 15)),
                     reads=[B("hnT32"), B("wr")], writes=[PB[bk]], signal=(kc == 15))
            if router_pending:
                router_math(*router_pending.pop())
            router_pending.append((tt, bk))

        router_pending = []

        def router_math(tt, bk):
            R = rt[:, tt, :]
            br_ = B("rt", tt)
            S.op("dve", lambda e, bk=bk: e.tensor_tensor(out=R[:, 0:36], in0=ps[bk][:, 0:36], in1=br_sb, op=ALU.add), reads=[PB[bk], B("wr")], writes=[br_])
            S.op("dve", lambda e: e.tensor_reduce(out=R[:, 36:37], in_=R[:, 0:4], axis=mybir.AxisListType.X, op=ALU.max), reads=[br_], writes=[br_])
            S.op("dve", lambda e: e.tensor_scalar(out=R[:, 40:44], in0=R[:, 0:4], scalar1=R[:, 36:37], scalar2=None, op0=ALU.is_ge), reads=[br_], writes=[br_])
            S.op("dve", lambda e: e.tensor_scalar(out=R[:, 44:48], in0=R[:, 0:4], scalar1=R[:, 36:37], scalar2=None, op0=ALU.subtract), reads=[br_], writes=[br_])
            S.op("act", lambda e: e.activation(out=R[:, 44:48], in_=R[:, 44:48], func=AF.Exp, accum_out=R[:, 37:38]), reads=[br_], writes=[br_])
            S.op("dve", lambda e: e.reciprocal(out=R[:, 38:39], in_=R[:, 37:38]), reads=[br_], writes=[br_])
            S.op("dve", lambda e: e.tensor_scalar(out=R[:, 48:56], in0=R[:, 4:12], scalar1=R[:, 40:41], scalar2=None, op0=ALU.mult), reads=[br_], writes=[br_])
            for gq in range(1, 4):
                S.op("dve", lambda e, gq=gq: e.scalar_tensor_tensor(out=R[:, 48:56], in0=R[:, 4 + 8 * gq:12 + 8 * gq], scalar=R[:, 40 + gq:41 + gq], in1=R[:, 48:56],
                                                                   op0=ALU.mult, op1=ALU.add), reads=[br_], writes=[br_])
            S.op("dve", lambda e: e.max(out=R[:, 56:64], in_=R[:, 48:56]), reads=[br_], writes=[br_])
            S.op("dve", lambda e: e.tensor_tensor(out=R[:, 64:65], in0=R[:, 57:58], in1=R[:, 56:57], op=ALU.subtract), reads=[br_], writes=[br_])
            S.op("act", lambda e: e.activation(out=R[:, 65:66], in_=R[:, 64:65], func=AF.Exp), reads=[br_], writes=[br_])
            S.op("dve", lambda e: e.tensor_scalar(out=R[:, 65:66], in0=R[:, 65:66], scalar1=1.0, scalar2=None, op0=ALU.add), reads=[br_], writes=[br_])
            S.op("dve", lambda e: e.reciprocal(out=R[:, 66:67], in_=R[:, 65:66]), reads=[br_], writes=[br_])
            S.op("dve", lambda e: e.tensor_scalar(out=R[:, 67:68], in0=R[:, 66:67], scalar1=-1.0, scalar2=1.0, op0=ALU.mult, op1=ALU.add), reads=[br_], writes=[br_])
            S.op("dve", lambda e: e.tensor_scalar(out=R[:, 66:68], in0=R[:, 66:68], scalar1=R[:, 38:39], scalar2=None, op0=ALU.mult), reads=[br_], writes=[br_])
            S.op("dve", lambda e: e.tensor_scalar(out=R[:, 72:80], in0=R[:, 48:56], scalar1=R[:, 56:57], scalar2=R[:, 66:67], op0=ALU.is_equal, op1=ALU.mult),
                 reads=[br_], writes=[br_])
            S.op("dve", lambda e: e.tensor_scalar(out=R[:, 80:88], in0=R[:, 48:56], scalar1=R[:, 57:58], scalar2=R[:, 67:68], op0=ALU.is_equal, op1=ALU.mult),
                 reads=[br_], writes=[br_])
            S.op("dve", lambda e: e.tensor_tensor(out=R[:, 72:80], in0=R[:, 72:80], in1=R[:, 80:88], op=ALU.add), reads=[br_], writes=[br_])
            for gq in range(4):
                S.op("dve", lambda e, gq=gq: e.tensor_scalar(out=comb[:, tt, gq * 8:(gq + 1) * 8], in0=R[:, 72:80], scalar1=R[:, 40 + gq:41 + gq], scalar2=None, op0=ALU.mult),
                     reads=[br_], writes=[B("comb", tt)])
        norm_tiles(None, 8, g_moe, sinkJ, xin=x1_tiles)
        router_math(*router_pending.pop())
        tap("comb", comb, [B("comb", tt) for tt in range(8)])
        S.barrier(exclude=list(ering.b))
        AR.release(mJ0)
        mJ = AR.mark()
        hid = [AR.alloc([128, 4, NOWN], BF16) for _ in range(2)]
        sil = [AR.alloc([128, 512], F32) for _ in range(2)]
        si = [0]
        for ex in range(32):
            wgt, bwg, wut, bwu, wdt_, bwd = pre_ex0 if ex == 0 else load_expert(ex)
            wdt = wdt_.rearrange("p a b -> p (a b)")[:, 0:4 * D].rearrange("p (a b) -> p a b", a=4)
            hsl = ex % 2
            for fc in range(4):
                for tc in range(2):
                    ba = next_bank(0, 2)
                    bu = 2 + (pbi[0] % 2)
                    for kc in range(16):
                        S.op("pe", lambda e, ba=ba, kc=kc, fc=fc, tc=tc, wgt=wgt: e.matmul(out=ps[ba][:, :], lhsT=wgt[:, kc, fc * 128:(fc + 1) * 128], rhs=hT3[:, kc, tc * 512:(tc + 1) * 512],
                                                                                      start=(kc == 0), stop=(kc == 15)),
                             reads=[bwg, bhT3], writes=[PB[ba]], signal=(kc == 15))
                    for kc in range(16):
                        S.op("pe", lambda e, bu=bu, kc=kc, fc=fc, tc=tc, wut=wut: e.matmul(out=ps[bu][:, :], lhsT=wut[:, kc, fc * 128:(fc + 1) * 128], rhs=hT3[:, kc, tc * 512:(tc + 1) * 512],
                                                                                      start=(kc == 0), stop=(kc == 15)),
                             reads=[bwu, bhT3], writes=[PB[bu]], signal=(kc == 15))
                    ss_ = si[0] % 2
                    si[0] += 1
                    S.op("act", lambda e, ba=ba, ss_=ss_: e.activation(out=sil[ss_], in_=ps[ba][:, :], func=AF.Silu), reads=[PB[ba]], writes=[B("sil", ss_)])
                    S.op("dve", lambda e, bu=bu, ss_=ss_, hsl=hsl, fc=fc, tc=tc: e.tensor_tensor(out=hid[hsl][:, fc, tc * 512:(tc + 1) * 512], in0=sil[ss_], in1=ps[bu][:, :], op=ALU.mult),
                         reads=[B("sil", ss_), PB[bu]], writes=[B("hid", hsl)])
            for tt in range(8):
                for dc in range(4):
                    bk = 4 + (pbi[0] % 4)
                    pbi[0] += 1
                    for fc in range(4):
                        S.op("pe", lambda e, bk=bk, fc=fc, tt=tt, dc=dc, hsl=hsl, wdt=wdt: e.matmul(out=ps[bk][:, :], lhsT=hid[hsl][:, fc, tt * 128:(tt + 1) * 128],
                                                                                               rhs=wdt[:, fc, dc * 512:(dc + 1) * 512], start=(fc == 0), stop=(fc == 3)),
                             reads=[B("hid", hsl), bwd], writes=[PB[bk]], signal=(fc == 3))
                    S.op("dve", lambda e, bk=bk, tt=tt, dc=dc, ex=ex: e.scalar_tensor_tensor(out=x1[:, tt, dc * 512:(dc + 1) * 512], in0=ps[bk][:, :], scalar=comb[:, tt, ex:ex + 1],
                                                                                         in1=x1[:, tt, dc * 512:(dc + 1) * 512], op0=ALU.mult, op1=ALU.add),
                         reads=[PB[bk], B("comb", tt), B("x1", tt)], writes=[B("x1", tt)])
        tap("x3", x1, [B("x1", tt) for tt in range(8)])
        S.barrier()
        AR.release(mJ)
        yout = [AR.alloc([128, D], F32) for _ in range(2)]
        n_hb = yout
        n_junk = [AR.alloc([128, D], BF16)] * 2

        def sinkK(tt, hb, bh):
            S.tail.append(S.dma("sp", out_d[tt * 128:(tt + 1) * 128, :], hb, reads=[bh]))
        norm_tiles(None, 8, g_final, sinkK, xin=x1_tiles)
        S.emit()
    return nc


def _consts(h):
    c = {}
    c["c_ident"] = np.eye(128, dtype=np.float32)
    j = np.arange(128)
    c["c_negtri"] = -(j[:, None] > j[None, :]).astype(np.float32)
    c["c_negones"] = -np.ones((128, 128), np.float32)
    r = np.zeros((32, 32), np.float32)
    for m in range(16):
        r[m + 16, m] = -1.0
        r[m, m + 16] = 1.0
    c["c_r32"] = r
    inv = (500000.0 ** (-np.arange(16, dtype=np.float32) * (2.0 / 32))).astype(np.float32)
    invf = np.zeros((32, 2), np.float32)
    invf[:, 0] = np.concatenate([inv, inv]) / np.float32(2 * np.pi)
    c["c_invf"] = invf
    rel = [h, 3 - h]
    k = np.arange(128)[:, None]
    q = np.arange(256)[None, :]
    tq = np.array(rel)[q // 128] * 128 + q % 128
    cbi = np.zeros((128, 4, 256), np.float32); cbs = np.zeros((128, 4, 256), np.float32)
    for jj in range(4):
        s = jj * 128 + k
        cbi[:, jj, :] = np.where(s <= tq, 0.0, NEG)
        cbs[:, jj, :] = np.where(s < tq, 0.0, NEG)
    c["c_cbi"] = cbi.reshape(128, -1); c["c_cbs"] = cbs.reshape(128, -1)
    wb = np.zeros((128, 8, 256), np.float32)
    for jj in range(8):
        s = (jj - 4) * 128 + k
        dd = tq - s
        wb[:, jj, :] = np.where((dd >= 0) & (dd < 512), 0.0, NEG)
    c["c_wb"] = wb.reshape(128, -1)
    own_tiles = [2 * i + ((i % 2) ^ h) for i in range(8)]
    own_tok = np.concatenate([np.arange(g * 128, (g + 1) * 128) for g in own_tiles])
    cc = np.arange(128)[:, None]
    vis = (cc <= 126) & (cc * 16 + 31 <= own_tok[None, :])
    c["c_cmpb"] = np.where(vis, 0.0, NEG).astype(np.float32)
    jb = np.arange(32)[None, :]
    cur = (own_tok // 64)[:, None]
    forced = (jb == 0) | (jb == cur) | (jb == cur - 1)
    valid = jb <= cur
    fbv = np.where(valid, np.where(forced, 1000.0, 0.0), -1e9).astype(np.float32)
    c["c_fb"] = fbv.reshape(8, 128, 32).transpose(1, 0, 2).reshape(128, -1).copy()
    c["c_vm"] = valid.astype(np.float32).reshape(8, 128, 32).transpose(1, 0, 2).reshape(128, -1).copy()
    E = np.zeros((32, 16, 128), np.float32)
    for G in range(16):
        for kk in range(128):
            E[2 * G + kk // 64, G, kk] = 1.0
    c["c_E"] = E.reshape(32, -1)
    cs = np.arange(127) * 16
    ss = np.arange(32) * 64
    ov = ((cs[:, None] < ss[None, :] + 64) & (cs[:, None] + 32 > ss[None, :])).astype(np.float32)
    ovl = np.zeros((128, 33), np.float32)
    ovl[:127, :32] = ov
    ovl[:127, 32] = 1.0
    c["c_ovl"] = ovl
    return c, own_tok


_NC_CACHE = {}


def kernel(**inputs):
    x = np.asarray(inputs["x"], np.float32)
    f = lambda k: np.ascontiguousarray(np.asarray(inputs[k]))
    shared = {
        "g_mix": f("g_mix").reshape(1, D), "g_cross": f("g_cross").reshape(1, D), "g_mem": f("g_mem").reshape(1, D),
        "g_moe": f("g_moe").reshape(1, D), "g_final": f("g_final").reshape(1, D),
        "w_in": f("w_in").reshape(D, IN_W),
        "cmp_pe_k": f("cmp_pe_k").reshape(32, 128), "cmp_w1_k": f("cmp_w1_k").reshape(4096, 128), "cmp_w2_k": f("cmp_w2_k").reshape(128, 128),
        "cmp_pe_v": f("cmp_pe_v").reshape(32, 128), "cmp_w1_v": f("cmp_w1_v").reshape(4096, 128), "cmp_w2_v": f("cmp_w2_v").reshape(128, 128),
        "w_br_nsa": f("w_br_nsa").reshape(1024, D), "w_br_sb": f("w_br_sb").reshape(1024, D), "w_o": f("w_o").reshape(D, D),
        "w_cq": f("w_cq").reshape(D, 512), "w_ck": f("w_ck").reshape(D, 512), "w_cv": f("w_cv").reshape(D, 512), "w_co": f("w_co").reshape(512, D),
        "w_r": np.ascontiguousarray(np.concatenate([f("w_rg").reshape(D, 4), f("w_re").reshape(D, 32)], axis=1)),
        "b_r": np.ascontiguousarray(np.concatenate([f("b_rg").reshape(1, 4), f("b_re").reshape(1, 32)], axis=1)),
        "w_eg": f("w_eg").reshape(32, D, 512), "w_eu": f("w_eu").reshape(32, D, 512), "w_ed": f("w_ed").reshape(32, 512, D),
    }
    pos = np.asarray(inputs["positions"]).astype(np.int32)
    memv = np.asarray(inputs["mem"], np.float32)
    in_maps = []
    owns = []
    for c in range(8):
        b, h = c // 2, c % 2
        cst, own_tok = _consts(h)
        owns.append(own_tok)
        m = dict(shared)
        m.update(cst)
        m["x_nat"] = np.ascontiguousarray(x[b])
        m["x_own"] = np.ascontiguousarray(x[b][own_tok])
        m["pos_nat"] = np.ascontiguousarray(pos[b].reshape(1, T))
        m["pos_own"] = np.ascontiguousarray(pos[b][own_tok].reshape(1, NOWN))
        m["mem"] = np.ascontiguousarray(memv[b])
        in_maps.append(m)
    if "nc" not in _NC_CACHE:
        _NC_CACHE["nc"] = build()
    nc = _NC_CACHE["nc"]
    res = run_bass_kernel_spmd(nc, in_maps, core_ids=list(range(8)))
    out = np.zeros((4, T, D), np.float32)
    for c in range(8):
        out[c // 2][owns[c]] = res.results[c]["out"]
    return out
```

```python
import types
import numpy as np
from contextlib import ExitStack
import concourse.bass as bass
import concourse.mybir as mybir
from concourse.bass_utils import run_bass_kernel_spmd

F32 = mybir.dt.float32
BF16 = mybir.dt.bfloat16
I32 = mybir.dt.int32
U8 = mybir.dt.uint8
AF = mybir.ActivationFunctionType
ALU = mybir.AluOpType

D = 2048
T = 2048
NOWN = 1024
HD = 128
NEG = -30000.0
SCALE = HD ** -0.5
C_QN, C_KC, C_VC, C_KS, C_VS, C_KW, C_VW, C_G, C_QS, C_KSB, C_VSB, C_GA, C_GB = (
    0, 1024, 1280, 1536, 1792, 2048, 2304, 2560, 2584, 3608, 4632, 5656, 7704)
IN_W = 9752


def _freeze(fn):
    if fn.__closure__ is None:
        return fn
    cells = []
    for c in fn.__closure__:
        try:
            cells.append(types.CellType(c.cell_contents))
        except ValueError:
            cells.append(c)
    return types.FunctionType(fn.__code__, fn.__globals__, fn.__name__, fn.__defaults__, tuple(cells))


class Buf:
    __slots__ = ("name", "w", "r", "dsem", "dcnt", "excl")

    def __init__(self, name):
        self.name = name
        self.excl = False
        self.w = None
        self.r = []
        self.dsem = None
        self.dcnt = 0


class Sched:
    ENGS = ("pe", "act", "dve", "pool", "sp")

    def __init__(self, nc, stack):
        self.nc = nc
        self.stack = stack
        self.sem = {e: stack.enter_context(nc.semaphore("s_" + e)) for e in self.ENGS}
        self.cnt = {e: 0 for e in self.ENGS}
        self.q = {e: [] for e in self.ENGS}
        self.waited = {e: {} for e in self.ENGS}
        self.nsem = 5
        self.ninst = 0
        self.tail = []
        self.bufs = {}
        self.dma_bufs = []

    def B(self, *key):
        b = self.bufs.get(key)
        if b is None:
            b = Buf(str(key))
            self.bufs[key] = b
        return b

    def _wait(self, eng, ev):
        if ev is None:
            return
        sem, val = ev
        if sem is self.sem[eng]:
            if eng in ("pe", "sp") or val > self.cnt[eng]:
                return
        w = self.waited[eng]
        if w.get(id(sem), 0) >= val:
            return
        w[id(sem)] = val
        self.q[eng].append(("wait", sem, val))

    def _deps(self, eng, reads, writes):
        for b in reads:
            self._wait(eng, b.w)
            if b.excl:
                for ev in b.r:
                    if ev[0] is not self.sem[eng]:
                        self._wait(eng, ev)
        for b in writes:
            self._wait(eng, b.w)
            for ev in b.r:
                self._wait(eng, ev)

    def _commit(self, ev, reads, writes):
        for b in reads:
            b.r = [x for x in b.r if x[0] is not ev[0]] + [ev]
        for b in writes:
            b.w = ev
            b.r = []

    def op(self, eng, fn, reads=(), writes=(), signal=True):
        self._deps(eng, reads, writes)
        if signal:
            self.cnt[eng] += 1
            ev = (self.sem[eng], self.cnt[eng])
        else:
            ev = (self.sem[eng], self.cnt[eng] + 1)
        self.q[eng].append(("op", _freeze(fn), signal))
        self._commit(ev, reads, writes)
        self.ninst += 1
        return ev

    def _dsem(self, b):
        if b.dsem is None:
            b.dsem = self.stack.enter_context(self.nc.semaphore("d%d" % self.nsem))
            self.nsem += 1
            self.dma_bufs.append(b)
        return b.dsem

    def dma(self, eng, out, in_, reads=(), writes=(), owner=None):
        self._deps(eng, reads, writes)
        owner = owner or (writes[0] if writes else reads[0])
        sem = self._dsem(owner)
        owner.dcnt += 16
        ev = (sem, owner.dcnt)
        self.q[eng].append(("dma", out, in_, sem))
        self._commit(ev, reads, writes)
        self.ninst += 1
        return ev

    def barrier(self, exclude=()):
        self.marks = getattr(self, "marks", [])
        self.marks.append({e: sum(1 for it in self.q[e] if it[0] != "wait") for e in self.ENGS})
        evs = [(self.sem[e], self.cnt[e]) for e in self.ENGS if self.cnt[e] > 0]
        evs += [(b.dsem, b.dcnt) for b in self.dma_bufs if b.dcnt > 0 and b not in exclude]
        for e in self.ENGS:
            for ev in evs:
                self._wait(e, ev)

    def check(self):
        val = {}
        pc = {e: 0 for e in self.ENGS}
        prog = True
        while prog:
            prog = False
            for e in self.ENGS:
                q = self.q[e]
                while pc[e] < len(q):
                    it = q[pc[e]]
                    if it[0] == "wait":
                        if val.get(id(it[1]), 0) < it[2]:
                            break
                    elif it[0] == "op":
                        if it[2]:
                            val[id(self.sem[e])] = val.get(id(self.sem[e]), 0) + 1
                    else:
                        val[id(it[3])] = val.get(id(it[3]), 0) + 16
                    pc[e] += 1
                    prog = True
        stuck = {e: (pc[e], len(self.q[e])) for e in self.ENGS if pc[e] < len(self.q[e])}
        if stuck:
            for e, (p, n) in stuck.items():
                it = self.q[e][p]
                print("DEADLOCK", e, p, n, it[0], it[2], "have", val.get(id(it[1]), 0))
            raise RuntimeError("deadlock in schedule: %s" % stuck)
        print("schedule ok: insts", {e: len(self.q[e]) for e in self.ENGS}, "nsem", self.nsem)

    def emit(self):
        nc = self.nc
        for ev in self.tail:
            self._wait("sp", ev)
        self.check()
        with nc.Block() as block:
            def run(eng):
                def body(e):
                    sem_e = self.sem[eng]
                    for item in self.q[eng]:
                        k = item[0]
                        if k == "wait":
                            e.wait_ge(item[1], item[2])
                        elif k == "op":
                            ins = item[1](e)
                            if item[2]:
                                ins.then_inc(sem_e, 1)
                        else:
                            _, out, in_, sem = item
                            e.dma_start(out=out, in_=in_).then_inc(sem, 16)
                return body
            block.tensor(run("pe"))
            block.scalar(run("act"))
            block.vector(run("dve"))
            block.gpsimd(run("pool"))
            block.sync(run("sp"))


class Arena:
    def __init__(self, nc, nbytes):
        self.t = nc.alloc_sbuf_tensor("arena", [128, nbytes], U8)
        self.n = nbytes
        self.off = 0
        self.top = nbytes

    def alloc(self, shape, dt, top=False):
        esz = {F32: 4, BF16: 2, I32: 4}[dt]
        used = 1
        for s in shape[1:]:
            used *= s
        n = (used * esz + 63) // 64 * 64
        assert self.off + n <= self.top, ("SBUF arena overflow", self.off, n, self.top)
        if top:
            self.top -= n
            o = self.top
        else:
            o = self.off
            self.off += n
        v = self.t[0:shape[0], o:o + n].bitcast(dt)[:, 0:used]
        if len(shape) == 3:
            v = v.rearrange("p (a b) -> p a b", a=shape[1])
        elif len(shape) == 4:
            v = v.rearrange("p (a b c) -> p a b c", a=shape[1], b=shape[2])
        return v

    def mark(self):
        return self.off

    def release(self, m):
        self.off = m

    def release_top(self):
        self.top = self.n


def build(stop_after=99, taps=()):
    nc = bass.Bass("TRN2", target_bir_lowering=False)
    din = {}

    def dram_in(name, shape, dt=F32):
        din[name] = nc.dram_tensor(name, list(shape), dt, kind="ExternalInput").ap()
        return din[name]

    x_nat = dram_in("x_nat", [T, D])
    x_own = dram_in("x_own", [NOWN, D])
    pos_nat = dram_in("pos_nat", [1, T], I32)
    pos_own = dram_in("pos_own", [1, NOWN], I32)
    mem = dram_in("mem", [256, D])
    g_mix = dram_in("g_mix", [1, D]); g_cross = dram_in("g_cross", [1, D]); g_mem = dram_in("g_mem", [1, D])
    g_moe = dram_in("g_moe", [1, D]); g_final = dram_in("g_final", [1, D])
    w_in = dram_in("w_in", [D, IN_W])
    cmp_pe_k = dram_in("cmp_pe_k", [32, 128]); cmp_w1_k = dram_in("cmp_w1_k", [4096, 128]); cmp_w2_k = dram_in("cmp_w2_k", [128, 128])
    cmp_pe_v = dram_in("cmp_pe_v", [32, 128]); cmp_w1_v = dram_in("cmp_w1_v", [4096, 128]); cmp_w2_v = dram_in("cmp_w2_v", [128, 128])
    w_br_nsa = dram_in("w_br_nsa", [1024, D]); w_br_sb = dram_in("w_br_sb", [1024, D]); w_o = dram_in("w_o", [D, D])
    w_cq = dram_in("w_cq", [D, 512]); w_ck = dram_in("w_ck", [D, 512]); w_cv = dram_in("w_cv", [D, 512]); w_co = dram_in("w_co", [512, D])
    w_r = dram_in("w_r", [D, 36])
    b_r = dram_in("b_r", [1, 36])
    w_eg = dram_in("w_eg", [32, D, 512]); w_eu = dram_in("w_eu", [32, D, 512]); w_ed = dram_in("w_ed", [32, 512, D])
    c_ident = dram_in("c_ident", [128, 128]); c_negtri = dram_in("c_negtri", [128, 128]); c_negones = dram_in("c_negones", [128, 128])
    c_r32 = dram_in("c_r32", [32, 32]); c_invf = dram_in("c_invf", [32, 2])
    c_cbi = dram_in("c_cbi", [128, 4 * 256]); c_cbs = dram_in("c_cbs", [128, 4 * 256]); c_wb = dram_in("c_wb", [128, 8 * 256])
    c_cmpb = dram_in("c_cmpb", [128, NOWN]); c_fb = dram_in("c_fb", [128, 8 * 32]); c_vm = dram_in("c_vm", [128, 8 * 32])
    c_E = dram_in("c_E", [32, 16 * 128]); c_ovl = dram_in("c_ovl", [128, 33])
    out_d = nc.dram_tensor("out", [NOWN, D], F32, kind="ExternalOutput").ap()
    tap_d = {}
    for nm, shp, dt in taps:
        tap_d[nm] = nc.dram_tensor("tap_" + nm, list(shp), dt, kind="ExternalOutput").ap()
    scr_hTown = nc.dram_tensor("scr_hTown", [128, 16, NOWN], BF16, kind="Internal").ap()
    scr_qsb = nc.dram_tensor("scr_qsb", [8, 128, NOWN], BF16, kind="Internal").ap()
    scr_ksb = nc.dram_tensor("scr_ksb", [8, 128, T], BF16, kind="Internal").ap()
    scr_vsb = nc.dram_tensor("scr_vsb", [128, 16, 1024], BF16, kind="Internal").ap()

    with ExitStack() as st:
        S = Sched(nc, st)
        B = S.B
        AR = Arena(nc, 206 * 1024)
        ps = [nc.alloc_psum_tensor("ps%d" % i, [128, 512], F32) for i in range(8)]
        PB = [B("ps", i) for i in range(8)]
        for pb_ in PB:
            pb_.excl = True

        def tap(name, src_ap, src_bufs):
            if name in tap_d:
                S.tail.append(S.dma("sp", tap_d[name], src_ap, reads=src_bufs))

        identf = AR.alloc([128, 128], F32)
        ident = AR.alloc([128, 128], BF16)
        r32 = AR.alloc([32, 32], F32)
        invf = AR.alloc([32, 2], F32)
        epsc = AR.alloc([128, 2], F32)
        stats = AR.alloc([128, 256], F32)
        stage_c = AR.alloc([128, 2048], F32)
        m_ess = AR.mark()
        negtri = AR.alloc([128, 128], BF16)
        negones = AR.alloc([128, 128], BF16)
        bC = B("const")

        def load_const_bf16(dst, src, ncols):
            S.dma("sp", stage_c[0:dst.shape[0], 0:ncols], src, writes=[B("stage_c"), B("gb")])
            S.op("dve", lambda e: e.tensor_copy(out=dst, in_=stage_c[0:dst.shape[0], 0:ncols]),
                 reads=[B("stage_c")], writes=[bC])

        S.dma("sp", identf, c_ident[:, :], writes=[bC])
        S.dma("sp", r32, c_r32[:, :], writes=[bC])
        S.dma("sp", invf, c_invf[:, :], writes=[bC])
        load_const_bf16(ident, c_ident[:, :], 128)
        cbi = AR.alloc([128, 4, 256], BF16); cbs = AR.alloc([128, 4, 256], BF16); wbm = AR.alloc([128, 8, 256], BF16)
        cmpb = AR.alloc([128, NOWN], BF16)
        Emat = AR.alloc([32, 16, 128], BF16)
        ovl = AR.alloc([128, 33], BF16)
        fb = AR.alloc([128, 8, 32], F32); vm = AR.alloc([128, 8, 32], F32)

        def load_masks():
            load_const_bf16(negtri, c_negtri[:, :], 128)
            load_const_bf16(negones, c_negones[:, :], 128)
            load_const_bf16(cbi.rearrange("p a b -> p (a b)"), c_cbi[:, :], 1024)
            load_const_bf16(cbs.rearrange("p a b -> p (a b)"), c_cbs[:, :], 1024)
            load_const_bf16(wbm.rearrange("p a b -> p (a b)"), c_wb[:, :], 2048)
            load_const_bf16(cmpb, c_cmpb[:, :], 1024)
            load_const_bf16(Emat.rearrange("p a b -> p (a b)"), c_E[:, :], 2048)
            load_const_bf16(ovl, c_ovl[:, :], 33)
            S.dma("sp", fb.rearrange("p a b -> p (a b)"), c_fb[:, :], writes=[bC])
            S.dma("sp", vm.rearrange("p a b -> p (a b)"), c_vm[:, :], writes=[bC])
        S.op("pool", lambda e: e.memset(stats, 0.0), writes=[B("stats")])
        S.op("pool", lambda e: e.memset(epsc, 1e-6), writes=[bC])
        gb = stage_c
        KT_slc = AR.alloc([128, 2, T], BF16)
        KT_win = AR.alloc([128, 2, T], BF16)
        Vx_slc = AR.alloc([128, 16, 2, 130], BF16)
        Vx_win = AR.alloc([128, 16, 2, 130], BF16)
        kcT = AR.alloc([128, 2, 128], BF16)
        vcx = AR.alloc([128, 2, 162], BF16)
        gates_sb = AR.alloc([128, 8, 24], F32)
        m_nsa = AR.mark()

        stat_col = [0]

        norm_cur = {}

        def norm_tiles(x_dram, ntiles, g_dram, sink, xin=None, tag="n"):
            mid_fn, end_fn = sink if isinstance(sink, tuple) else (sink, None)
            S.dma("sp", gb, g_dram[0:1, :].to_broadcast([128, D]), writes=[B("gb"), B("stage_c")])
            info = {}

            def s1(tt):
                sl = tt % 2
                if xin is None:
                    xs = tt % len(n_xt)
                    xt = n_xt[xs]; bx = B("n_xt", xs)
                    S.dma("sp", xt, x_dram[tt * 128:(tt + 1) * 128, :], writes=[bx])
                else:
                    xt, bx = xin(tt)
                col = stat_col[0] % 64
                stat_col[0] += 1
                c4 = col * 4
                bs = B("stats", col)
                info[tt] = (xt, bx, c4, bs, col)
                S.op("act", lambda e: e.activation(out=n_junk[sl], in_=xt, func=AF.Square, scale=float(D) ** -0.5, accum_out=stats[:, c4:c4 + 1]),
                     reads=[bx, B("stats")], writes=[B("n_junk", sl), bs])
                S.op("act", lambda e: e.activation(out=stats[:, c4 + 2:c4 + 3], in_=stats[:, c4:c4 + 1], func=AF.Sqrt, bias=epsc[:, 0:1]),
                     reads=[bs, bC], writes=[bs])

            def s2(tt):
                xt, bx, c4, bs, col = info[tt]
                sl = tt % 2
                S.op("dve", lambda e: e.reciprocal(out=stats[:, c4 + 3:c4 + 4], in_=stats[:, c4 + 2:c4 + 3]), reads=[bs], writes=[bs])
                hb = n_hb[sl]; bh = B("n_hb", sl)
                S.op("dve", lambda e: e.scalar_tensor_tensor(out=hb, in0=xt, scalar=stats[:, c4 + 3:c4 + 4], in1=gb, op0=ALU.mult, op1=ALU.mult),
                     reads=[bx, bs, B("gb")], writes=[bh])
                S.op("pool", lambda e: e.memset(stats[:, c4:c4 + 1], 0.0), reads=[bs], writes=[bs])
                norm_cur["c4"] = c4; norm_cur["col"] = col
                mid_fn(tt, hb, bh)

            s1(0)
            if ntiles > 1:
                s1(1)
            s2(0)
            for tt in range(ntiles):
                if tt + 2 < ntiles:
                    s1(tt + 2)
                if tt + 1 < ntiles:
                    s2(tt + 1)
                if end_fn is not None:
                    end_fn(tt)

        def transpose_pe(hb, bh, pbank):
            for half in range(2):
                pst = ps[pbank + half].bitcast(BF16)
                bp = PB[pbank + half]
                for k8 in range(8):
                    kc = half * 8 + k8
                    S.op("pe", lambda e: e.transpose(out=pst[:, k8 * 128:(k8 + 1) * 128], in_=hb[:, kc * 128:(kc + 1) * 128], identity=ident),
                         reads=[bh, bC], writes=[bp], signal=(k8 == 7))

        def transpose_evac(dstT, dst_bufs, tt, pbank):
            for half in range(2):
                pst = ps[pbank + half].bitcast(BF16)
                bp = PB[pbank + half]
                dst = dstT[:, half * 8:(half + 1) * 8, tt * 128:(tt + 1) * 128]
                src = pst[:, 0:1024].rearrange("p (k t) -> p k t", k=8)
                if half == 0:
                    S.op("act", lambda e: e.activation(out=dst, in_=src, func=AF.Copy), reads=[bp], writes=dst_bufs)
                else:
                    S.op("dve", lambda e: e.tensor_copy(out=dst, in_=src), reads=[bp], writes=dst_bufs)

        def transpose_to(hb, bh, dstT, dst_bufs, tt, pbank):
            transpose_pe(hb, bh, pbank)
            transpose_evac(dstT, dst_bufs, tt, pbank)

        def tsink(dstT, dbuf):
            return (lambda tt, hb, bh: transpose_pe(hb, bh, (tt % 2) * 2),
                    lambda tt: transpose_evac(dstT, [dbuf], tt, (tt % 2) * 2))

        class Ring:
            def __init__(self, name, nslots, shape, top=False):
                self.t = [AR.alloc(shape, BF16, top=top) for _ in range(nslots)]
                self.b = [B(name, i) for i in range(nslots)]
                self.i = 0
                self.n = nslots

            def load(self, src_aps):
                t = self.t[self.i]; b = self.b[self.i]
                self.i = (self.i + 1) % self.n
                for dv, sa in src_aps:
                    S.dma("pool", dv(t), sa, writes=[b])
                return t, b

        def wsrc(w, r0, nrows, c0, ncols):
            return w[r0:r0 + nrows, c0:c0 + ncols].rearrange("(k p) c -> p k c", p=128)

        hT_nat = AR.alloc([128, 16, T], BF16)
        m_A = AR.mark()
        hT_own = AR.alloc([128, 16, NOWN], BF16)
        m_A2 = AR.mark()
        n_xt = [AR.alloc([128, D], F32) for _ in range(3)]
        n_junk = [AR.alloc([128, D], BF16) for _ in range(2)]
        n_hb = [AR.alloc([128, D], BF16) for _ in range(2)]
        bhTn = B("hT_nat"); bhTo = B("hT_own")

        norm_tiles(x_own, 8, g_mix, tsink(hT_own, bhTo))
        norm_tiles(x_nat, 16, g_mix, tsink(hT_nat, bhTn))
        load_masks()
        S.dma("sp", scr_hTown[:, :, :], hT_own, reads=[bhTo], writes=[B("scr_hTown")], owner=B("scr_hTown"))
        tap("hT_own", hT_own, [bhTo])
        if stop_after <= 0:
            S.emit(); return nc
        S.barrier()
        AR.release(m_A2)

        scr_qn = nc.dram_tensor("scr_qn", [8, 128, NOWN], BF16, kind="Internal").ap()
        RC = 256
        state = {}

        def alloc_proj_tmps():
            state["posi"] = AR.alloc([32, RC], I32); state["posf"] = AR.alloc([32, RC], F32)
            state["tmpa"] = AR.alloc([32, RC], F32); state["tmpi"] = AR.alloc([32, RC], I32); state["tmpm"] = AR.alloc([32, RC], F32)
            state["wring"] = Ring("wring", 2, [128, 16, 256])
            state["raw32"] = [AR.alloc([32, 512], F32) for _ in range(2)]
            state["ropt1"] = [AR.alloc([32, 512], F32) for _ in range(2)]
            state["ropt2"] = [AR.alloc([32, 512], F32) for _ in range(2)]
            state["stg"] = [AR.alloc([128, 512], BF16) for _ in range(3)]

        def rope_tables(pos_dram, n, cs):
            bp = B("ropetmp")
            posi, posf, tmpa, tmpi, tmpm = (state[k] for k in ("posi", "posf", "tmpa", "tmpi", "tmpm"))
            for c0 in range(0, n, RC):
                S.dma("sp", posi, pos_dram[0:1, c0:c0 + RC].to_broadcast([32, RC]), writes=[bp])
                S.op("dve", lambda e: e.tensor_copy(out=posf, in_=posi), reads=[bp], writes=[bp])
                for which in range(2):
                    S.op("dve", lambda e, which=which: e.tensor_scalar(out=tmpa, in0=posf, scalar1=invf[:, 0:1],
                                                                      scalar2=(0.25 if which == 0 else 0.0), op0=ALU.mult, op1=ALU.add),
                         reads=[bp, bC], writes=[bp])
                    S.op("dve", lambda e: e.tensor_copy(out=tmpi, in_=tmpa), reads=[bp], writes=[bp])
                    S.op("dve", lambda e: e.tensor_copy(out=tmpm, in_=tmpi), reads=[bp], writes=[bp])
                    S.op("dve", lambda e: e.tensor_sub(out=tmpa, in0=tmpa, in1=tmpm), reads=[bp], writes=[bp])
                    S.op("dve", lambda e: e.tensor_scalar(out=tmpm, in0=tmpa, scalar1=0.5, scalar2=None, op0=ALU.is_gt), reads=[bp], writes=[bp])
                    S.op("dve", lambda e: e.tensor_sub(out=tmpa, in0=tmpa, in1=tmpm), reads=[bp], writes=[bp])
                    S.op("dve", lambda e: e.tensor_scalar(out=tmpm, in0=tmpa, scalar1=-0.5, scalar2=None, op0=ALU.is_lt), reads=[bp], writes=[bp])
                    S.op("dve", lambda e: e.tensor_add(out=tmpa, in0=tmpa, in1=tmpm), reads=[bp], writes=[bp])
                    S.op("act", lambda e, which=which, c0=c0: e.activation(out=cs[:, which, c0:c0 + RC], in_=tmpa, func=AF.Sin, scale=2.0 * np.pi),
                         reads=[bp], writes=[B("cs")])

        pbi = [0]

        def next_bank(lo, n):
            i = lo + (pbi[0] % n)
            pbi[0] += 1
            return i

        rope_i = [0]
        stg_i = [0]

        def evac_rope(pp, bp, dst, dbufs, cs, t0):
            raw32, ropt1, ropt2 = state["raw32"], state["ropt1"], state["ropt2"]
            sl = rope_i[0] % 2
            rope_i[0] += 1
            S.op("act", lambda e: e.activation(out=dst, in_=pp, func=AF.Copy), reads=[bp], writes=dbufs)
            S.op("dve", lambda e: e.tensor_copy(out=raw32[sl], in_=pp[0:32, :]), reads=[bp], writes=[B("raw32", sl)])
            rb = 6 + sl
            S.op("pe", lambda e: e.matmul(out=ps[rb][0:32, :], lhsT=r32, rhs=raw32[sl], start=True, stop=True),
                 reads=[B("raw32", sl), bC], writes=[PB[rb]])
            S.op("dve", lambda e: e.tensor_tensor(out=ropt1[sl], in0=raw32[sl], in1=cs[:, 0, t0:t0 + 512], op=ALU.mult),
                 reads=[B("raw32", sl), B("cs")], writes=[B("ropt1", sl)])
            S.op("dve", lambda e: e.tensor_tensor(out=ropt2[sl], in0=ps[rb][0:32, :], in1=cs[:, 1, t0:t0 + 512], op=ALU.mult),
                 reads=[PB[rb], B("cs")], writes=[B("ropt2", sl)])
            S.op("dve", lambda e: e.tensor_tensor(out=dst[0:32, :], in0=ropt1[sl], in1=ropt2[sl], op=ALU.add),
                 reads=[B("ropt1", sl), B("ropt2", sl)], writes=dbufs)

        def proj_fm(hT, bhT, ntok, col0, ncols, evac):
            wring = state["wring"]
            pending = None
            for g0 in range(0, ncols, 256):
                gw = min(256, ncols - g0)
                wt, wbuf = wring.load([(lambda t, gw=gw: t[:, :, 0:gw], wsrc(w_in, 0, D, col0 + g0, gw))])
                for cc in range(gw // 128):
                    for tc in range(ntok // 512):
                        bk = next_bank(0, 4)
                        for kc in range(16):
                            S.op("pe", lambda e, bk=bk, wt=wt, cc=cc, kc=kc, tc=tc: e.matmul(
                                out=ps[bk][:, :], lhsT=wt[:, kc, cc * 128:(cc + 1) * 128], rhs=hT[:, kc, tc * 512:(tc + 1) * 512],
                                start=(kc == 0), stop=(kc == 15)),
                                reads=[wbuf, bhT], writes=[PB[bk]], signal=(kc == 15))
                        if pending is not None:
                            evac(*pending)
                        pending = (g0 // 128 + cc, tc, ps[bk][:, :], PB[bk])
            if pending is not None:
                evac(*pending)

        def proj_tm(hT, bhT, ntiles, col0, ncols, evac):
            wring = state["wring"]
            for g0 in range(0, ncols, 256):
                gw = min(256, ncols - g0)
                wt, wbuf = wring.load([(lambda t, gw=gw: t[:, :, 0:gw], wsrc(w_in, 0, D, col0 + g0, gw))])
                for tt in range(ntiles):
                    bk = next_bank(0, 4)
                    for kc in range(16):
                        S.op("pe", lambda e, bk=bk, wt=wt, kc=kc, tt=tt, gw=gw: e.matmul(
                            out=ps[bk][:, 0:gw], lhsT=hT[:, kc, tt * 128:(tt + 1) * 128], rhs=wt[:, kc, 0:gw],
                            start=(kc == 0), stop=(kc == 15)),
                            reads=[wbuf, bhT], writes=[PB[bk]], signal=(kc == 15))
                    evac(tt, g0, gw, ps[bk][:, :], PB[bk])

        def stage_slot():
            sl = stg_i[0] % 3
            stg_i[0] += 1
            return sl

        def spill_evac(dram_fn):
            def f(c, tc, pp, bp):
                stg = state["stg"]
                sl = stage_slot()
                if sl % 2:
                    S.op("act", lambda e: e.activation(out=stg[sl], in_=pp, func=AF.Copy), reads=[bp], writes=[B("stg", sl)])
                else:
                    S.op("dve", lambda e: e.tensor_copy(out=stg[sl], in_=pp), reads=[bp], writes=[B("stg", sl)])
                dst, dbuf = dram_fn(c, tc)
                S.dma("sp", dst, stg[sl], reads=[B("stg", sl)], writes=[dbuf], owner=B("stg", sl))
            return f

        def rope_spill_evac(dram_fn, cs):
            def f(c, tc, pp, bp):
                stg = state["stg"]
                sl = stage_slot()
                evac_rope(pp, bp, stg[sl], [B("stg", sl)], cs, tc * 512)
                dst, dbuf = dram_fn(c, tc)
                S.dma("sp", dst, stg[sl], reads=[B("stg", sl)], writes=[dbuf], owner=B("stg", sl))
            return f

        cs_own = AR.alloc([32, 2, NOWN], F32)
        alloc_proj_tmps()
        rope_tables(pos_own, NOWN, cs_own)
        proj_fm(hT_own, bhTo, NOWN, C_QN, 1024,
                rope_spill_evac(lambda c, tc: (scr_qn[c, :, tc * 512:(tc + 1) * 512], B("scr_qn")), cs_own))
        proj_fm(hT_own, bhTo, NOWN, C_QS, 1024,
                spill_evac(lambda c, tc: (scr_qsb[c, :, tc * 512:(tc + 1) * 512], B("scr_qsb"))))
        proj_tm(hT_own, bhTo, 8, C_G, 24,
                lambda tt, g0, gw, pp, bp: S.op("act", lambda e: e.activation(out=gates_sb[:, tt, :], in_=pp[:, 0:24], func=AF.Sigmoid),
                                               reads=[bp], writes=[B("gates")]))
        tap("gates", gates_sb, [B("gates")])
        if stop_after <= 1:
            S.emit(); return nc
        S.barrier()
        AR.release(m_A)

        cs_nat = AR.alloc([32, 2, T], F32)
        alloc_proj_tmps()
        rope_tables(pos_nat, T, cs_nat)
        proj_fm(hT_nat, bhTn, T, C_KSB, 1024,
                spill_evac(lambda c, tc: (scr_ksb[c, :, tc * 512:(tc + 1) * 512], B("scr_ksb"))))

        def vsb_evac(tt, g0, gw, pp, bp):
            stg = state["stg"]
            sl = stage_slot()
            S.op("dve", lambda e: e.tensor_copy(out=stg[sl][:, 0:gw], in_=pp[:, 0:gw]), reads=[bp], writes=[B("stg", sl)])
            S.dma("sp", scr_vsb[:, tt, g0:g0 + gw], stg[sl][:, 0:gw], reads=[B("stg", sl)], writes=[B("scr_vsb")], owner=B("stg", sl))
        proj_tm(hT_nat, bhTn, 16, C_VSB, 1024, vsb_evac)
        if stop_after <= 1.5:
            S.emit(); return nc

        tokT = AR.alloc([128, 2, T], BF16)
        w1c = AR.alloc([128, 32, 128], BF16)
        w2c = AR.alloc([128, 128], BF16)
        peT = AR.alloc([128, 32], BF16)
        pe_tm = AR.alloc([32, 128], BF16)
        cbias = AR.alloc([128, 2], F32)
        hidT = AR.alloc([128, 128], BF16)

        S.op("pool", lambda e: e.memset(Vx_slc.rearrange("p a b c -> p (a b c)"), 1.0), writes=[B("Vx_slc")])
        S.op("pool", lambda e: e.memset(Vx_win.rearrange("p a b c -> p (a b c)"), 1.0), writes=[B("Vx_win")])
        S.op("pool", lambda e: e.memset(kcT.rearrange("p a b -> p (a b)"), 0.0), writes=[B("kcT")])
        S.op("pool", lambda e: e.memset(vcx.rearrange("p a b -> p (a b)"), 0.0), writes=[B("vcx")])
        proj_fm(hT_nat, bhTn, T, C_KS, 256,
                lambda c, tc, pp, bp: evac_rope(pp, bp, KT_slc[:, c, tc * 512:(tc + 1) * 512], [B("KT_slc")], cs_nat, tc * 512))
        proj_fm(hT_nat, bhTn, T, C_KW, 256,
                lambda c, tc, pp, bp: evac_rope(pp, bp, KT_win[:, c, tc * 512:(tc + 1) * 512], [B("KT_win")], cs_nat, tc * 512))

        def v_evac(Vx, vb):
            def f(tt, g0, gw, pp, bp):
                S.op("dve", lambda e: e.tensor_copy(out=Vx[:, tt, :, 0:128], in_=pp[:, 0:256].rearrange("p (h d) -> p h d", h=2)),
                     reads=[bp], writes=[vb])
            return f
        proj_tm(hT_nat, bhTn, 16, C_VS, 256, v_evac(Vx_slc, B("Vx_slc")))
        proj_tm(hT_nat, bhTn, 16, C_VW, 256, v_evac(Vx_win, B("Vx_win")))

        bcw = B("cmpw")
        btok = B("tokT")
        for kv in range(2):
            if kv == 0:
                proj_fm(hT_nat, bhTn, T, C_KC, 256,
                        lambda c, tc, pp, bp: evac_rope(pp, bp, tokT[:, c, tc * 512:(tc + 1) * 512], [btok], cs_nat, tc * 512))
            else:
                proj_fm(hT_nat, bhTn, T, C_VC, 256,
                        lambda c, tc, pp, bp: S.op("act", lambda e: e.activation(out=tokT[:, c, tc * 512:(tc + 1) * 512], in_=pp, func=AF.Copy),
                                                   reads=[bp], writes=[btok]))
            w1d, w2d, ped = ((cmp_w1_k, cmp_w2_k, cmp_pe_k), (cmp_w1_v, cmp_w2_v, cmp_pe_v))[kv]
            S.dma("pool", w1c, w1d.rearrange("(l p) f -> p l f", p=128), writes=[bcw])
            S.dma("pool", w2c, w2d[:, :], writes=[bcw])
            S.dma("pool", pe_tm, ped[:, :], writes=[bcw])
            pst = ps[4].bitcast(BF16)
            S.op("pe", lambda e, pst=pst: e.transpose(out=pst[:, 0:32], in_=pe_tm, identity=ident[0:32, 0:32]),
                 reads=[bcw, bC], writes=[PB[4]])
            S.op("dve", lambda e, pst=pst: e.tensor_copy(out=peT, in_=pst[:, 0:32]), reads=[PB[4]], writes=[B("peT")])
            for l in range(32):
                S.op("pe", lambda e, l=l: e.matmul(out=ps[5][:, 0:1], lhsT=w1c[:, l, :], rhs=peT[:, l:l + 1], start=(l == 0), stop=(l == 31)),
                     reads=[bcw, B("peT")], writes=[PB[5]], signal=(l == 31))
            S.op("dve", lambda e, kv=kv: e.tensor_copy(out=cbias[:, kv:kv + 1], in_=ps[5][:, 0:1]), reads=[PB[5]], writes=[B("cbias")])
            for hh in range(2):
                bk = next_bank(0, 4)
                for l in range(32):
                    S.op("pe", lambda e, bk=bk, l=l, hh=hh: e.matmul(
                        out=ps[bk][:, 0:127], lhsT=w1c[:, l, :], rhs=tokT[:, hh, l:l + 16 * 126 + 1:16], start=(l == 0), stop=(l == 31)),
                        reads=[bcw, btok], writes=[PB[bk]], signal=(l == 31))
                S.op("act", lambda e, bk=bk, kv=kv: e.activation(out=hidT[:, 0:127], in_=ps[bk][:, 0:127], func=AF.Silu, bias=cbias[:, kv:kv + 1]),
                     reads=[PB[bk], B("cbias")], writes=[B("hidT")])
                bk2 = next_bank(0, 4)
                if kv == 0:
                    S.op("pe", lambda e, bk2=bk2: e.matmul(out=ps[bk2][:, 0:127], lhsT=w2c, rhs=hidT[:, 0:127], start=True, stop=True),
                         reads=[bcw, B("hidT")], writes=[PB[bk2]])
                    S.op("dve", lambda e, bk2=bk2, hh=hh: e.tensor_copy(out=kcT[:, hh, 0:127], in_=ps[bk2][:, 0:127]), reads=[PB[bk2]], writes=[B("kcT")])
                else:
                    S.op("pe", lambda e, bk2=bk2: e.matmul(out=ps[bk2][0:127, 0:128], lhsT=hidT[:, 0:127], rhs=w2c, start=True, stop=True),
                         reads=[bcw, B("hidT")], writes=[PB[bk2]])
                    S.op("dve", lambda e, bk2=bk2, hh=hh: e.tensor_copy(out=vcx[0:127, hh, 0:128], in_=ps[bk2][0:127, 0:128]),
                         reads=[PB[bk2]], writes=[B("vcx")])
        for hh in range(2):
            S.op("dve", lambda e, hh=hh: e.tensor_copy(out=vcx[:, hh, 128:161], in_=ovl), reads=[bC], writes=[B("vcx")])
        tap("kcT", kcT, [B("kcT")]); tap("vcx", vcx, [B("vcx")]); tap("KT_slc", KT_slc, [B("KT_slc")]); tap("Vx_slc", Vx_slc, [B("Vx_slc")])
        if stop_after <= 2:
            S.emit(); return nc
        S.barrier()
        AR.release(m_nsa)

        oT_nsa = AR.alloc([128, 8, NOWN], BF16, top=True)
        oT_sb = AR.alloc([128, 8, NOWN], BF16, top=True)
        QT_nsa = AR.alloc([128, 8, NOWN], BF16)
        for hq in range(8):
            S.dma("sp", QT_nsa[:, hq, :], scr_qn[hq, :, :], reads=[B("scr_qn")], writes=[B("QT_nsa", hq)])
        tap("QT_nsa", QT_nsa, [B("QT_nsa", c) for c in range(8)])
        o_acc = AR.alloc([128, 8, 4, 128], F32)
        o_bf = AR.alloc([128, 8, 4, 128], BF16)
        imp = AR.alloc([128, 8, 32], F32)
        imp_tmp = AR.alloc([128, 4, 32], F32)
        sc = AR.alloc([128, 8, 64], F32)
        m8 = AR.alloc([128, 8, 16], F32)
        sel = AR.alloc([128, 8, 32], F32)
        mbT = AR.alloc([32, NOWN], BF16)
        coef = AR.alloc([128, 512], F32)
        eT = [AR.alloc([128, 512], BF16) for _ in range(2)]
        PT = [AR.alloc([128, 2, 256], BF16) for _ in range(3)]
        coef_i = [0]

        def coef_slot():
            i = coef_i[0] % 128
            coef_i[0] += 1
            return i * 4, B("coef", i)

        pt_i = [0]
        for kvh in range(2):
            for g in range(4):
                head = kvh * 4 + g
                for tc in range(2):
                    bk = next_bank(0, 3)
                    S.op("pe", lambda e: e.matmul(out=ps[bk][:, :], lhsT=kcT[:, kvh, :], rhs=QT_nsa[:, head, tc * 512:(tc + 1) * 512], start=True, stop=False),
                         reads=[B("kcT"), B("QT_nsa", head)], writes=[PB[bk]], signal=False)
                    S.op("pe", lambda e: e.matmul(out=ps[bk][:, :], lhsT=ident, rhs=cmpb[:, tc * 512:(tc + 1) * 512], start=False, stop=True),
                         reads=[bC], writes=[PB[bk]])
                    sl = pt_i[0] % 2
                    pt_i[0] += 1
                    S.op("act", lambda e: e.activation(out=eT[sl], in_=ps[bk][:, :], func=AF.Exp, scale=SCALE),
                         reads=[PB[bk]], writes=[B("eT", sl)])
                    par = (g * 2 + tc) % 2
                    bo_o = 3 + par
                    bo_i = 5 + par
                    for j in range(4):
                        S.op("pe", lambda e: e.matmul(out=ps[bo_o][:, j * 128:(j + 1) * 128], lhsT=eT[sl][:, j * 128:(j + 1) * 128], rhs=vcx[:, kvh, 0:128],
                                                      start=True, stop=True),
                             reads=[B("eT", sl), B("vcx")], writes=[PB[bo_o]], signal=(j == 3))
                    for j in range(4):
                        S.op("pe", lambda e: e.matmul(out=ps[bo_i][:, j * 33:(j + 1) * 33], lhsT=eT[sl][:, j * 128:(j + 1) * 128], rhs=vcx[:, kvh, 128:161],
                                                      start=True, stop=True),
                             reads=[B("eT", sl), B("vcx")], writes=[PB[bo_i]], signal=(j == 3))
                    ca, bca = coef_slot(); cb_, bcb = coef_slot(); cg_, bcg = coef_slot()
                    pi3 = ps[bo_i][:, 0:132].rearrange("p (j c) -> p j c", j=4)
                    po3 = ps[bo_o][:, :].rearrange("p (j c) -> p j c", j=4)
                    tis = list(range(tc * 4, tc * 4 + 4))
                    S.op("dve", lambda e: e.tensor_scalar(out=coef[:, ca:ca + 4], in0=pi3[:, :, 32], scalar1=1e-30, scalar2=None, op0=ALU.max),
                         reads=[PB[bo_i]], writes=[bca])
                    S.op("dve", lambda e: e.reciprocal(out=coef[:, cb_:cb_ + 4], in_=coef[:, ca:ca + 4]), reads=[bca], writes=[bcb])
                    S.op("dve", lambda e: e.tensor_tensor(out=coef[:, cg_:cg_ + 4], in0=coef[:, cb_:cb_ + 4], in1=gates_sb[:, tc * 4:(tc + 1) * 4, head * 3],
                                                          op=ALU.mult), reads=[bcb, B("gates")], writes=[bcg])
                    bimps = [B("imp", ti) for ti in tis]
                    rden_b = coef[:, cb_:cb_ + 4].unsqueeze(2).to_broadcast([128, 4, 32])
                    if g == 0:
                        S.op("dve", lambda e: e.tensor_tensor(out=imp[:, tc * 4:(tc + 1) * 4, :], in0=pi3[:, :, 0:32], in1=rden_b, op=ALU.mult),
                             reads=[PB[bo_i], bcb], writes=bimps)
                    else:
                        S.op("dve", lambda e: e.tensor_tensor(out=imp_tmp, in0=pi3[:, :, 0:32], in1=rden_b, op=ALU.mult),
                             reads=[PB[bo_i], bcb], writes=[B("imp_tmp")])
                        S.op("dve", lambda e: e.tensor_tensor(out=imp[:, tc * 4:(tc + 1) * 4, :], in0=imp[:, tc * 4:(tc + 1) * 4, :], in1=imp_tmp, op=ALU.add),
                             reads=[B("imp_tmp")] + bimps, writes=bimps)
                    cg_b = coef[:, cg_:cg_ + 4].unsqueeze(2).to_broadcast([128, 4, 128])
                    S.op("dve", lambda e: e.tensor_tensor(out=o_acc[:, tc * 4:(tc + 1) * 4, g, :], in0=po3, in1=cg_b, op=ALU.mult),
                         reads=[PB[bo_o], bcg], writes=[B("o_acc", ti, g) for ti in tis])
            for ti in range(8):
                bimp = B("imp", ti)
                S.op("dve", lambda e, ti=ti: e.tensor_tensor(out=sc[:, ti, 0:32], in0=imp[:, ti, :], in1=fb[:, ti, :], op=ALU.add),
                     reads=[bimp, bC], writes=[B("sc", ti)])
                S.op("dve", lambda e, ti=ti: e.max(out=m8[:, ti, 0:8], in_=sc[:, ti, 0:32]), reads=[B("sc", ti)], writes=[B("m8", ti)])
                S.op("dve", lambda e, ti=ti: e.match_replace(out=sc[:, ti, 32:64], in_to_replace=m8[:, ti, 0:8], in_values=sc[:, ti, 0:32], imm_value=-3e9),
                     reads=[B("sc", ti), B("m8", ti)], writes=[B("sc2", ti)])
                S.op("dve", lambda e, ti=ti: e.max(out=m8[:, ti, 8:16], in_=sc[:, ti, 32:64]), reads=[B("sc2", ti)], writes=[B("m8", ti)])
                S.op("dve", lambda e, ti=ti: e.tensor_scalar(out=sel[:, ti, :], in0=sc[:, ti, 0:32], scalar1=m8[:, ti, 15:16], scalar2=None, op0=ALU.is_ge),
                     reads=[B("sc", ti), B("m8", ti)], writes=[B("sel", ti)])
                S.op("dve", lambda e, ti=ti: e.tensor_tensor(out=sel[:, ti, :], in0=sel[:, ti, :], in1=vm[:, ti, :], op=ALU.mult),
                     reads=[B("sel", ti), bC], writes=[B("sel", ti)])
                bk = next_bank(0, 3)
                S.op("pe", lambda e, bk=bk, ti=ti: e.transpose(out=ps[bk][0:32, 0:128], in_=sel[:, ti, :], identity=identf),
                     reads=[B("sel", ti), bC], writes=[PB[bk]])
                S.op("dve", lambda e, bk=bk, ti=ti: e.tensor_scalar(out=mbT[:, ti * 128:(ti + 1) * 128], in0=ps[bk][0:32, 0:128], scalar1=-NEG, scalar2=NEG,
                                                                   op0=ALU.mult, op1=ALU.add),
                     reads=[PB[bk]], writes=[B("mbT")])
            if kvh == 0:
                tap("imp", imp, [B("imp", ti) for ti in range(8)])
                tap("sel", sel, [B("sel", ti) for ti in range(8)])
                if stop_after <= 2.5:
                    S.emit(); return nc
            items = []
            seq = 0
            for g in range(4):
                for p in range(4):
                    for branch in range(2):
                        tiles = list(range(0, 4 * p + 4)) if branch == 0 else list(range(max(0, 4 * p - 4), 4 * p + 4))
                        npairs = len(tiles) // 2
                        for m in range(npairs):
                            items.append(dict(g=g, p=p, branch=branch, tiles=tiles, m=m, npairs=npairs, seq=seq))
                        seq += 1

            def sel_front(it, i):
                g, p, branch, tiles, m = it["g"], it["p"], it["branch"], it["tiles"], it["m"]
                head = kvh * 4 + g
                KT = KT_slc if branch == 0 else KT_win
                bKT = B("KT_slc") if branch == 0 else B("KT_win")
                bk = i % 3
                sl = i % 3
                for s in range(2):
                    G = tiles[2 * m + s]
                    o_ap = ps[bk][:, s * 256:(s + 1) * 256]
                    extra = []
                    if branch == 0:
                        extra.append((Emat[:, G, :], mbT[:, p * 256:(p + 1) * 256], [bC, B("mbT")]))
                        if G >= 4 * p:
                            extra.append((ident, cbi[:, G - 4 * p, :], [bC]))
                    else:
                        extra.append((ident, wbm[:, G - (4 * p - 4), :], [bC]))
                    S.op("pe", lambda e: e.matmul(out=o_ap, lhsT=KT[:, kvh, G * 128:(G + 1) * 128], rhs=QT_nsa[:, head, p * 256:(p + 1) * 256],
                                                  start=True, stop=False),
                         reads=[bKT, B("QT_nsa", head)], writes=[PB[bk]], signal=False)
                    for xi, (l_ap, r_ap, rb) in enumerate(extra):
                        last = xi == len(extra) - 1
                        S.op("pe", lambda e: e.matmul(out=o_ap, lhsT=l_ap, rhs=r_ap, start=False, stop=last),
                             reads=rb, writes=[PB[bk]], signal=(last and s == 1))

            def sel_exp(it, i):
                bk = i % 3
                sl = i % 3
                S.op("act", lambda e: e.activation(out=PT[sl].rearrange("p a b -> p (a b)"), in_=ps[bk][:, :], func=AF.Exp, scale=SCALE),
                     reads=[PB[bk]], writes=[B("PT", sl)])

            def sel_back(it, i):
                g, p, branch, tiles, m, npairs = it["g"], it["p"], it["branch"], it["tiles"], it["m"], it["npairs"]
                head = kvh * 4 + g
                Vx = Vx_slc if branch == 0 else Vx_win
                bVx = B("Vx_slc") if branch == 0 else B("Vx_win")
                sl = i % 3
                bo0 = 3 + 2 * (it["seq"] % 2)
                for s in range(2):
                    G = tiles[2 * m + s]
                    for j in range(2):
                        first = (m == 0 and s == 0)
                        last = (m == npairs - 1 and s == 1)
                        bo = bo0 + j
                        S.op("pe", lambda e: e.matmul(out=ps[bo][:, 0:129], lhsT=PT[sl][:, s, j * 128:(j + 1) * 128], rhs=Vx[:, G, kvh, 0:129],
                                                      start=first, stop=last),
                             reads=[B("PT", sl), bVx], writes=[PB[bo]], signal=last)
                if m != npairs - 1:
                    return
                for j in range(2):
                    ti = 2 * p + j
                    bo = bo0 + j
                    c0, bc = coef_slot()
                    S.op("dve", lambda e: e.tensor_scalar(out=coef[:, c0:c0 + 1], in0=ps[bo][:, 128:129], scalar1=1e-30,
                                                          scalar2=None, op0=ALU.max), reads=[PB[bo]], writes=[bc])
                    S.op("dve", lambda e: e.reciprocal(out=coef[:, c0 + 1:c0 + 2], in_=coef[:, c0:c0 + 1]), reads=[bc], writes=[bc])
                    S.op("dve", lambda e: e.tensor_tensor(out=coef[:, c0 + 2:c0 + 3], in0=coef[:, c0 + 1:c0 + 2],
                                                          in1=gates_sb[:, ti, head * 3 + 1 + branch:head * 3 + 2 + branch], op=ALU.mult),
                         reads=[bc, B("gates")], writes=[bc])
                    dst = o_acc[:, ti, g, :] if branch == 0 else o_bf[:, ti, g, :]
                    dbuf = B("o_acc", ti, g) if branch == 0 else B("o_bf", ti, g)
                    S.op("dve", lambda e: e.scalar_tensor_tensor(out=dst, in0=ps[bo][:, 0:128], scalar=coef[:, c0 + 2:c0 + 3],
                                                                 in1=o_acc[:, ti, g, :], op0=ALU.mult, op1=ALU.add),
                         reads=[PB[bo], bc, B("o_acc", ti, g)], writes=[dbuf])
                if p == 3 and branch == 1:
                    for half in range(2):
                        bkt = 7
                        pst = ps[bkt].bitcast(BF16)
                        for j in range(4):
                            ti = half * 4 + j
                            S.op("pe", lambda e: e.transpose(out=pst[:, j * 128:(j + 1) * 128], in_=o_bf[:, ti, g, :], identity=ident),
                                 reads=[B("o_bf", ti, g), bC], writes=[PB[bkt]], signal=(j == 3))
                        S.op("act", lambda e: e.activation(out=oT_nsa[:, head, half * 512:(half + 1) * 512], in_=pst[:, 0:512], func=AF.Copy),
                             reads=[PB[bkt]], writes=[B("oT_nsa", head)])

            sel_front(items[0], 0)
            sel_front(items[1], 1)
            sel_exp(items[0], 0)
            for i, it in enumerate(items):
                if i + 2 < len(items):
                    sel_front(items[i + 2], i + 2)
                if i + 1 < len(items):
                    sel_exp(items[i + 1], i + 1)
                sel_back(it, i)
        tap("oT_nsa", oT_nsa, [B("oT_nsa", h) for h in range(8)])
        if stop_after <= 3:
            S.emit(); return nc
        S.barrier()
        AR.release(m_nsa)

        sbq = [AR.alloc([128, NOWN], BF16) for _ in range(2)]
        sbk = [AR.alloc([128, T], BF16) for _ in range(2)]
        sbv = [AR.alloc([128, 16, 128], BF16) for _ in range(2)]
        e_sb = [AR.alloc([128, 512], F32) for _ in range(2)]
        sp_sb = [AR.alloc([128, 512], F32) for _ in range(2)]
        spb = [AR.alloc([128, 2, 256], BF16) for _ in range(2)]
        t_sb = [AR.alloc([128, 2, 256], F32) for _ in range(2)]
        AT = [AR.alloc([128, 2, 256], BF16) for _ in range(2)]
        Rsum = AR.alloc([128, 256], F32)
        items = []
        for head in range(8):
            for p in range(4):
                npairs = 2 * p + 2
                for mi, m in enumerate(range(npairs - 1, -1, -1)):
                    items.append(dict(head=head, p=p, mi=mi, m=m, npairs=npairs))
        loaded = set()

        def sb_s1(it, i):
            head, p, mi, m = it["head"], it["p"], it["mi"], it["m"]
            hs = head % 2
            if head not in loaded:
                loaded.add(head)
                S.dma("sp", sbq[hs], scr_qsb[head, :, :], reads=[B("scr_qsb")], writes=[B("sbq", hs)])
                S.dma("sp", sbk[hs], scr_ksb[head, :, :], reads=[B("scr_ksb")], writes=[B("sbk", hs)])
                S.dma("sp", sbv[hs], scr_vsb[:, :, head * 128:(head + 1) * 128], reads=[B("scr_vsb")], writes=[B("sbv", hs)])
            bz = i % 3
            for s in range(2):
                G = 2 * m + s
                o_ap = ps[bz][:, s * 256:(s + 1) * 256]
                diag = G >= 4 * p
                S.op("pe", lambda e: e.matmul(out=o_ap, lhsT=sbk[hs][:, G * 128:(G + 1) * 128], rhs=sbq[hs][:, p * 256:(p + 1) * 256],
                                              start=True, stop=(not diag)),
                     reads=[B("sbk", hs), B("sbq", hs)], writes=[PB[bz]], signal=(s == 1 and not diag))
                if diag:
                    S.op("pe", lambda e: e.matmul(out=o_ap, lhsT=ident, rhs=cbs[:, G - 4 * p, :], start=False, stop=True),
                         reads=[bC], writes=[PB[bz]], signal=(s == 1))

        sb_dve_cast = False
        SB_BF16_SP = True
        sb_cast_pending = []

        def sb_s2(it, i):
            sl = i % 2
            bz = i % 3
            bcn = 3 + i % 2
            btot = 5
            S.op("act", lambda e: e.activation(out=e_sb[sl], in_=ps[bz][:, :], func=AF.Exp, scale=SCALE),
                 reads=[PB[bz]], writes=[B("e_sb", sl)])
            if SB_BF16_SP:
                S.op("act", lambda e: e.activation(out=spb[sl].rearrange("p a b -> p (a b)"), in_=e_sb[sl], func=AF.Ln, bias=1.0),
                     reads=[B("e_sb", sl)], writes=[B("spb", sl)])
                sb_s2b(it, i)
                return
            S.op("act", lambda e: e.activation(out=sp_sb[sl], in_=e_sb[sl], func=AF.Ln, bias=1.0),
                 reads=[B("e_sb", sl)], writes=[B("sp_sb", sl)])
            if not sb_dve_cast:
                S.op("act", lambda e: e.activation(out=spb[sl].rearrange("p a b -> p (a b)"), in_=e_sb[sl], func=AF.Ln, bias=1.0),
                     reads=[B("e_sb", sl)], writes=[B("spb", sl)])
            else:
                sb_cast_pending.append((sl, it, i))
                return
            sb_s2b(it, i)

        def sb_s2b(it, i):
            sl = i % 2
            bcn = 3 + i % 2
            btot = 5
            S.op("pe", lambda e: e.matmul(out=ps[bcn][:, 256:512], lhsT=negtri, rhs=spb[sl][:, 1, :], start=True, stop=True),
                 reads=[bC, B("spb", sl)], writes=[PB[bcn]], signal=False)
            S.op("pe", lambda e: e.matmul(out=ps[bcn][:, 0:256], lhsT=negtri, rhs=spb[sl][:, 0, :], start=True, stop=False),
                 reads=[bC, B("spb", sl)], writes=[PB[bcn]], signal=False)
            S.op("pe", lambda e: e.matmul(out=ps[bcn][:, 0:256], lhsT=negones, rhs=spb[sl][:, 1, :], start=False, stop=True),
                 reads=[bC, B("spb", sl)], writes=[PB[bcn]])
            if it["mi"] < it["npairs"] - 1:
                S.op("pe", lambda e: e.matmul(out=ps[btot][:, 0:256], lhsT=negones, rhs=spb[sl][:, 0, :], start=True, stop=False),
                     reads=[bC, B("spb", sl)], writes=[PB[btot]], signal=False)
                S.op("pe", lambda e: e.matmul(out=ps[btot][:, 0:256], lhsT=negones, rhs=spb[sl][:, 1, :], start=False, stop=True),
                     reads=[bC, B("spb", sl)], writes=[PB[btot]])

        s3_dve_done = set()

        def sb_s3_dve(it, i):
            if i in s3_dve_done:
                return
            s3_dve_done.add(i)
            sl = i % 2
            bz = i % 3
            bcn = 3 + i % 2
            tf = t_sb[sl].rearrange("p a b -> p (a b)")
            if SB_BF16_SP and it["mi"] == 0:
                spo, spbuf = spb[sl].rearrange("p a b -> p (a b)"), B("spb", sl)
            else:
                spo, spbuf = sp_sb[sl], B("sp_sb", sl)
            S.op("dve", lambda e: e.scalar_tensor_tensor(out=tf, in0=ps[bz][:, :], scalar=SCALE, in1=spo, op0=ALU.mult, op1=ALU.subtract),
                 reads=[PB[bz], spbuf], writes=[B("t_sb", sl)])
            S.op("dve", lambda e: e.tensor_tensor(out=tf, in0=tf, in1=ps[bcn][:, :], op=ALU.add),
                 reads=[PB[bcn], B("t_sb", sl)], writes=[B("t_sb", sl)])

        def sb_s3(it, i):
            head, p, mi, m, npairs = it["head"], it["p"], it["mi"], it["m"], it["npairs"]
            hs = head % 2
            sl = i % 2
            bo = 6 + (p % 2)
            tf = t_sb[sl].rearrange("p a b -> p (a b)")
            sb_s3_dve(it, i)
            S.op("act", lambda e: e.activation(out=AT[sl].rearrange("p a b -> p (a b)"), in_=tf, func=AF.Exp),
                 reads=[B("t_sb", sl)], writes=[B("AT", sl)])
            for s in range(2):
                G = 2 * m + s
                first = (mi == 0 and s == 0)
                last = (mi == npairs - 1 and s == 1)
                S.op("pe", lambda e: e.matmul(out=ps[bo][:, 0:256], lhsT=sbv[hs][:, G, :], rhs=AT[sl][:, s, :], start=first, stop=last),
                     reads=[B("sbv", hs), B("AT", sl)], writes=[PB[bo]], signal=last)
            if mi == npairs - 1:
                S.op("act", lambda e: e.activation(out=oT_sb[:, head, p * 256:(p + 1) * 256], in_=ps[bo][:, 0:256], func=AF.Copy),
                     reads=[PB[bo]], writes=[B("oT_sb", head)])

        n_it = len(items)
        sb_s1(items[0], 0)
        sb_s1(items[1], 1)
        sb_s2(items[0], 0)
        if sb_dve_cast:
            slc, itc, ic = sb_cast_pending.pop()
            S.op("dve", lambda e: e.tensor_copy(out=spb[slc].rearrange("p a b -> p (a b)"), in_=sp_sb[slc]),
                 reads=[B("sp_sb", slc)], writes=[B("spb", slc)])
            sb_s2b(itc, ic)
        for i, it in enumerate(items):
            if it["mi"] == 0:
                S.op("dve", lambda e: e.tensor_copy(out=Rsum, in_=ps[5][:, 0:256]), reads=[PB[5]], writes=[B("Rsum")])
            elif it["mi"] < it["npairs"] - 1:
                S.op("dve", lambda e: e.tensor_tensor(out=Rsum, in0=Rsum, in1=ps[5][:, 0:256], op=ALU.add),
                     reads=[PB[5], B("Rsum")], writes=[B("Rsum")])
            if i + 2 < n_it:
                sb_s1(items[i + 2], i + 2)
            if i + 1 < n_it:
                sb_s2(items[i + 1], i + 1)
                if sb_dve_cast:
                    sb_s3_dve(it, i)
                    slc, itc, ic = sb_cast_pending.pop()
                    S.op("dve", lambda e: e.tensor_copy(out=spb[slc].rearrange("p a b -> p (a b)"), in_=sp_sb[slc]),
                         reads=[B("sp_sb", slc)], writes=[B("spb", slc)])
                    sb_s2b(itc, ic)
                if items[i + 1]["mi"] > 0:
                    sn = (i + 1) % 2
                    if SB_BF16_SP:
                        S.op("pool", lambda e: e.tensor_tensor(out=sp_sb[sn].rearrange("p (a b) -> p a b", a=2), in0=spb[sn],
                                                               in1=Rsum.unsqueeze(1).to_broadcast([128, 2, 256]), op=ALU.subtract),
                             reads=[B("spb", sn), B("Rsum")], writes=[B("sp_sb", sn)])
                    else:
                        S.op("pool", lambda e: e.tensor_tensor(out=sp_sb[sn].rearrange("p (a b) -> p a b", a=2), in0=sp_sb[sn].rearrange("p (a b) -> p a b", a=2),
                                                               in1=Rsum.unsqueeze(1).to_broadcast([128, 2, 256]), op=ALU.subtract),
                             reads=[B("sp_sb", sn), B("Rsum")], writes=[B("sp_sb", sn)])
            sb_s3(it, i)
        tap("oT_sb", oT_sb, [B("oT_sb", h) for h in range(8)])
        if stop_after <= 4:
            S.emit(); return nc
        S.barrier()
        AR.release(m_ess)

        mT = AR.alloc([128, 16, NOWN], BF16, top=True)
        hT2 = AR.alloc([128, 16, NOWN], BF16)
        bhT2 = B("hT2")
        S.dma("sp", hT2, scr_hTown[:, :, :], reads=[B("scr_hTown")], writes=[bhT2])
        wg = Ring("wg", 4, [128, 16, 128])
        wb_ = Ring("wb", 4, [128, 8, 128])
        gsb = [AR.alloc([128, 512], F32) for _ in range(2)]
        ysb = [AR.alloc([128, 512], F32) for _ in range(2)]
        gi = [0]
        for fc in range(16):
            wga, bga = wg.load([(lambda t: t, wsrc(w_in, 0, D, C_GA + fc * 128, 128))])
            wgb, bgb = wg.load([(lambda t: t, wsrc(w_in, 0, D, C_GB + fc * 128, 128))])
            wba, bba = wb_.load([(lambda t: t, wsrc(w_br_nsa, 0, 1024, fc * 128, 128))])
            wbb, bbb = wb_.load([(lambda t: t, wsrc(w_br_sb, 0, 1024, fc * 128, 128))])
            for tc in range(2):
                tsl = slice(tc * 512, (tc + 1) * 512)
                res = []
                for (wgt, bwg, wbr, bwb, oT, obn) in ((wga, bga, wba, bba, oT_nsa, "oT_nsa"), (wgb, bgb, wbb, bbb, oT_sb, "oT_sb")):
                    bkg = next_bank(0, 4)
                    for kc in range(16):
                        S.op("pe", lambda e, bkg=bkg, wgt=wgt, kc=kc, tsl=tsl: e.matmul(out=ps[bkg][:, :], lhsT=wgt[:, kc, :], rhs=hT2[:, kc, tsl],
                                                                                    start=(kc == 0), stop=(kc == 15)),
                             reads=[bwg, bhT2], writes=[PB[bkg]], signal=(kc == 15))
                    sl = gi[0] % 2
                    S.op("act", lambda e, bkg=bkg, sl=sl: e.activation(out=gsb[sl], in_=ps[bkg][:, :], func=AF.Sigmoid), reads=[PB[bkg]], writes=[B("gsb", sl)])
                    bky = 4 + (gi[0] % 4)
                    gi[0] += 1
                    for kc in range(8):
                        S.op("pe", lambda e, bky=bky, wbr=wbr, kc=kc, tsl=tsl, oT=oT: e.matmul(out=ps[bky][:, :], lhsT=wbr[:, kc, :], rhs=oT[:, kc, tsl],
                                                                                           start=(kc == 0), stop=(kc == 7)),
                             reads=[bwb] + [B(obn, h) for h in range(8)], writes=[PB[bky]], signal=(kc == 7))
                    res.append((sl, bky))
                (sa, ya), (sb_, yb) = res
                S.op("dve", lambda e, sa=sa, ya=ya: e.tensor_tensor(out=ysb[0], in0=gsb[sa], in1=ps[ya][:, :], op=ALU.mult),
                     reads=[B("gsb", sa), PB[ya]], writes=[B("ysb", 0)])
                S.op("dve", lambda e, sb_=sb_, yb=yb: e.tensor_tensor(out=ysb[1], in0=gsb[sb_], in1=ps[yb][:, :], op=ALU.mult),
                     reads=[B("gsb", sb_), PB[yb]], writes=[B("ysb", 1)])
                S.op("dve", lambda e, fc=fc, tsl=tsl: e.tensor_tensor(out=mT[:, fc, tsl], in0=ysb[0], in1=ysb[1], op=ALU.add),
                     reads=[B("ysb", 0), B("ysb", 1)], writes=[B("mT")])
        tap("mT", mT, [B("mT")])
        wo_start = AR.mark()
        wo_sb = AR.alloc([128, 16, D], BF16)
        wo_end = AR.mark()
        for q4 in range(4):
            S.dma("pool", wo_sb[:, q4 * 4:(q4 + 1) * 4, :], wsrc(w_o, q4 * 512, 512, 0, D), writes=[B("wo", q4)])
        S.barrier(exclude=[B("wo", q4) for q4 in range(4)])
        AR.release(m_ess)
        x1 = AR.alloc([128, 8, D], F32)
        m_res = AR.mark()
        assert AR.mark() <= wo_start, (AR.mark(), wo_start)
        AR.off = wo_end
        S.dma("sp", x1, x_own.rearrange("(a p) d -> p a d", p=128), writes=[B("x1")])
        for tt in range(8):
            for dc in range(4):
                bk = next_bank(0, 4)
                for kc in range(16):
                    S.op("pe", lambda e, bk=bk, kc=kc, tt=tt, dc=dc: e.matmul(out=ps[bk][:, :], lhsT=mT[:, kc, tt * 128:(tt + 1) * 128], rhs=wo_sb[:, kc, dc * 512:(dc + 1) * 512],
                                                                         start=(kc == 0), stop=(kc == 15)),
                         reads=[B("mT"), B("wo", kc // 4)], writes=[PB[bk]], signal=(kc == 15))
                S.op("dve", lambda e, bk=bk, tt=tt, dc=dc: e.tensor_tensor(out=x1[:, tt, dc * 512:(dc + 1) * 512], in0=x1[:, tt, dc * 512:(dc + 1) * 512], in1=ps[bk][:, :], op=ALU.add),
                     reads=[PB[bk], B("x1")], writes=[B("x1", tt)])
        tap("x1", x1, [B("x1", tt) for tt in range(8)])
        if stop_after <= 5:
            S.emit(); return nc
        S.barrier()
        AR.release(m_res)
        AR.release_top()

        hT3 = AR.alloc([128, 16, NOWN], BF16); bhT3 = B("hT3")
        m_I = AR.mark()
        QcT = AR.alloc([128, 4, NOWN], BF16)
        KcT = AR.alloc([128, 4, 256], BF16)
        Vcx = AR.alloc([128, 2, 4, 130], BF16)
        oc_bf = AR.alloc([128, 8, 512], BF16)
        ocT = AR.alloc([128, 4, NOWN], BF16)
        wco = AR.alloc([128, 4, D], BF16)
        PTc = [AR.alloc([128, 2, 512], BF16) for _ in range(2)]
        coefx = AR.alloc([128, 256], F32)
        memT = AR.alloc([128, 16, 256], BF16)
        m_I1 = AR.mark()
        n_xt = [AR.alloc([128, D], F32) for _ in range(2)]
        n_junk = [AR.alloc([128, D], BF16) for _ in range(2)]
        n_hb = [AR.alloc([128, D], BF16) for _ in range(2)]

        def x1_tiles(tt):
            return x1[:, tt, :], B("x1", tt)
        cnt_t = [0]

        def sinkT(dstT, dbuf):
            def f(tt, hb, bh):
                pb = (cnt_t[0] % 2) * 2
                cnt_t[0] += 1
                transpose_to(hb, bh, dstT, [dbuf], tt, pb)
            return f
        norm_tiles(None, 8, g_cross, tsink(hT3, bhT3), xin=x1_tiles)
        norm_tiles(mem, 2, g_mem, tsink(memT, B("memT")))
        S.barrier()
        AR.release(m_I1)
        wc = [AR.alloc([128, 16, 512], BF16) for _ in range(2)]
        S.dma("pool", wc[0], wsrc(w_ck, 0, D, 0, 512), writes=[B("wc", 0)])
        S.dma("pool", wc[1], wsrc(w_cv, 0, D, 0, 512), writes=[B("wc", 1)])
        S.dma("pool", wco, wsrc(w_co, 0, 512, 0, D), writes=[B("wco")])
        for hh in range(4):
            bk = next_bank(4, 4)
            for kc in range(16):
                S.op("pe", lambda e, bk=bk, kc=kc, hh=hh: e.matmul(out=ps[bk][:, 0:256], lhsT=wc[0][:, kc, hh * 128:(hh + 1) * 128], rhs=memT[:, kc, :],
                                                              start=(kc == 0), stop=(kc == 15)),
                     reads=[B("wc", 0), B("memT")], writes=[PB[bk]], signal=(kc == 15))
            S.op("act", lambda e, bk=bk, hh=hh: e.activation(out=KcT[:, hh, :], in_=ps[bk][:, 0:256], func=AF.Copy), reads=[PB[bk]], writes=[B("KcT")])
        S.op("pool", lambda e: e.memset(Vcx.rearrange("p a b c -> p (a b c)"), 1.0), writes=[B("Vcx")])
        for mt in range(2):
            bk = next_bank(4, 4)
            for kc in range(16):
                S.op("pe", lambda e, bk=bk, kc=kc, mt=mt: e.matmul(out=ps[bk][:, :], lhsT=memT[:, kc, mt * 128:(mt + 1) * 128], rhs=wc[1][:, kc, :],
                                                              start=(kc == 0), stop=(kc == 15)),
                     reads=[B("wc", 1), B("memT")], writes=[PB[bk]], signal=(kc == 15))
            S.op("dve", lambda e, bk=bk, mt=mt: e.tensor_copy(out=Vcx[:, mt, :, 0:128], in_=ps[bk][:, :].rearrange("p (h d) -> p h d", h=4)),
                 reads=[PB[bk]], writes=[B("Vcx")])
        S.dma("pool", wc[0], wsrc(w_cq, 0, D, 0, 512), writes=[B("wc", 0)])
        for hh in range(4):
            for tc in range(2):
                bk = next_bank(4, 4)
                for kc in range(16):
                    S.op("pe", lambda e, bk=bk, kc=kc, hh=hh, tc=tc: e.matmul(out=ps[bk][:, :], lhsT=wc[0][:, kc, hh * 128:(hh + 1) * 128], rhs=hT3[:, kc, tc * 512:(tc + 1) * 512],
                                                                         start=(kc == 0), stop=(kc == 15)),
                         reads=[B("wc", 0), bhT3], writes=[PB[bk]], signal=(kc == 15))
                S.op("act", lambda e, bk=bk, hh=hh, tc=tc: e.activation(out=QcT[:, hh, tc * 512:(tc + 1) * 512], in_=ps[bk][:, :], func=AF.Copy),
                     reads=[PB[bk]], writes=[B("QcT", hh)])
        ci = [0]
        for hh in range(4):
            for tc in range(2):
                sl = ci[0] % 2
                ci[0] += 1
                for mt in range(2):
                    bk = next_bank(4, 4)
                    S.op("pe", lambda e, bk=bk, hh=hh, tc=tc, mt=mt: e.matmul(out=ps[bk][:, :], lhsT=KcT[:, hh, mt * 128:(mt + 1) * 128], rhs=QcT[:, hh, tc * 512:(tc + 1) * 512],
                                                                         start=True, stop=True),
                         reads=[B("KcT"), B("QcT", hh)], writes=[PB[bk]])
                    S.op("act", lambda e, bk=bk, sl=sl, mt=mt: e.activation(out=PTc[sl][:, mt, :], in_=ps[bk][:, :], func=AF.Exp, scale=SCALE),
                         reads=[PB[bk]], writes=[B("PTc", sl)])
                for j in range(4):
                    ti = tc * 4 + j
                    bo = next_bank(0, 4)
                    for mt in range(2):
                        S.op("pe", lambda e, bo=bo, sl=sl, mt=mt, j=j, hh=hh: e.matmul(out=ps[bo][:, 0:129], lhsT=PTc[sl][:, mt, j * 128:(j + 1) * 128], rhs=Vcx[:, mt, hh, 0:129],
                                                                                  start=(mt == 0), stop=(mt == 1)),
                             reads=[B("PTc", sl), B("Vcx")], writes=[PB[bo]], signal=(mt == 1))
                    c0, bc = coef_slot_x = ((ci[0] * 8 + j) % 64) * 4, B("coefx", (ci[0] * 8 + j) % 64)
                    S.op("dve", lambda e, bo=bo, c0=c0: e.reciprocal(out=coefx[:, c0 + 1:c0 + 2], in_=ps[bo][:, 128:129]), reads=[PB[bo]], writes=[bc])
                    S.op("act", lambda e, bo=bo, c0=c0, ti=ti, hh=hh: e.activation(out=oc_bf[:, ti, hh * 128:(hh + 1) * 128], in_=ps[bo][:, 0:128], func=AF.Copy,
                                                                              scale=coefx[:, c0 + 1:c0 + 2]),
                         reads=[PB[bo], bc], writes=[B("oc_bf", ti)])
        for hh in range(4):
            for half in range(2):
                bk = next_bank(4, 4)
                pst = ps[bk].bitcast(BF16)
                for j in range(4):
                    ti = half * 4 + j
                    S.op("pe", lambda e, pst=pst, j=j, ti=ti, hh=hh: e.transpose(out=pst[:, j * 128:(j + 1) * 128], in_=oc_bf[:, ti, hh * 128:(hh + 1) * 128], identity=ident),
                         reads=[B("oc_bf", ti), bC], writes=[PB[bk]], signal=(j == 3))
                S.op("act", lambda e, pst=pst, hh=hh, half=half: e.activation(out=ocT[:, hh, half * 512:(half + 1) * 512], in_=pst[:, 0:512], func=AF.Copy),
                     reads=[PB[bk]], writes=[B("ocT")])
        for tt in range(8):
            for dc in range(4):
                bk = next_bank(0, 4)
                for kc in range(4):
                    S.op("pe", lambda e, bk=bk, kc=kc, tt=tt, dc=dc: e.matmul(out=ps[bk][:, :], lhsT=ocT[:, kc, tt * 128:(tt + 1) * 128], rhs=wco[:, kc, dc * 512:(dc + 1) * 512],
                                                                         start=(kc == 0), stop=(kc == 3)),
                         reads=[B("ocT"), B("wco")], writes=[PB[bk]], signal=(kc == 3))
                S.op("dve", lambda e, bk=bk, tt=tt, dc=dc: e.tensor_tensor(out=x1[:, tt, dc * 512:(dc + 1) * 512], in0=x1[:, tt, dc * 512:(dc + 1) * 512], in1=ps[bk][:, :], op=ALU.add),
                     reads=[PB[bk], B("x1", tt)], writes=[B("x1", tt)])
        tap("x2", x1, [B("x1", tt) for tt in range(8)])
        if stop_after <= 6:
            S.emit(); return nc
        S.barrier()
        AR.release(m_I)

        n_xt = None
        comb = AR.alloc([128, 8, 32], F32)
        mJ0 = AR.mark()
        ering = Ring("ering", 4, [128, 16, 512], top=True)

        def load_expert(ex):
            wgt, bwg = ering.load([(lambda t: t[:, 0:8, :], wsrc(w_eg[ex], 0, 1024, 0, 512)), (lambda t: t[:, 8:16, :], wsrc(w_eg[ex], 1024, 1024, 0, 512))])
            wut, bwu = ering.load([(lambda t: t[:, 0:8, :], wsrc(w_eu[ex], 0, 1024, 0, 512)), (lambda t: t[:, 8:16, :], wsrc(w_eu[ex], 1024, 1024, 0, 512))])
            wdt_, bwd = ering.load([(lambda t: t.rearrange("p a b -> p (a b)")[:, 0:4 * D].rearrange("p (a b) -> p a b", a=4), wsrc(w_ed[ex], 0, 512, 0, D))])
            return wgt, bwg, wut, bwu, wdt_, bwd
        pre_ex0 = load_expert(0)
        n_junk = [AR.alloc([128, D], BF16) for _ in range(1)] * 2
        n_hb = [AR.alloc([128, D], BF16) for _ in range(2)]
        hn32 = AR.alloc([128, D], F32)
        hnT32 = AR.alloc([128, 16, 128], F32)
        wr_sb = AR.alloc([128, 16, 36], F32)
        br_sb = AR.alloc([128, 36], F32)
        rt = AR.alloc([128, 8, 96], F32)
        S.dma("sp", wr_sb, w_r.rearrange("(k p) c -> p k c", p=128), writes=[B("wr")])
        S.dma("sp", br_sb, b_r[0:1, :].to_broadcast([128, 36]), writes=[B("wr")])

        def sinkJ(tt, hb, bh):
            pb = (cnt_t[0] % 2) * 2
            cnt_t[0] += 1
            transpose_to(hb, bh, hT3, [bhT3], tt, pb)
            c4 = norm_cur["c4"]
            S.op("dve", lambda e, tt=tt, c4=c4: e.scalar_tensor_tensor(out=hn32, in0=x1[:, tt, :], scalar=stats[:, c4 + 3:c4 + 4], in1=gb, op0=ALU.mult, op1=ALU.mult),
                 reads=[B("x1", tt), B("stats", norm_cur["col"]), B("gb")], writes=[B("hn32")])
            for q in range(4):
                bk = 4 + q % 2
                for k4 in range(4):
                    kc = q * 4 + k4
                    S.op("pe", lambda e, bk=bk, k4=k4, kc=kc: e.transpose(out=ps[bk][:, k4 * 128:(k4 + 1) * 128], in_=hn32[:, kc * 128:(kc + 1) * 128], identity=identf),
                         reads=[B("hn32"), bC], writes=[PB[bk]], signal=(k4 == 3))
                S.op("dve", lambda e, bk=bk, q=q: e.tensor_copy(out=hnT32[:, q * 4:(q + 1) * 4, :], in_=ps[bk][:, :].rearrange("p (k t) -> p k t", k=4)),
                     reads=[PB[bk]], writes=[B("hnT32")])
            bk = 6 + tt % 2
            for kc in range(16):
                S.op("pe", lambda e, bk=bk, kc=kc: e.matmul(out=ps[bk][:, 0:36], lhsT=hnT32[:, kc, :], rhs=wr_sb[:, kc, :], start=(kc == 0), stop=(kc == 15)),
                     reads=[B("hnT32"), B("wr")], writes=[PB[bk]], signal=(kc == 15))
            if router_pending:
                router_math(*router_pending.pop())
            router_pending.append((tt, bk))

        router_pending = []

        def router_math(tt, bk):
            R = rt[:, tt, :]
            br_ = B("rt", tt)
            S.op("dve", lambda e, bk=bk: e.tensor_tensor(out=R[:, 0:36], in0=ps[bk][:, 0:36], in1=br_sb, op=ALU.add), reads=[PB[bk], B("wr")], writes=[br_])
            S.op("dve", lambda e: e.tensor_reduce(out=R[:, 36:37], in_=R[:, 0:4], axis=mybir.AxisListType.X, op=ALU.max), reads=[br_], writes=[br_])
            S.op("dve", lambda e: e.tensor_scalar(out=R[:, 40:44], in0=R[:, 0:4], scalar1=R[:, 36:37], scalar2=None, op0=ALU.is_ge), reads=[br_], writes=[br_])
            S.op("dve", lambda e: e.tensor_scalar(out=R[:, 44:48], in0=R[:, 0:4], scalar1=R[:, 36:37], scalar2=None, op0=ALU.subtract), reads=[br_], writes=[br_])
            S.op("act", lambda e: e.activation(out=R[:, 44:48], in_=R[:, 44:48], func=AF.Exp, accum_out=R[:, 37:38]), reads=[br_], writes=[br_])
            S.op("dve", lambda e: e.reciprocal(out=R[:, 38:39], in_=R[:, 37:38]), reads=[br_], writes=[br_])
            S.op("dve", lambda e: e.tensor_scalar(out=R[:, 48:56], in0=R[:, 4:12], scalar1=R[:, 40:41], scalar2=None, op0=ALU.mult), reads=[br_], writes=[br_])
            for gq in range(1, 4):
                S.op("dve", lambda e, gq=gq: e.scalar_tensor_tensor(out=R[:, 48:56], in0=R[:, 4 + 8 * gq:12 + 8 * gq], scalar=R[:, 40 + gq:41 + gq], in1=R[:, 48:56],
                                                                   op0=ALU.mult, op1=ALU.add), reads=[br_], writes=[br_])
            S.op("dve", lambda e: e.max(out=R[:, 56:64], in_=R[:, 48:56]), reads=[br_], writes=[br_])
            S.op("dve", lambda e: e.tensor_tensor(out=R[:, 64:65], in0=R[:, 57:58], in1=R[:, 56:57], op=ALU.subtract), reads=[br_], writes=[br_])
            S.op("act", lambda e: e.activation(out=R[:, 65:66], in_=R[:, 64:65], func=AF.Exp), reads=[br_], writes=[br_])
            S.op("dve", lambda e: e.tensor_scalar(out=R[:, 65:66], in0=R[:, 65:66], scalar1=1.0, scalar2=None, op0=ALU.add), reads=[br_], writes=[br_])
            S.op("dve", lambda e: e.reciprocal(out=R[:, 66:67], in_=R[:, 65:66]), reads=[br_], writes=[br_])
            S.op("dve", lambda e: e.tensor_scalar(out=R[:, 67:68], in0=R[:, 66:67], scalar1=-1.0, scalar2=1.0, op0=ALU.mult, op1=ALU.add), reads=[br_], writes=[br_])
            S.op("dve", lambda e: e.tensor_scalar(out=R[:, 66:68], in0=R[:, 66:68], scalar1=R[:, 38:39], scalar2=None, op0=ALU.mult), reads=[br_], writes=[br_])
            S.op("dve", lambda e: e.tensor_scalar(out=R[:, 72:80], in0=R[:, 48:56], scalar1=R[:, 56:57], scalar2=R[:, 66:67], op0=ALU.is_equal, op1=ALU.mult),
                 reads=[br_], writes=[br_])
            S.op("dve", lambda e: e.tensor_scalar(out=R[:, 80:88], in0=R[:, 48:56], scalar1=R[:, 57:58], scalar2=R[:, 67:68], op0=ALU.is_equal, op1=ALU.mult),
                 reads=[br_], writes=[br_])
            S.op("dve", lambda e: e.tensor_tensor(out=R[:, 72:80], in0=R[:, 72:80], in1=R[:, 80:88], op=ALU.add), reads=[br_], writes=[br_])
            for gq in range(4):
                S.op("dve", lambda e, gq=gq: e.tensor_scalar(out=comb[:, tt, gq * 8:(gq + 1) * 8], in0=R[:, 72:80], scalar1=R[:, 40 + gq:41 + gq], scalar2=None, op0=ALU.mult),
                     reads=[br_], writes=[B("comb", tt)])
        norm_tiles(None, 8, g_moe, sinkJ, xin=x1_tiles)
        router_math(*router_pending.pop())
        tap("comb", comb, [B("comb", tt) for tt in range(8)])
        S.barrier(exclude=list(ering.b))
        AR.release(mJ0)
        mJ = AR.mark()
        hid = [AR.alloc([128, 4, NOWN], BF16) for _ in range(2)]
        sil = [AR.alloc([128, 512], F32) for _ in range(2)]
        si = [0]
        for ex in range(32):
            wgt, bwg, wut, bwu, wdt_, bwd = pre_ex0 if ex == 0 else load_expert(ex)
            wdt = wdt_.rearrange("p a b -> p (a b)")[:, 0:4 * D].rearrange("p (a b) -> p a b", a=4)
            hsl = ex % 2
            for fc in range(4):
                for tc in range(2):
                    ba = next_bank(0, 2)
                    bu = 2 + (pbi[0] % 2)
                    for kc in range(16):
                        S.op("pe", lambda e, ba=ba, kc=kc, fc=fc, tc=tc, wgt=wgt: e.matmul(out=ps[ba][:, :], lhsT=wgt[:, kc, fc * 128:(fc + 1) * 128], rhs=hT3[:, kc, tc * 512:(tc + 1) * 512],
                                                                                      start=(kc == 0), stop=(kc == 15)),
                             reads=[bwg, bhT3], writes=[PB[ba]], signal=(kc == 15))
                    for kc in range(16):
                        S.op("pe", lambda e, bu=bu, kc=kc, fc=fc, tc=tc, wut=wut: e.matmul(out=ps[bu][:, :], lhsT=wut[:, kc, fc * 128:(fc + 1) * 128], rhs=hT3[:, kc, tc * 512:(tc + 1) * 512],
                                                                                      start=(kc == 0), stop=(kc == 15)),
                             reads=[bwu, bhT3], writes=[PB[bu]], signal=(kc == 15))
                    ss_ = si[0] % 2
                    si[0] += 1
                    S.op("act", lambda e, ba=ba, ss_=ss_: e.activation(out=sil[ss_], in_=ps[ba][:, :], func=AF.Silu), reads=[PB[ba]], writes=[B("sil", ss_)])
                    S.op("dve", lambda e, bu=bu, ss_=ss_, hsl=hsl, fc=fc, tc=tc: e.tensor_tensor(out=hid[hsl][:, fc, tc * 512:(tc + 1) * 512], in0=sil[ss_], in1=ps[bu][:, :], op=ALU.mult),
                         reads=[B("sil", ss_), PB[bu]], writes=[B("hid", hsl)])
            for tt in range(8):
                for dc in range(4):
                    bk = 4 + (pbi[0] % 4)
                    pbi[0] += 1
                    for fc in range(4):
                        S.op("pe", lambda e, bk=bk, fc=fc, tt=tt, dc=dc, hsl=hsl, wdt=wdt: e.matmul(out=ps[bk][:, :], lhsT=hid[hsl][:, fc, tt * 128:(tt + 1) * 128],
                                                                                               rhs=wdt[:, fc, dc * 512:(dc + 1) * 512], start=(fc == 0), stop=(fc == 3)),
                             reads=[B("hid", hsl), bwd], writes=[PB[bk]], signal=(fc == 3))
                    S.op("dve", lambda e, bk=bk, tt=tt, dc=dc, ex=ex: e.scalar_tensor_tensor(out=x1[:, tt, dc * 512:(dc + 1) * 512], in0=ps[bk][:, :], scalar=comb[:, tt, ex:ex + 1],
                                                                                         in1=x1[:, tt, dc * 512:(dc + 1) * 512], op0=ALU.mult, op1=ALU.add),
                         reads=[PB[bk], B("comb", tt), B("x1", tt)], writes=[B("x1", tt)])
        tap("x3", x1, [B("x1", tt) for tt in range(8)])
        S.barrier()
        AR.release(mJ)
        yout = [AR.alloc([128, D], F32) for _ in range(2)]
        n_hb = yout
        n_junk = [AR.alloc([128, D], BF16)] * 2

        def sinkK(tt, hb, bh):
            S.tail.append(S.dma("sp", out_d[tt * 128:(tt + 1) * 128, :], hb, reads=[bh]))
        norm_tiles(None, 8, g_final, sinkK, xin=x1_tiles)
        S.emit()
    return nc


def _consts(h):
    c = {}
    c["c_ident"] = np.eye(128, dtype=np.float32)
    j = np.arange(128)
    c["c_negtri"] = -(j[:, None] > j[None, :]).astype(np.float32)
    c["c_negones"] = -np.ones((128, 128), np.float32)
    r = np.zeros((32, 32), np.float32)
    for m in range(16):
        r[m + 16, m] = -1.0
        r[m, m + 16] = 1.0
    c["c_r32"] = r
    inv = (500000.0 ** (-np.arange(16, dtype=np.float32) * (2.0 / 32))).astype(np.float32)
    invf = np.zeros((32, 2), np.float32)
    invf[:, 0] = np.concatenate([inv, inv]) / np.float32(2 * np.pi)
    c["c_invf"] = invf
    rel = [h, 3 - h]
    k = np.arange(128)[:, None]
    q = np.arange(256)[None, :]
    tq = np.array(rel)[q // 128] * 128 + q % 128
    cbi = np.zeros((128, 4, 256), np.float32); cbs = np.zeros((128, 4, 256), np.float32)
    for jj in range(4):
        s = jj * 128 + k
        cbi[:, jj, :] = np.where(s <= tq, 0.0, NEG)
        cbs[:, jj, :] = np.where(s < tq, 0.0, NEG)
    c["c_cbi"] = cbi.reshape(128, -1); c["c_cbs"] = cbs.reshape(128, -1)
    wb = np.zeros((128, 8, 256), np.float32)
    for jj in range(8):
        s = (jj - 4) * 128 + k
        dd = tq - s
        wb[:, jj, :] = np.where((dd >= 0) & (dd < 512), 0.0, NEG)
    c["c_wb"] = wb.reshape(128, -1)
    own_tiles = [2 * i + ((i % 2) ^ h) for i in range(8)]
    own_tok = np.concatenate([np.arange(g * 128, (g + 1) * 128) for g in own_tiles])
    cc = np.arange(128)[:, None]
    vis = (cc <= 126) & (cc * 16 + 31 <= own_tok[None, :])
    c["c_cmpb"] = np.where(vis, 0.0, NEG).astype(np.float32)
    jb = np.arange(32)[None, :]
    cur = (own_tok // 64)[:, None]
    forced = (jb == 0) | (jb == cur) | (jb == cur - 1)
    valid = jb <= cur
    fbv = np.where(valid, np.where(forced, 1000.0, 0.0), -1e9).astype(np.float32)
    c["c_fb"] = fbv.reshape(8, 128, 32).transpose(1, 0, 2).reshape(128, -1).copy()
    c["c_vm"] = valid.astype(np.float32).reshape(8, 128, 32).transpose(1, 0, 2).reshape(128, -1).copy()
    E = np.zeros((32, 16, 128), np.float32)
    for G in range(16):
        for kk in range(128):
            E[2 * G + kk // 64, G, kk] = 1.0
    c["c_E"] = E.reshape(32, -1)
    cs = np.arange(127) * 16
    ss = np.arange(32) * 64
    ov = ((cs[:, None] < ss[None, :] + 64) & (cs[:, None] + 32 > ss[None, :])).astype(np.float32)
    ovl = np.zeros((128, 33), np.float32)
    ovl[:127, :32] = ov
    ovl[:127, 32] = 1.0
    c["c_ovl"] = ovl
    return c, own_tok


_NC_CACHE = {}


def kernel(**inputs):
    x = np.asarray(inputs["x"], np.float32)
    f = lambda k: np.ascontiguousarray(np.asarray(inputs[k]))
    shared = {
        "g_mix": f("g_mix").reshape(1, D), "g_cross": f("g_cross").reshape(1, D), "g_mem": f("g_mem").reshape(1, D),
        "g_moe": f("g_moe").reshape(1, D), "g_final": f("g_final").reshape(1, D),
        "w_in": f("w_in").reshape(D, IN_W),
        "cmp_pe_k": f("cmp_pe_k").reshape(32, 128), "cmp_w1_k": f("cmp_w1_k").reshape(4096, 128), "cmp_w2_k": f("cmp_w2_k").reshape(128, 128),
        "cmp_pe_v": f("cmp_pe_v").reshape(32, 128), "cmp_w1_v": f("cmp_w1_v").reshape(4096, 128), "cmp_w2_v": f("cmp_w2_v").reshape(128, 128),
        "w_br_nsa": f("w_br_nsa").reshape(1024, D), "w_br_sb": f("w_br_sb").reshape(1024, D), "w_o": f("w_o").reshape(D, D),
        "w_cq": f("w_cq").reshape(D, 512), "w_ck": f("w_ck").reshape(D, 512), "w_cv": f("w_cv").reshape(D, 512), "w_co": f("w_co").reshape(512, D),
        "w_r": np.ascontiguousarray(np.concatenate([f("w_rg").reshape(D, 4), f("w_re").reshape(D, 32)], axis=1)),
        "b_r": np.ascontiguousarray(np.concatenate([f("b_rg").reshape(1, 4), f("b_re").reshape(1, 32)], axis=1)),
        "w_eg": f("w_eg").reshape(32, D, 512), "w_eu": f("w_eu").reshape(32, D, 512), "w_ed": f("w_ed").reshape(32, 512, D),
    }
    pos = np.asarray(inputs["positions"]).astype(np.int32)
    memv = np.asarray(inputs["mem"], np.float32)
    in_maps = []
    owns = []
    for c in range(8):
        b, h = c // 2, c % 2
        cst, own_tok = _consts(h)
        owns.append(own_tok)
        m = dict(shared)
        m.update(cst)
        m["x_nat"] = np.ascontiguousarray(x[b])
        m["x_own"] = np.ascontiguousarray(x[b][own_tok])
        m["pos_nat"] = np.ascontiguousarray(pos[b].reshape(1, T))
        m["pos_own"] = np.ascontiguousarray(pos[b][own_tok].reshape(1, NOWN))
        m["mem"] = np.ascontiguousarray(memv[b])
        in_maps.append(m)
    if "nc" not in _NC_CACHE:
        _NC_CACHE["nc"] = build()
    nc = _NC_CACHE["nc"]
    res = run_bass_kernel_spmd(nc, in_maps, core_ids=list(range(8)))
    out = np.zeros((4, T, D), np.float32)
    for c in range(8):
        out[c // 2][owns[c]] = res.results[c]["out"]
    return out
```

```python
import types
import numpy as np
from contextlib import ExitStack
import concourse.bass as bass
import concourse.mybir as mybir
from concourse.bass_utils import run_bass_kernel_spmd

F32 = mybir.dt.float32
BF16 = mybir.dt.bfloat16
I32 = mybir.dt.int32
U8 = mybir.dt.uint8
AF = mybir.ActivationFunctionType
ALU = mybir.AluOpType

D = 2048
T = 2048
NOWN = 1024
HD = 128
NEG = -30000.0
SCALE = HD ** -0.5
C_QN, C_KC, C_VC, C_KS, C_VS, C_KW, C_VW, C_G, C_QS, C_KSB, C_VSB, C_GA, C_GB = (
    0, 1024, 1280, 1536, 1792, 2048, 2304, 2560, 2584, 3608, 4632, 5656, 7704)
IN_W = 9752


def _freeze(fn):
    if fn.__closure__ is None:
        return fn
    cells = []
    for c in fn.__closure__:
        try:
            cells.append(types.CellType(c.cell_contents))
        except ValueError:
            cells.append(c)
    return types.FunctionType(fn.__code__, fn.__globals__, fn.__name__, fn.__defaults__, tuple(cells))


class Buf:
    __slots__ = ("name", "w", "r", "dsem", "dcnt", "excl")

    def __init__(self, name):
        self.name = name
        self.excl = False
        self.w = None
        self.r = []
        self.dsem = None
        self.dcnt = 0


class Sched:
    ENGS = ("pe", "act", "dve", "pool", "sp")

    def __init__(self, nc, stack):
        self.nc = nc
        self.stack = stack
        self.sem = {e: stack.enter_context(nc.semaphore("s_" + e)) for e in self.ENGS}
        self.cnt = {e: 0 for e in self.ENGS}
        self.q = {e: [] for e in self.ENGS}
        self.waited = {e: {} for e in self.ENGS}
        self.nsem = 5
        self.ninst = 0
        self.tail = []
        self.bufs = {}
        self.dma_bufs = []

    def B(self, *key):
        b = self.bufs.get(key)
        if b is None:
            b = Buf(str(key))
            self.bufs[key] = b
        return b

    def _wait(self, eng, ev):
        if ev is None:
            return
        sem, val = ev
        if sem is self.sem[eng]:
            if eng in ("pe", "sp") or val > self.cnt[eng]:
                return
        w = self.waited[eng]
        if w.get(id(sem), 0) >= val:
            return
        w[id(sem)] = val
        self.q[eng].append(("wait", sem, val))

    def _deps(self, eng, reads, writes):
        for b in reads:
            self._wait(eng, b.w)
            if b.excl:
                for ev in b.r:
                    if ev[0] is not self.sem[eng]:
                        self._wait(eng, ev)
        for b in writes:
            self._wait(eng, b.w)
            for ev in b.r:
                self._wait(eng, ev)

    def _commit(self, ev, reads, writes):
        for b in reads:
            b.r = [x for x in b.r if x[0] is not ev[0]] + [ev]
        for b in writes:
            b.w = ev
            b.r = []

    def op(self, eng, fn, reads=(), writes=(), signal=True):
        self._deps(eng, reads, writes)
        if signal:
            self.cnt[eng] += 1
            ev = (self.sem[eng], self.cnt[eng])
        else:
            ev = (self.sem[eng], self.cnt[eng] + 1)
        self.q[eng].append(("op", _freeze(fn), signal))
        self._commit(ev, reads, writes)
        self.ninst += 1
        return ev

    def _dsem(self, b):
        if b.dsem is None:
            b.dsem = self.stack.enter_context(self.nc.semaphore("d%d" % self.nsem))
            self.nsem += 1
            self.dma_bufs.append(b)
        return b.dsem

    def dma(self, eng, out, in_, reads=(), writes=(), owner=None):
        self._deps(eng, reads, writes)
        owner = owner or (writes[0] if writes else reads[0])
        sem = self._dsem(owner)
        owner.dcnt += 16
        ev = (sem, owner.dcnt)
        self.q[eng].append(("dma", out, in_, sem))
        self._commit(ev, reads, writes)
        self.ninst += 1
        return ev

    def barrier(self, exclude=()):
        self.marks = getattr(self, "marks", [])
        self.marks.append({e: sum(1 for it in self.q[e] if it[0] != "wait") for e in self.ENGS})
        evs = [(self.sem[e], self.cnt[e]) for e in self.ENGS if self.cnt[e] > 0]
        evs += [(b.dsem, b.dcnt) for b in self.dma_bufs if b.dcnt > 0 and b not in exclude]
        for e in self.ENGS:
            for ev in evs:
                self._wait(e, ev)

    def check(self):
        val = {}
        pc = {e: 0 for e in self.ENGS}
        prog = True
        while prog:
            prog = False
            for e in self.ENGS:
                q = self.q[e]
                while pc[e] < len(q):
                    it = q[pc[e]]
                    if it[0] == "wait":
                        if val.get(id(it[1]), 0) < it[2]:
                            break
                    elif it[0] == "op":
                        if it[2]:
                            val[id(self.sem[e])] = val.get(id(self.sem[e]), 0) + 1
                    else:
                        val[id(it[3])] = val.get(id(it[3]), 0) + 16
                    pc[e] += 1
                    prog = True
        stuck = {e: (pc[e], len(self.q[e])) for e in self.ENGS if pc[e] < len(self.q[e])}
        if stuck:
            for e, (p, n) in stuck.items():
                it = self.q[e][p]
                print("DEADLOCK", e, p, n, it[0], it[2], "have", val.get(id(it[1]), 0))
            raise RuntimeError("deadlock in schedule: %s" % stuck)
        print("schedule ok: insts", {e: len(self.q[e]) for e in self.ENGS}, "nsem", self.nsem)

    def emit(self):
        nc = self.nc
        for ev in self.tail:
            self._wait("sp", ev)
        self.check()
        with nc.Block() as block:
            def run(eng):
                def body(e):
                    sem_e = self.sem[eng]
                    for item in self.q[eng]:
                        k = item[0]
                        if k == "wait":
                            e.wait_ge(item[1], item[2])
                        elif k == "op":
                            ins = item[1](e)
                            if item[2]:
                                ins.then_inc(sem_e, 1)
                        else:
                            _, out, in_, sem = item
                            e.dma_start(out=out, in_=in_).then_inc(sem, 16)
                return body
            block.tensor(run("pe"))
            block.scalar(run("act"))
            block.vector(run("dve"))
            block.gpsimd(run("pool"))
            block.sync(run("sp"))


class Arena:
    def __init__(self, nc, nbytes):
        self.t = nc.alloc_sbuf_tensor("arena", [128, nbytes], U8)
        self.n = nbytes
        self.off = 0
        self.top = nbytes

    def alloc(self, shape, dt, top=False):
        esz = {F32: 4, BF16: 2, I32: 4}[dt]
        used = 1
        for s in shape[1:]:
            used *= s
        n = (used * esz + 63) // 64 * 64
        assert self.off + n <= self.top, ("SBUF arena overflow", self.off, n, self.top)
        if top:
            self.top -= n
            o = self.top
        else:
            o = self.off
            self.off += n
        v = self.t[0:shape[0], o:o + n].bitcast(dt)[:, 0:used]
        if len(shape) == 3:
            v = v.rearrange("p (a b) -> p a b", a=shape[1])
        elif len(shape) == 4:
            v = v.rearrange("p (a b c) -> p a b c", a=shape[1], b=shape[2])
        return v

    def mark(self):
        return self.off

    def release(self, m):
        self.off = m

    def release_top(self):
        self.top = self.n


def build(stop_after=99, taps=()):
    nc = bass.Bass("TRN2", target_bir_lowering=False)
    din = {}

    def dram_in(name, shape, dt=F32):
        din[name] = nc.dram_tensor(name, list(shape), dt, kind="ExternalInput").ap()
        return din[name]

    x_nat = dram_in("x_nat", [T, D])
    x_own = dram_in("x_own", [NOWN, D])
    pos_nat = dram_in("pos_nat", [1, T], I32)
    pos_own = dram_in("pos_own", [1, NOWN], I32)
    mem = dram_in("mem", [256, D])
    g_mix = dram_in("g_mix", [1, D]); g_cross = dram_in("g_cross", [1, D]); g_mem = dram_in("g_mem", [1, D])
    g_moe = dram_in("g_moe", [1, D]); g_final = dram_in("g_final", [1, D])
    w_in = dram_in("w_in", [D, IN_W])
    cmp_pe_k = dram_in("cmp_pe_k", [32, 128]); cmp_w1_k = dram_in("cmp_w1_k", [4096, 128]); cmp_w2_k = dram_in("cmp_w2_k", [128, 128])
    cmp_pe_v = dram_in("cmp_pe_v", [32, 128]); cmp_w1_v = dram_in("cmp_w1_v", [4096, 128]); cmp_w2_v = dram_in("cmp_w2_v", [128, 128])
    w_br_nsa = dram_in("w_br_nsa", [1024, D]); w_br_sb = dram_in("w_br_sb", [1024, D]); w_o = dram_in("w_o", [D, D])
    w_cq = dram_in("w_cq", [D, 512]); w_ck = dram_in("w_ck", [D, 512]); w_cv = dram_in("w_cv", [D, 512]); w_co = dram_in("w_co", [512, D])
    w_r = dram_in("w_r", [D, 36])
    b_r = dram_in("b_r", [1, 36])
    w_eg = dram_in("w_eg", [32, D, 512]); w_eu = dram_in("w_eu", [32, D, 512]); w_ed = dram_in("w_ed", [32, 512, D])
    c_ident = dram_in("c_ident", [128, 128]); c_negtri = dram_in("c_negtri", [128, 128]); c_negones = dram_in("c_negones", [128, 128])
    c_r32 = dram_in("c_r32", [32, 32]); c_invf = dram_in("c_invf", [32, 2])
    c_cbi = dram_in("c_cbi", [128, 4 * 256]); c_cbs = dram_in("c_cbs", [128, 4 * 256]); c_wb = dram_in("c_wb", [128, 8 * 256])
    c_cmpb = dram_in("c_cmpb", [128, NOWN]); c_fb = dram_in("c_fb", [128, 8 * 32]); c_vm = dram_in("c_vm", [128, 8 * 32])
    c_E = dram_in("c_E", [32, 16 * 128]); c_ovl = dram_in("c_ovl", [128, 33])
    out_d = nc.dram_tensor("out", [NOWN, D], F32, kind="ExternalOutput").ap()
    tap_d = {}
    for nm, shp, dt in taps:
        tap_d[nm] = nc.dram_tensor("tap_" + nm, list(shp), dt, kind="ExternalOutput").ap()
    scr_hTown = nc.dram_tensor("scr_hTown", [128, 16, NOWN], BF16, kind="Internal").ap()
    scr_qsb = nc.dram_tensor("scr_qsb", [8, 128, NOWN], BF16, kind="Internal").ap()
    scr_ksb = nc.dram_tensor("scr_ksb", [8, 128, T], BF16, kind="Internal").ap()
    scr_vsb = nc.dram_tensor("scr_vsb", [128, 16, 1024], BF16, kind="Internal").ap()

    with ExitStack() as st:
        S = Sched(nc, st)
        B = S.B
        AR = Arena(nc, 206 * 1024)
        ps = [nc.alloc_psum_tensor("ps%d" % i, [128, 512], F32) for i in range(8)]
        PB = [B("ps", i) for i in range(8)]
        for pb_ in PB:
            pb_.excl = True

        def tap(name, src_ap, src_bufs):
            if name in tap_d:
                S.tail.append(S.dma("sp", tap_d[name], src_ap, reads=src_bufs))

        identf = AR.alloc([128, 128], F32)
        ident = AR.alloc([128, 128], BF16)
        r32 = AR.alloc([32, 32], F32)
        invf = AR.alloc([32, 2], F32)
        epsc = AR.alloc([128, 2], F32)
        stats = AR.alloc([128, 256], F32)
        stage_c = AR.alloc([128, 2048], F32)
        m_ess = AR.mark()
        negtri = AR.alloc([128, 128], BF16)
        negones = AR.alloc([128, 128], BF16)
        bC = B("const")

        def load_const_bf16(dst, src, ncols, buf=None):
            S.dma("pool", dst, src, writes=[buf or bC])

        S.dma("sp", identf, c_ident[:, :], writes=[bC])
        S.dma("sp", r32, c_r32[:, :], writes=[bC])
        S.dma("sp", invf, c_invf[:, :], writes=[bC])
        load_const_bf16(ident, c_ident[:, :], 128)
        cbi = AR.alloc([128, 4, 256], BF16); cbs = AR.alloc([128, 4, 256], BF16); wbm = AR.alloc([128, 8, 256], BF16)
        cmpb = AR.alloc([128, NOWN], BF16)
        Emat = AR.alloc([32, 16, 128], BF16)
        ovl = AR.alloc([128, 33], BF16)
        fb = AR.alloc([128, 8, 32], F32); vm = AR.alloc([128, 8, 32], F32)

        def load_masks():
            bM = B("masks")
            load_const_bf16(negtri, c_negtri[:, :], 128, bM)
            load_const_bf16(negones, c_negones[:, :], 128, bM)
            load_const_bf16(cbi.rearrange("p a b -> p (a b)"), c_cbi[:, :], 1024, bM)
            load_const_bf16(cbs.rearrange("p a b -> p (a b)"), c_cbs[:, :], 1024, bM)
            load_const_bf16(wbm.rearrange("p a b -> p (a b)"), c_wb[:, :], 2048, bM)
            load_const_bf16(cmpb, c_cmpb[:, :], 1024, bM)
            load_const_bf16(Emat.rearrange("p a b -> p (a b)"), c_E[:, :], 2048, bM)
            load_const_bf16(ovl, c_ovl[:, :], 33, bM)
            S.dma("pool", fb.rearrange("p a b -> p (a b)"), c_fb[:, :], writes=[bM])
            S.dma("pool", vm.rearrange("p a b -> p (a b)"), c_vm[:, :], writes=[bM])
        S.op("pool", lambda e: e.memset(stats, 0.0), writes=[B("stats")])
        S.op("pool", lambda e: e.memset(epsc, 1e-6), writes=[bC])
        load_masks()
        gb = stage_c
        KT_slc = AR.alloc([128, 2, T], BF16)
        KT_win = AR.alloc([128, 2, T], BF16)
        Vx_slc = AR.alloc([128, 16, 2, 130], BF16)
        Vx_win = AR.alloc([128, 16, 2, 130], BF16)
        kcT = AR.alloc([128, 2, 128], BF16)
        vcx = AR.alloc([128, 2, 162], BF16)
        gates_sb = AR.alloc([128, 8, 24], F32)
        m_nsa = AR.mark()

        stat_col = [0]

        norm_cur = {}

        def norm_tiles(x_dram, ntiles, g_dram, sink, xin=None, tag="n"):
            mid_fn, end_fn = sink if isinstance(sink, tuple) else (sink, None)
            S.dma("sp", gb, g_dram[0:1, :].to_broadcast([128, D]), writes=[B("gb"), B("stage_c")])
            info = {}

            def s1(tt):
                sl = tt % 2
                if xin is None:
                    xs = tt % len(n_xt)
                    xt = n_xt[xs]; bx = B("n_xt", xs)
                    S.dma("sp", xt, x_dram[tt * 128:(tt + 1) * 128, :], writes=[bx])
                else:
                    xt, bx = xin(tt)
                col = stat_col[0] % 64
                stat_col[0] += 1
                c4 = col * 4
                bs = B("stats", col)
                info[tt] = (xt, bx, c4, bs, col)
                S.op("act", lambda e: e.activation(out=n_junk[sl], in_=xt, func=AF.Square, scale=float(D) ** -0.5, accum_out=stats[:, c4:c4 + 1]),
                     reads=[bx, B("stats")], writes=[B("n_junk", sl), bs])
                S.op("act", lambda e: e.activation(out=stats[:, c4 + 2:c4 + 3], in_=stats[:, c4:c4 + 1], func=AF.Sqrt, bias=epsc[:, 0:1]),
                     reads=[bs, bC], writes=[bs])

            def s2(tt):
                xt, bx, c4, bs, col = info[tt]
                sl = tt % 2
                S.op("dve", lambda e: e.reciprocal(out=stats[:, c4 + 3:c4 + 4], in_=stats[:, c4 + 2:c4 + 3]), reads=[bs], writes=[bs])
                hb = n_hb[sl]; bh = B("n_hb", sl)
                S.op("dve", lambda e: e.scalar_tensor_tensor(out=hb, in0=xt, scalar=stats[:, c4 + 3:c4 + 4], in1=gb, op0=ALU.mult, op1=ALU.mult),
                     reads=[bx, bs, B("gb")], writes=[bh])
                S.op("pool", lambda e: e.memset(stats[:, c4:c4 + 1], 0.0), reads=[bs], writes=[bs])
                norm_cur["c4"] = c4; norm_cur["col"] = col
                mid_fn(tt, hb, bh)

            s1(0)
            if ntiles > 1:
                s1(1)
            s2(0)
            for tt in range(ntiles):
                if tt + 2 < ntiles:
                    s1(tt + 2)
                if tt + 1 < ntiles:
                    s2(tt + 1)
                if end_fn is not None:
                    end_fn(tt)

        def transpose_pe(hb, bh, pbank):
            for half in range(2):
                pst = ps[pbank + half].bitcast(BF16)
                bp = PB[pbank + half]
                for k8 in range(8):
                    kc = half * 8 + k8
                    S.op("pe", lambda e: e.transpose(out=pst[:, k8 * 128:(k8 + 1) * 128], in_=hb[:, kc * 128:(kc + 1) * 128], identity=ident),
                         reads=[bh, bC], writes=[bp], signal=(k8 == 7))

        def transpose_evac(dstT, dst_bufs, tt, pbank):
            for half in range(2):
                pst = ps[pbank + half].bitcast(BF16)
                bp = PB[pbank + half]
                dst = dstT[:, half * 8:(half + 1) * 8, tt * 128:(tt + 1) * 128]
                src = pst[:, 0:1024].rearrange("p (k t) -> p k t", k=8)
                if half == 0:
                    S.op("act", lambda e: e.activation(out=dst, in_=src, func=AF.Copy), reads=[bp], writes=dst_bufs)
                else:
                    S.op("dve", lambda e: e.tensor_copy(out=dst, in_=src), reads=[bp], writes=dst_bufs)

        def transpose_to(hb, bh, dstT, dst_bufs, tt, pbank):
            transpose_pe(hb, bh, pbank)
            transpose_evac(dstT, dst_bufs, tt, pbank)

        def tsink(dstT, dbuf):
            return (lambda tt, hb, bh: transpose_pe(hb, bh, (tt % 2) * 2),
                    lambda tt: transpose_evac(dstT, [dbuf], tt, (tt % 2) * 2))

        class Ring:
            def __init__(self, name, nslots, shape, top=False):
                self.t = [AR.alloc(shape, BF16, top=top) for _ in range(nslots)]
                self.b = [B(name, i) for i in range(nslots)]
                self.i = 0
                self.n = nslots

            def load(self, src_aps):
                t = self.t[self.i]; b = self.b[self.i]
                self.i = (self.i + 1) % self.n
                for dv, sa in src_aps:
                    S.dma("pool", dv(t), sa, writes=[b])
                return t, b

        def wsrc(w, r0, nrows, c0, ncols):
            return w[r0:r0 + nrows, c0:c0 + ncols].rearrange("(k p) c -> p k c", p=128)

        hT_nat = AR.alloc([128, 16, T], BF16)
        m_A = AR.mark()
        hT_own = AR.alloc([128, 16, NOWN], BF16)
        m_A2 = AR.mark()
        n_xt = [AR.alloc([128, D], F32) for _ in range(3)]
        n_junk = [AR.alloc([128, D], BF16) for _ in range(2)]
        n_hb = [AR.alloc([128, D], BF16) for _ in range(2)]
        bhTn = B("hT_nat"); bhTo = B("hT_own")

        norm_tiles(x_own, 8, g_mix, tsink(hT_own, bhTo))
        norm_tiles(x_nat, 16, g_mix, tsink(hT_nat, bhTn))
        S.dma("sp", scr_hTown[:, :, :], hT_own, reads=[bhTo], writes=[B("scr_hTown")], owner=B("scr_hTown"))
        tap("hT_own", hT_own, [bhTo])
        if stop_after <= 0:
            S.emit(); return nc
        S.barrier()
        AR.release(m_A2)

        scr_qn = nc.dram_tensor("scr_qn", [8, 128, NOWN], BF16, kind="Internal").ap()
        RC = 256
        state = {}

        def alloc_proj_tmps():
            state["posi"] = AR.alloc([32, RC], I32); state["posf"] = AR.alloc([32, RC], F32)
            state["tmpa"] = AR.alloc([32, RC], F32); state["tmpi"] = AR.alloc([32, RC], I32); state["tmpm"] = AR.alloc([32, RC], F32)
            state["wring"] = Ring("wring", 2, [128, 16, 256])
            state["raw32"] = [AR.alloc([32, 512], F32) for _ in range(2)]
            state["ropt1"] = [AR.alloc([32, 512], F32) for _ in range(2)]
            state["ropt2"] = [AR.alloc([32, 512], F32) for _ in range(2)]
            state["stg"] = [AR.alloc([128, 512], BF16) for _ in range(3)]

        def rope_tables(pos_dram, n, cs):
            bp = B("ropetmp")
            posi, posf, tmpa, tmpi, tmpm = (state[k] for k in ("posi", "posf", "tmpa", "tmpi", "tmpm"))
            for c0 in range(0, n, RC):
                S.dma("sp", posi, pos_dram[0:1, c0:c0 + RC].to_broadcast([32, RC]), writes=[bp])
                S.op("dve", lambda e: e.tensor_copy(out=posf, in_=posi), reads=[bp], writes=[bp])
                for which in range(2):
                    S.op("dve", lambda e, which=which: e.tensor_scalar(out=tmpa, in0=posf, scalar1=invf[:, 0:1],
                                                                      scalar2=(0.25 if which == 0 else 0.0), op0=ALU.mult, op1=ALU.add),
                         reads=[bp, bC], writes=[bp])
                    S.op("dve", lambda e: e.tensor_copy(out=tmpi, in_=tmpa), reads=[bp], writes=[bp])
                    S.op("dve", lambda e: e.tensor_copy(out=tmpm, in_=tmpi), reads=[bp], writes=[bp])
                    S.op("dve", lambda e: e.tensor_sub(out=tmpa, in0=tmpa, in1=tmpm), reads=[bp], writes=[bp])
                    S.op("dve", lambda e: e.tensor_scalar(out=tmpm, in0=tmpa, scalar1=0.5, scalar2=None, op0=ALU.is_gt), reads=[bp], writes=[bp])
                    S.op("dve", lambda e: e.tensor_sub(out=tmpa, in0=tmpa, in1=tmpm), reads=[bp], writes=[bp])
                    S.op("dve", lambda e: e.tensor_scalar(out=tmpm, in0=tmpa, scalar1=-0.5, scalar2=None, op0=ALU.is_lt), reads=[bp], writes=[bp])
                    S.op("dve", lambda e: e.tensor_add(out=tmpa, in0=tmpa, in1=tmpm), reads=[bp], writes=[bp])
                    S.op("act", lambda e, which=which, c0=c0: e.activation(out=cs[:, which, c0:c0 + RC], in_=tmpa, func=AF.Sin, scale=2.0 * np.pi),
                         reads=[bp], writes=[B("cs")])

        pbi = [0]

        def next_bank(lo, n):
            i = lo + (pbi[0] % n)
            pbi[0] += 1
            return i

        rope_i = [0]
        stg_i = [0]

        def evac_rope(pp, bp, dst, dbufs, cs, t0):
            raw32, ropt1, ropt2 = state["raw32"], state["ropt1"], state["ropt2"]
            sl = rope_i[0] % 2
            rope_i[0] += 1
            S.op("act", lambda e: e.activation(out=dst, in_=pp, func=AF.Copy), reads=[bp], writes=dbufs)
            S.op("dve", lambda e: e.tensor_copy(out=raw32[sl], in_=pp[0:32, :]), reads=[bp], writes=[B("raw32", sl)])
            rb = 6 + sl
            S.op("pe", lambda e: e.matmul(out=ps[rb][0:32, :], lhsT=r32, rhs=raw32[sl], start=True, stop=True),
                 reads=[B("raw32", sl), bC], writes=[PB[rb]])
            S.op("dve", lambda e: e.tensor_tensor(out=ropt1[sl], in0=raw32[sl], in1=cs[:, 0, t0:t0 + 512], op=ALU.mult),
                 reads=[B("raw32", sl), B("cs")], writes=[B("ropt1", sl)])
            S.op("dve", lambda e: e.tensor_tensor(out=ropt2[sl], in0=ps[rb][0:32, :], in1=cs[:, 1, t0:t0 + 512], op=ALU.mult),
                 reads=[PB[rb], B("cs")], writes=[B("ropt2", sl)])
            S.op("dve", lambda e: e.tensor_tensor(out=dst[0:32, :], in0=ropt1[sl], in1=ropt2[sl], op=ALU.add),
                 reads=[B("ropt1", sl), B("ropt2", sl)], writes=dbufs)

        def proj_fm(hT, bhT, ntok, col0, ncols, evac):
            wring = state["wring"]
            pending = None
            for g0 in range(0, ncols, 256):
                gw = min(256, ncols - g0)
                wt, wbuf = wring.load([(lambda t, gw=gw: t[:, :, 0:gw], wsrc(w_in, 0, D, col0 + g0, gw))])
                for cc in range(gw // 128):
                    for tc in range(ntok // 512):
                        bk = next_bank(0, 4)
                        for kc in range(16):
                            S.op("pe", lambda e, bk=bk, wt=wt, cc=cc, kc=kc, tc=tc: e.matmul(
                                out=ps[bk][:, :], lhsT=wt[:, kc, cc * 128:(cc + 1) * 128], rhs=hT[:, kc, tc * 512:(tc + 1) * 512],
                                start=(kc == 0), stop=(kc == 15)),
                                reads=[wbuf, bhT], writes=[PB[bk]], signal=(kc == 15))
                        if pending is not None:
                            evac(*pending)
                        pending = (g0 // 128 + cc, tc, ps[bk][:, :], PB[bk])
            if pending is not None:
                evac(*pending)

        def proj_tm(hT, bhT, ntiles, col0, ncols, evac):
            wring = state["wring"]
            for g0 in range(0, ncols, 256):
                gw = min(256, ncols - g0)
                wt, wbuf = wring.load([(lambda t, gw=gw: t[:, :, 0:gw], wsrc(w_in, 0, D, col0 + g0, gw))])
                for tt in range(ntiles):
                    bk = next_bank(0, 4)
                    for kc in range(16):
                        S.op("pe", lambda e, bk=bk, wt=wt, kc=kc, tt=tt, gw=gw: e.matmul(
                            out=ps[bk][:, 0:gw], lhsT=hT[:, kc, tt * 128:(tt + 1) * 128], rhs=wt[:, kc, 0:gw],
                            start=(kc == 0), stop=(kc == 15)),
                            reads=[wbuf, bhT], writes=[PB[bk]], signal=(kc == 15))
                    evac(tt, g0, gw, ps[bk][:, :], PB[bk])

        def stage_slot():
            sl = stg_i[0] % 3
            stg_i[0] += 1
            return sl

        def spill_evac(dram_fn):
            def f(c, tc, pp, bp):
                stg = state["stg"]
                sl = stage_slot()
                if sl % 2:
                    S.op("act", lambda e: e.activation(out=stg[sl], in_=pp, func=AF.Copy), reads=[bp], writes=[B("stg", sl)])
                else:
                    S.op("dve", lambda e: e.tensor_copy(out=stg[sl], in_=pp), reads=[bp], writes=[B("stg", sl)])
                dst, dbuf = dram_fn(c, tc)
                S.dma("sp", dst, stg[sl], reads=[B("stg", sl)], writes=[dbuf], owner=B("stg", sl))
            return f

        def rope_spill_evac(dram_fn, cs):
            def f(c, tc, pp, bp):
                stg = state["stg"]
                sl = stage_slot()
                evac_rope(pp, bp, stg[sl], [B("stg", sl)], cs, tc * 512)
                dst, dbuf = dram_fn(c, tc)
                S.dma("sp", dst, stg[sl], reads=[B("stg", sl)], writes=[dbuf], owner=B("stg", sl))
            return f

        cs_own = AR.alloc([32, 2, NOWN], F32)
        alloc_proj_tmps()
        rope_tables(pos_own, NOWN, cs_own)
        proj_fm(hT_own, bhTo, NOWN, C_QN, 1024,
                rope_spill_evac(lambda c, tc: (scr_qn[c, :, tc * 512:(tc + 1) * 512], B("scr_qn")), cs_own))
        proj_fm(hT_own, bhTo, NOWN, C_QS, 1024,
                spill_evac(lambda c, tc: (scr_qsb[c, :, tc * 512:(tc + 1) * 512], B("scr_qsb"))))
        proj_tm(hT_own, bhTo, 8, C_G, 24,
                lambda tt, g0, gw, pp, bp: S.op("act", lambda e: e.activation(out=gates_sb[:, tt, :], in_=pp[:, 0:24], func=AF.Sigmoid),
                                               reads=[bp], writes=[B("gates")]))
        tap("gates", gates_sb, [B("gates")])
        if stop_after <= 1:
            S.emit(); return nc
        S.barrier()
        AR.release(m_A)

        cs_nat = AR.alloc([32, 2, T], F32)
        alloc_proj_tmps()
        rope_tables(pos_nat, T, cs_nat)
        proj_fm(hT_nat, bhTn, T, C_KSB, 1024,
                spill_evac(lambda c, tc: (scr_ksb[c, :, tc * 512:(tc + 1) * 512], B("scr_ksb"))))

        def vsb_evac(tt, g0, gw, pp, bp):
            stg = state["stg"]
            sl = stage_slot()
            S.op("dve", lambda e: e.tensor_copy(out=stg[sl][:, 0:gw], in_=pp[:, 0:gw]), reads=[bp], writes=[B("stg", sl)])
            S.dma("sp", scr_vsb[:, tt, g0:g0 + gw], stg[sl][:, 0:gw], reads=[B("stg", sl)], writes=[B("scr_vsb")], owner=B("stg", sl))
        proj_tm(hT_nat, bhTn, 16, C_VSB, 1024, vsb_evac)
        if stop_after <= 1.5:
            S.emit(); return nc

        tokT = AR.alloc([128, 2, T], BF16)
        w1c = AR.alloc([128, 32, 128], BF16)
        w2c = AR.alloc([128, 128], BF16)
        peT = AR.alloc([128, 32], BF16)
        pe_tm = AR.alloc([32, 128], BF16)
        cbias = AR.alloc([128, 2], F32)
        hidT = AR.alloc([128, 128], BF16)

        S.op("pool", lambda e: e.memset(Vx_slc.rearrange("p a b c -> p (a b c)"), 1.0), writes=[B("Vx_slc")])
        S.op("pool", lambda e: e.memset(Vx_win.rearrange("p a b c -> p (a b c)"), 1.0), writes=[B("Vx_win")])
        S.op("pool", lambda e: e.memset(kcT.rearrange("p a b -> p (a b)"), 0.0), writes=[B("kcT")])
        S.op("pool", lambda e: e.memset(vcx.rearrange("p a b -> p (a b)"), 0.0), writes=[B("vcx")])
        proj_fm(hT_nat, bhTn, T, C_KS, 256,
                lambda c, tc, pp, bp: evac_rope(pp, bp, KT_slc[:, c, tc * 512:(tc + 1) * 512], [B("KT_slc")], cs_nat, tc * 512))
        proj_fm(hT_nat, bhTn, T, C_KW, 256,
                lambda c, tc, pp, bp: evac_rope(pp, bp, KT_win[:, c, tc * 512:(tc + 1) * 512], [B("KT_win")], cs_nat, tc * 512))

        def v_evac(Vx, vb):
            def f(tt, g0, gw, pp, bp):
                S.op("dve", lambda e: e.tensor_copy(out=Vx[:, tt, :, 0:128], in_=pp[:, 0:256].rearrange("p (h d) -> p h d", h=2)),
                     reads=[bp], writes=[vb])
            return f
        proj_tm(hT_nat, bhTn, 16, C_VS, 256, v_evac(Vx_slc, B("Vx_slc")))
        proj_tm(hT_nat, bhTn, 16, C_VW, 256, v_evac(Vx_win, B("Vx_win")))

        bcw = B("cmpw")
        btok = B("tokT")
        for kv in range(2):
            if kv == 0:
                proj_fm(hT_nat, bhTn, T, C_KC, 256,
                        lambda c, tc, pp, bp: evac_rope(pp, bp, tokT[:, c, tc * 512:(tc + 1) * 512], [btok], cs_nat, tc * 512))
            else:
                proj_fm(hT_nat, bhTn, T, C_VC, 256,
                        lambda c, tc, pp, bp: S.op("act", lambda e: e.activation(out=tokT[:, c, tc * 512:(tc + 1) * 512], in_=pp, func=AF.Copy),
                                                   reads=[bp], writes=[btok]))
            w1d, w2d, ped = ((cmp_w1_k, cmp_w2_k, cmp_pe_k), (cmp_w1_v, cmp_w2_v, cmp_pe_v))[kv]
            S.dma("pool", w1c, w1d.rearrange("(l p) f -> p l f", p=128), writes=[bcw])
            S.dma("pool", w2c, w2d[:, :], writes=[bcw])
            S.dma("pool", pe_tm, ped[:, :], writes=[bcw])
            pst = ps[4].bitcast(BF16)
            S.op("pe", lambda e, pst=pst: e.transpose(out=pst[:, 0:32], in_=pe_tm, identity=ident[0:32, 0:32]),
                 reads=[bcw, bC], writes=[PB[4]])
            S.op("dve", lambda e, pst=pst: e.tensor_copy(out=peT, in_=pst[:, 0:32]), reads=[PB[4]], writes=[B("peT")])
            for l in range(32):
                S.op("pe", lambda e, l=l: e.matmul(out=ps[5][:, 0:1], lhsT=w1c[:, l, :], rhs=peT[:, l:l + 1], start=(l == 0), stop=(l == 31)),
                     reads=[bcw, B("peT")], writes=[PB[5]], signal=(l == 31))
            S.op("dve", lambda e, kv=kv: e.tensor_copy(out=cbias[:, kv:kv + 1], in_=ps[5][:, 0:1]), reads=[PB[5]], writes=[B("cbias")])
            for hh in range(2):
                bk = next_bank(0, 4)
                for l in range(32):
                    S.op("pe", lambda e, bk=bk, l=l, hh=hh: e.matmul(
                        out=ps[bk][:, 0:127], lhsT=w1c[:, l, :], rhs=tokT[:, hh, l:l + 16 * 126 + 1:16], start=(l == 0), stop=(l == 31)),
                        reads=[bcw, btok], writes=[PB[bk]], signal=(l == 31))
                S.op("act", lambda e, bk=bk, kv=kv: e.activation(out=hidT[:, 0:127], in_=ps[bk][:, 0:127], func=AF.Silu, bias=cbias[:, kv:kv + 1]),
                     reads=[PB[bk], B("cbias")], writes=[B("hidT")])
                bk2 = next_bank(0, 4)
                if kv == 0:
                    S.op("pe", lambda e, bk2=bk2: e.matmul(out=ps[bk2][:, 0:127], lhsT=w2c, rhs=hidT[:, 0:127], start=True, stop=True),
                         reads=[bcw, B("hidT")], writes=[PB[bk2]])
                    S.op("dve", lambda e, bk2=bk2, hh=hh: e.tensor_copy(out=kcT[:, hh, 0:127], in_=ps[bk2][:, 0:127]), reads=[PB[bk2]], writes=[B("kcT")])
                else:
                    S.op("pe", lambda e, bk2=bk2: e.matmul(out=ps[bk2][0:127, 0:128], lhsT=hidT[:, 0:127], rhs=w2c, start=True, stop=True),
                         reads=[bcw, B("hidT")], writes=[PB[bk2]])
                    S.op("dve", lambda e, bk2=bk2, hh=hh: e.tensor_copy(out=vcx[0:127, hh, 0:128], in_=ps[bk2][0:127, 0:128]),
                         reads=[PB[bk2]], writes=[B("vcx")])
        for hh in range(2):
            S.op("dve", lambda e, hh=hh: e.tensor_copy(out=vcx[:, hh, 128:161], in_=ovl), reads=[bC], writes=[B("vcx")])
        tap("kcT", kcT, [B("kcT")]); tap("vcx", vcx, [B("vcx")]); tap("KT_slc", KT_slc, [B("KT_slc")]); tap("Vx_slc", Vx_slc, [B("Vx_slc")])
        if stop_after <= 2:
            S.emit(); return nc
        S.barrier()
        AR.release(m_nsa)

        oT_nsa = AR.alloc([128, 8, NOWN], BF16, top=True)
        oT_sb = AR.alloc([128, 8, NOWN], BF16, top=True)
        QT_nsa = AR.alloc([128, 8, NOWN], BF16)
        for hq in range(8):
            S.dma("sp", QT_nsa[:, hq, :], scr_qn[hq, :, :], reads=[B("scr_qn")], writes=[B("QT_nsa", hq)])
        tap("QT_nsa", QT_nsa, [B("QT_nsa", c) for c in range(8)])
        o_acc = AR.alloc([128, 8, 4, 128], F32)
        o_bf = AR.alloc([128, 8, 4, 128], BF16)
        imp = AR.alloc([128, 8, 32], F32)
        imp_tmp = AR.alloc([128, 4, 32], F32)
        sc = AR.alloc([128, 8, 64], F32)
        m8 = AR.alloc([128, 8, 16], F32)
        sel = AR.alloc([128, 8, 32], F32)
        mbT = AR.alloc([32, NOWN], BF16)
        coef = AR.alloc([128, 512], F32)
        eT = [AR.alloc([128, 512], BF16) for _ in range(2)]
        PT = [AR.alloc([128, 2, 256], BF16) for _ in range(3)]
        coef_i = [0]

        def coef_slot():
            i = coef_i[0] % 128
            coef_i[0] += 1
            return i * 4, B("coef", i)

        pt_i = [0]
        for kvh in range(2):
            for g in range(4):
                head = kvh * 4 + g
                for tc in range(2):
                    bk = next_bank(0, 3)
                    S.op("pe", lambda e: e.matmul(out=ps[bk][:, :], lhsT=kcT[:, kvh, :], rhs=QT_nsa[:, head, tc * 512:(tc + 1) * 512], start=True, stop=False),
                         reads=[B("kcT"), B("QT_nsa", head)], writes=[PB[bk]], signal=False)
                    S.op("pe", lambda e: e.matmul(out=ps[bk][:, :], lhsT=ident, rhs=cmpb[:, tc * 512:(tc + 1) * 512], start=False, stop=True),
                         reads=[bC], writes=[PB[bk]])
                    sl = pt_i[0] % 2
                    pt_i[0] += 1
                    S.op("act", lambda e: e.activation(out=eT[sl], in_=ps[bk][:, :], func=AF.Exp, scale=SCALE),
                         reads=[PB[bk]], writes=[B("eT", sl)])
                    par = (g * 2 + tc) % 2
                    bo_o = 3 + par
                    bo_i = 5 + par
                    for j in range(4):
                        S.op("pe", lambda e: e.matmul(out=ps[bo_o][:, j * 128:(j + 1) * 128], lhsT=eT[sl][:, j * 128:(j + 1) * 128], rhs=vcx[:, kvh, 0:128],
                                                      start=True, stop=True),
                             reads=[B("eT", sl), B("vcx")], writes=[PB[bo_o]], signal=(j == 3))
                    for j in range(4):
                        S.op("pe", lambda e: e.matmul(out=ps[bo_i][:, j * 33:(j + 1) * 33], lhsT=eT[sl][:, j * 128:(j + 1) * 128], rhs=vcx[:, kvh, 128:161],
                                                      start=True, stop=True),
                             reads=[B("eT", sl), B("vcx")], writes=[PB[bo_i]], signal=(j == 3))
                    ca, bca = coef_slot(); cb_, bcb = coef_slot(); cg_, bcg = coef_slot()
                    pi3 = ps[bo_i][:, 0:132].rearrange("p (j c) -> p j c", j=4)
                    po3 = ps[bo_o][:, :].rearrange("p (j c) -> p j c", j=4)
                    tis = list(range(tc * 4, tc * 4 + 4))
                    S.op("dve", lambda e: e.tensor_scalar(out=coef[:, ca:ca + 4], in0=pi3[:, :, 32], scalar1=1e-30, scalar2=None, op0=ALU.max),
                         reads=[PB[bo_i]], writes=[bca])
                    S.op("dve", lambda e: e.reciprocal(out=coef[:, cb_:cb_ + 4], in_=coef[:, ca:ca + 4]), reads=[bca], writes=[bcb])
                    S.op("dve", lambda e: e.tensor_tensor(out=coef[:, cg_:cg_ + 4], in0=coef[:, cb_:cb_ + 4], in1=gates_sb[:, tc * 4:(tc + 1) * 4, head * 3],
                                                          op=ALU.mult), reads=[bcb, B("gates")], writes=[bcg])
                    bimps = [B("imp", ti) for ti in tis]
                    rden_b = coef[:, cb_:cb_ + 4].unsqueeze(2).to_broadcast([128, 4, 32])
                    if g == 0:
                        S.op("dve", lambda e: e.tensor_tensor(out=imp[:, tc * 4:(tc + 1) * 4, :], in0=pi3[:, :, 0:32], in1=rden_b, op=ALU.mult),
                             reads=[PB[bo_i], bcb], writes=bimps)
                    else:
                        S.op("dve", lambda e: e.tensor_tensor(out=imp_tmp, in0=pi3[:, :, 0:32], in1=rden_b, op=ALU.mult),
                             reads=[PB[bo_i], bcb], writes=[B("imp_tmp")])
                        S.op("dve", lambda e: e.tensor_tensor(out=imp[:, tc * 4:(tc + 1) * 4, :], in0=imp[:, tc * 4:(tc + 1) * 4, :], in1=imp_tmp, op=ALU.add),
                             reads=[B("imp_tmp")] + bimps, writes=bimps)
                    cg_b = coef[:, cg_:cg_ + 4].unsqueeze(2).to_broadcast([128, 4, 128])
                    S.op("dve", lambda e: e.tensor_tensor(out=o_acc[:, tc * 4:(tc + 1) * 4, g, :], in0=po3, in1=cg_b, op=ALU.mult),
                         reads=[PB[bo_o], bcg], writes=[B("o_acc", ti, g) for ti in tis])
            for ti in range(8):
                bimp = B("imp", ti)
                S.op("dve", lambda e, ti=ti: e.tensor_tensor(out=sc[:, ti, 0:32], in0=imp[:, ti, :], in1=fb[:, ti, :], op=ALU.add),
                     reads=[bimp, bC], writes=[B("sc", ti)])
                S.op("dve", lambda e, ti=ti: e.max(out=m8[:, ti, 0:8], in_=sc[:, ti, 0:32]), reads=[B("sc", ti)], writes=[B("m8", ti)])
                S.op("dve", lambda e, ti=ti: e.match_replace(out=sc[:, ti, 32:64], in_to_replace=m8[:, ti, 0:8], in_values=sc[:, ti, 0:32], imm_value=-3e9),
                     reads=[B("sc", ti), B("m8", ti)], writes=[B("sc2", ti)])
                S.op("dve", lambda e, ti=ti: e.max(out=m8[:, ti, 8:16], in_=sc[:, ti, 32:64]), reads=[B("sc2", ti)], writes=[B("m8", ti)])
                S.op("dve", lambda e, ti=ti: e.tensor_scalar(out=sel[:, ti, :], in0=sc[:, ti, 0:32], scalar1=m8[:, ti, 15:16], scalar2=None, op0=ALU.is_ge),
                     reads=[B("sc", ti), B("m8", ti)], writes=[B("sel", ti)])
                S.op("dve", lambda e, ti=ti: e.tensor_tensor(out=sel[:, ti, :], in0=sel[:, ti, :], in1=vm[:, ti, :], op=ALU.mult),
                     reads=[B("sel", ti), bC], writes=[B("sel", ti)])
                bk = next_bank(0, 3)
                S.op("pe", lambda e, bk=bk, ti=ti: e.transpose(out=ps[bk][0:32, 0:128], in_=sel[:, ti, :], identity=identf),
                     reads=[B("sel", ti), bC], writes=[PB[bk]])
                S.op("dve", lambda e, bk=bk, ti=ti: e.tensor_scalar(out=mbT[:, ti * 128:(ti + 1) * 128], in0=ps[bk][0:32, 0:128], scalar1=-NEG, scalar2=NEG,
                                                                   op0=ALU.mult, op1=ALU.add),
                     reads=[PB[bk]], writes=[B("mbT")])
            if kvh == 0:
                tap("imp", imp, [B("imp", ti) for ti in range(8)])
                tap("sel", sel, [B("sel", ti) for ti in range(8)])
                if stop_after <= 2.5:
                    S.emit(); return nc
            items = []
            seq = 0
            for g in range(4):
                for p in range(4):
                    for branch in range(2):
                        tiles = list(range(0, 4 * p + 4)) if branch == 0 else list(range(max(0, 4 * p - 4), 4 * p + 4))
                        npairs = len(tiles) // 2
                        for m in range(npairs):
                            items.append(dict(g=g, p=p, branch=branch, tiles=tiles, m=m, npairs=npairs, seq=seq))
                        seq += 1

            def sel_front(it, i):
                g, p, branch, tiles, m = it["g"], it["p"], it["branch"], it["tiles"], it["m"]
                head = kvh * 4 + g
                KT = KT_slc if branch == 0 else KT_win
                bKT = B("KT_slc") if branch == 0 else B("KT_win")
                bk = i % 3
                sl = i % 3
                for s in range(2):
                    G = tiles[2 * m + s]
                    o_ap = ps[bk][:, s * 256:(s + 1) * 256]
                    extra = []
                    if branch == 0:
                        extra.append((Emat[:, G, :], mbT[:, p * 256:(p + 1) * 256], [bC, B("mbT")]))
                        if G >= 4 * p:
                            extra.append((ident, cbi[:, G - 4 * p, :], [bC]))
                    else:
                        extra.append((ident, wbm[:, G - (4 * p - 4), :], [bC]))
                    S.op("pe", lambda e: e.matmul(out=o_ap, lhsT=KT[:, kvh, G * 128:(G + 1) * 128], rhs=QT_nsa[:, head, p * 256:(p + 1) * 256],
                                                  start=True, stop=False),
                         reads=[bKT, B("QT_nsa", head)], writes=[PB[bk]], signal=False)
                    for xi, (l_ap, r_ap, rb) in enumerate(extra):
                        last = xi == len(extra) - 1
                        S.op("pe", lambda e: e.matmul(out=o_ap, lhsT=l_ap, rhs=r_ap, start=False, stop=last),
                             reads=rb, writes=[PB[bk]], signal=(last and s == 1))

            def sel_exp(it, i):
                bk = i % 3
                sl = i % 3
                S.op("act", lambda e: e.activation(out=PT[sl].rearrange("p a b -> p (a b)"), in_=ps[bk][:, :], func=AF.Exp, scale=SCALE),
                     reads=[PB[bk]], writes=[B("PT", sl)])

            def sel_back(it, i):
                g, p, branch, tiles, m, npairs = it["g"], it["p"], it["branch"], it["tiles"], it["m"], it["npairs"]
                head = kvh * 4 + g
                Vx = Vx_slc if branch == 0 else Vx_win
                bVx = B("Vx_slc") if branch == 0 else B("Vx_win")
                sl = i % 3
                bo0 = 3 + 2 * (it["seq"] % 2)
                for s in range(2):
                    G = tiles[2 * m + s]
                    for j in range(2):
                        first = (m == 0 and s == 0)
                        last = (m == npairs - 1 and s == 1)
                        bo = bo0 + j
                        S.op("pe", lambda e: e.matmul(out=ps[bo][:, 0:129], lhsT=PT[sl][:, s, j * 128:(j + 1) * 128], rhs=Vx[:, G, kvh, 0:129],
                                                      start=first, stop=last),
                             reads=[B("PT", sl), bVx], writes=[PB[bo]], signal=last)
                if m != npairs - 1:
                    return
                for j in range(2):
                    ti = 2 * p + j
                    bo = bo0 + j
                    c0, bc = coef_slot()
                    S.op("dve", lambda e: e.tensor_scalar(out=coef[:, c0:c0 + 1], in0=ps[bo][:, 128:129], scalar1=1e-30,
                                                          scalar2=None, op0=ALU.max), reads=[PB[bo]], writes=[bc])
                    S.op("dve", lambda e: e.reciprocal(out=coef[:, c0 + 1:c0 + 2], in_=coef[:, c0:c0 + 1]), reads=[bc], writes=[bc])
                    S.op("dve", lambda e: e.tensor_tensor(out=coef[:, c0 + 2:c0 + 3], in0=coef[:, c0 + 1:c0 + 2],
                                                          in1=gates_sb[:, ti, head * 3 + 1 + branch:head * 3 + 2 + branch], op=ALU.mult),
                         reads=[bc, B("gates")], writes=[bc])
                    dst = o_acc[:, ti, g, :] if branch == 0 else o_bf[:, ti, g, :]
                    dbuf = B("o_acc", ti, g) if branch == 0 else B("o_bf", ti, g)
                    S.op("dve", lambda e: e.scalar_tensor_tensor(out=dst, in0=ps[bo][:, 0:128], scalar=coef[:, c0 + 2:c0 + 3],
                                                                 in1=o_acc[:, ti, g, :], op0=ALU.mult, op1=ALU.add),
                         reads=[PB[bo], bc, B("o_acc", ti, g)], writes=[dbuf])
                if p == 3 and branch == 1:
                    for half in range(2):
                        bkt = 7
                        pst = ps[bkt].bitcast(BF16)
                        for j in range(4):
                            ti = half * 4 + j
                            S.op("pe", lambda e: e.transpose(out=pst[:, j * 128:(j + 1) * 128], in_=o_bf[:, ti, g, :], identity=ident),
                                 reads=[B("o_bf", ti, g), bC], writes=[PB[bkt]], signal=(j == 3))
                        S.op("act", lambda e: e.activation(out=oT_nsa[:, head, half * 512:(half + 1) * 512], in_=pst[:, 0:512], func=AF.Copy),
                             reads=[PB[bkt]], writes=[B("oT_nsa", head)])

            sel_front(items[0], 0)
            sel_front(items[1], 1)
            sel_exp(items[0], 0)
            for i, it in enumerate(items):
                if i + 2 < len(items):
                    sel_front(items[i + 2], i + 2)
                if i + 1 < len(items):
                    sel_exp(items[i + 1], i + 1)
                sel_back(it, i)
        tap("oT_nsa", oT_nsa, [B("oT_nsa", h) for h in range(8)])
        if stop_after <= 3:
            S.emit(); return nc
        S.barrier()
        AR.release(m_nsa)

        sbq = [AR.alloc([128, NOWN], BF16) for _ in range(2)]
        sbk = [AR.alloc([128, T], BF16) for _ in range(2)]
        sbv = [AR.alloc([128, 16, 128], BF16) for _ in range(2)]
        e_sb = [AR.alloc([128, 512], F32) for _ in range(2)]
        sp_sb = [AR.alloc([128, 512], F32) for _ in range(2)]
        spb = [AR.alloc([128, 2, 256], BF16) for _ in range(2)]
        t_sb = [AR.alloc([128, 2, 256], F32) for _ in range(2)]
        AT = [AR.alloc([128, 2, 256], BF16) for _ in range(2)]
        Rsum = AR.alloc([128, 256], F32)
        items = []
        for head in range(8):
            for p in range(4):
                npairs = 2 * p + 2
                for mi, m in enumerate(range(npairs - 1, -1, -1)):
                    items.append(dict(head=head, p=p, mi=mi, m=m, npairs=npairs))
        loaded = set()

        def sb_s1(it, i):
            head, p, mi, m = it["head"], it["p"], it["mi"], it["m"]
            hs = head % 2
            if head not in loaded:
                loaded.add(head)
                S.dma("sp", sbq[hs], scr_qsb[head, :, :], reads=[B("scr_qsb")], writes=[B("sbq", hs)])
                S.dma("sp", sbk[hs], scr_ksb[head, :, :], reads=[B("scr_ksb")], writes=[B("sbk", hs)])
                S.dma("sp", sbv[hs], scr_vsb[:, :, head * 128:(head + 1) * 128], reads=[B("scr_vsb")], writes=[B("sbv", hs)])
            bz = i % 3
            for s in range(2):
                G = 2 * m + s
                o_ap = ps[bz][:, s * 256:(s + 1) * 256]
                diag = G >= 4 * p
                S.op("pe", lambda e: e.matmul(out=o_ap, lhsT=sbk[hs][:, G * 128:(G + 1) * 128], rhs=sbq[hs][:, p * 256:(p + 1) * 256],
                                              start=True, stop=(not diag)),
                     reads=[B("sbk", hs), B("sbq", hs)], writes=[PB[bz]], signal=(s == 1 and not diag))
                if diag:
                    S.op("pe", lambda e: e.matmul(out=o_ap, lhsT=ident, rhs=cbs[:, G - 4 * p, :], start=False, stop=True),
                         reads=[bC], writes=[PB[bz]], signal=(s == 1))

        sb_dve_cast = False
        SB_BF16_SP = True
        sb_cast_pending = []

        def sb_s2(it, i):
            sl = i % 2
            bz = i % 3
            bcn = 3 + i % 2
            btot = 5
            S.op("act", lambda e: e.activation(out=e_sb[sl], in_=ps[bz][:, :], func=AF.Exp, scale=SCALE),
                 reads=[PB[bz]], writes=[B("e_sb", sl)])
            if SB_BF16_SP:
                S.op("act", lambda e: e.activation(out=spb[sl].rearrange("p a b -> p (a b)"), in_=e_sb[sl], func=AF.Ln, bias=1.0),
                     reads=[B("e_sb", sl)], writes=[B("spb", sl)])
                sb_s2b(it, i)
                return
            S.op("act", lambda e: e.activation(out=sp_sb[sl], in_=e_sb[sl], func=AF.Ln, bias=1.0),
                 reads=[B("e_sb", sl)], writes=[B("sp_sb", sl)])
            if not sb_dve_cast:
                S.op("act", lambda e: e.activation(out=spb[sl].rearrange("p a b -> p (a b)"), in_=e_sb[sl], func=AF.Ln, bias=1.0),
                     reads=[B("e_sb", sl)], writes=[B("spb", sl)])
            else:
                sb_cast_pending.append((sl, it, i))
                return
            sb_s2b(it, i)

        def sb_s2b(it, i):
            sl = i % 2
            bcn = 3 + i % 2
            btot = 5
            S.op("pe", lambda e: e.matmul(out=ps[bcn][:, 256:512], lhsT=negtri, rhs=spb[sl][:, 1, :], start=True, stop=True),
                 reads=[bC, B("spb", sl)], writes=[PB[bcn]], signal=False)
            S.op("pe", lambda e: e.matmul(out=ps[bcn][:, 0:256], lhsT=negtri, rhs=spb[sl][:, 0, :], start=True, stop=False),
                 reads=[bC, B("spb", sl)], writes=[PB[bcn]], signal=False)
            S.op("pe", lambda e: e.matmul(out=ps[bcn][:, 0:256], lhsT=negones, rhs=spb[sl][:, 1, :], start=False, stop=True),
                 reads=[bC, B("spb", sl)], writes=[PB[bcn]])
            if it["mi"] < it["npairs"] - 1:
                S.op("pe", lambda e: e.matmul(out=ps[btot][:, 0:256], lhsT=negones, rhs=spb[sl][:, 0, :], start=True, stop=False),
                     reads=[bC, B("spb", sl)], writes=[PB[btot]], signal=False)
                S.op("pe", lambda e: e.matmul(out=ps[btot][:, 0:256], lhsT=negones, rhs=spb[sl][:, 1, :], start=False, stop=True),
                     reads=[bC, B("spb", sl)], writes=[PB[btot]])

        s3_dve_done = set()

        def sb_s3_dve(it, i):
            if i in s3_dve_done:
                return
            s3_dve_done.add(i)
            sl = i % 2
            bz = i % 3
            bcn = 3 + i % 2
            tf = t_sb[sl].rearrange("p a b -> p (a b)")
            if SB_BF16_SP and it["mi"] == 0:
                spo, spbuf = spb[sl].rearrange("p a b -> p (a b)"), B("spb", sl)
            else:
                spo, spbuf = sp_sb[sl], B("sp_sb", sl)
            S.op("dve", lambda e: e.scalar_tensor_tensor(out=tf, in0=ps[bz][:, :], scalar=SCALE, in1=spo, op0=ALU.mult, op1=ALU.subtract),
                 reads=[PB[bz], spbuf], writes=[B("t_sb", sl)])
            S.op("dve", lambda e: e.tensor_tensor(out=tf, in0=tf, in1=ps[bcn][:, :], op=ALU.add),
                 reads=[PB[bcn], B("t_sb", sl)], writes=[B("t_sb", sl)])

        def sb_s3(it, i):
            head, p, mi, m, npairs = it["head"], it["p"], it["mi"], it["m"], it["npairs"]
            hs = head % 2
            sl = i % 2
            bo = 6 + (p % 2)
            tf = t_sb[sl].rearrange("p a b -> p (a b)")
            sb_s3_dve(it, i)
            S.op("act", lambda e: e.activation(out=AT[sl].rearrange("p a b -> p (a b)"), in_=tf, func=AF.Exp),
                 reads=[B("t_sb", sl)], writes=[B("AT", sl)])
            for s in range(2):
                G = 2 * m + s
                first = (mi == 0 and s == 0)
                last = (mi == npairs - 1 and s == 1)
                S.op("pe", lambda e: e.matmul(out=ps[bo][:, 0:256], lhsT=sbv[hs][:, G, :], rhs=AT[sl][:, s, :], start=first, stop=last),
                     reads=[B("sbv", hs), B("AT", sl)], writes=[PB[bo]], signal=last)
            if mi == npairs - 1:
                S.op("act", lambda e: e.activation(out=oT_sb[:, head, p * 256:(p + 1) * 256], in_=ps[bo][:, 0:256], func=AF.Copy),
                     reads=[PB[bo]], writes=[B("oT_sb", head)])

        n_it = len(items)
        sb_s1(items[0], 0)
        sb_s1(items[1], 1)
        sb_s2(items[0], 0)
        if sb_dve_cast:
            slc, itc, ic = sb_cast_pending.pop()
            S.op("dve", lambda e: e.tensor_copy(out=spb[slc].rearrange("p a b -> p (a b)"), in_=sp_sb[slc]),
                 reads=[B("sp_sb", slc)], writes=[B("spb", slc)])
            sb_s2b(itc, ic)
        for i, it in enumerate(items):
            if it["mi"] == 0:
                S.op("dve", lambda e: e.tensor_copy(out=Rsum, in_=ps[5][:, 0:256]), reads=[PB[5]], writes=[B("Rsum")])
            elif it["mi"] < it["npairs"] - 1:
                S.op("dve", lambda e: e.tensor_tensor(out=Rsum, in0=Rsum, in1=ps[5][:, 0:256], op=ALU.add),
                     reads=[PB[5], B("Rsum")], writes=[B("Rsum")])
            if i + 2 < n_it:
                sb_s1(items[i + 2], i + 2)
            if i + 1 < n_it:
                sb_s2(items[i + 1], i + 1)
                if sb_dve_cast:
                    sb_s3_dve(it, i)
                    slc, itc, ic = sb_cast_pending.pop()
                    S.op("dve", lambda e: e.tensor_copy(out=spb[slc].rearrange("p a b -> p (a b)"), in_=sp_sb[slc]),
                         reads=[B("sp_sb", slc)], writes=[B("spb", slc)])
                    sb_s2b(itc, ic)
                if items[i + 1]["mi"] > 0:
                    sn = (i + 1) % 2
                    if SB_BF16_SP:
                        S.op("pool", lambda e: e.tensor_tensor(out=sp_sb[sn].rearrange("p (a b) -> p a b", a=2), in0=spb[sn],
                                                               in1=Rsum.unsqueeze(1).to_broadcast([128, 2, 256]), op=ALU.subtract),
                             reads=[B("spb", sn), B("Rsum")], writes=[B("sp_sb", sn)])
                    else:
                        S.op("pool", lambda e: e.tensor_tensor(out=sp_sb[sn].rearrange("p (a b) -> p a b", a=2), in0=sp_sb[sn].rearrange("p (a b) -> p a b", a=2),
                                                               in1=Rsum.unsqueeze(1).to_broadcast([128, 2, 256]), op=ALU.subtract),
                             reads=[B("sp_sb", sn), B("Rsum")], writes=[B("sp_sb", sn)])
            sb_s3(it, i)
        tap("oT_sb", oT_sb, [B("oT_sb", h) for h in range(8)])
        if stop_after <= 4:
            S.emit(); return nc
        S.barrier()
        AR.release(m_ess)

        mT = AR.alloc([128, 16, NOWN], BF16, top=True)
        hT2 = AR.alloc([128, 16, NOWN], BF16)
        bhT2 = B("hT2")
        S.dma("sp", hT2, scr_hTown[:, :, :], reads=[B("scr_hTown")], writes=[bhT2])
        wg = Ring("wg", 4, [128, 16, 128])
        wb_ = Ring("wb", 4, [128, 8, 128])
        gsb = [AR.alloc([128, 512], F32) for _ in range(2)]
        ysb = [AR.alloc([128, 512], F32) for _ in range(2)]
        gi = [0]
        for fc in range(16):
            wga, bga = wg.load([(lambda t: t, wsrc(w_in, 0, D, C_GA + fc * 128, 128))])
            wgb, bgb = wg.load([(lambda t: t, wsrc(w_in, 0, D, C_GB + fc * 128, 128))])
            wba, bba = wb_.load([(lambda t: t, wsrc(w_br_nsa, 0, 1024, fc * 128, 128))])
            wbb, bbb = wb_.load([(lambda t: t, wsrc(w_br_sb, 0, 1024, fc * 128, 128))])
            for tc in range(2):
                tsl = slice(tc * 512, (tc + 1) * 512)
                res = []
                for (wgt, bwg, wbr, bwb, oT, obn) in ((wga, bga, wba, bba, oT_nsa, "oT_nsa"), (wgb, bgb, wbb, bbb, oT_sb, "oT_sb")):
                    bkg = next_bank(0, 4)
                    for kc in range(16):
                        S.op("pe", lambda e, bkg=bkg, wgt=wgt, kc=kc, tsl=tsl: e.matmul(out=ps[bkg][:, :], lhsT=wgt[:, kc, :], rhs=hT2[:, kc, tsl],
                                                                                    start=(kc == 0), stop=(kc == 15)),
                             reads=[bwg, bhT2], writes=[PB[bkg]], signal=(kc == 15))
                    sl = gi[0] % 2
                    S.op("act", lambda e, bkg=bkg, sl=sl: e.activation(out=gsb[sl], in_=ps[bkg][:, :], func=AF.Sigmoid), reads=[PB[bkg]], writes=[B("gsb", sl)])
                    bky = 4 + (gi[0] % 4)
                    gi[0] += 1
                    for kc in range(8):
                        S.op("pe", lambda e, bky=bky, wbr=wbr, kc=kc, tsl=tsl, oT=oT: e.matmul(out=ps[bky][:, :], lhsT=wbr[:, kc, :], rhs=oT[:, kc, tsl],
                                                                                           start=(kc == 0), stop=(kc == 7)),
                             reads=[bwb] + [B(obn, h) for h in range(8)], writes=[PB[bky]], signal=(kc == 7))
                    res.append((sl, bky))
                (sa, ya), (sb_, yb) = res
                S.op("dve", lambda e, sa=sa, ya=ya: e.tensor_tensor(out=ysb[0], in0=gsb[sa], in1=ps[ya][:, :], op=ALU.mult),
                     reads=[B("gsb", sa), PB[ya]], writes=[B("ysb", 0)])
                S.op("dve", lambda e, sb_=sb_, yb=yb: e.tensor_tensor(out=ysb[1], in0=gsb[sb_], in1=ps[yb][:, :], op=ALU.mult),
                     reads=[B("gsb", sb_), PB[yb]], writes=[B("ysb", 1)])
                S.op("dve", lambda e, fc=fc, tsl=tsl: e.tensor_tensor(out=mT[:, fc, tsl], in0=ysb[0], in1=ysb[1], op=ALU.add),
                     reads=[B("ysb", 0), B("ysb", 1)], writes=[B("mT")])
        tap("mT", mT, [B("mT")])
        wo_start = AR.mark()
        wo_sb = AR.alloc([128, 16, D], BF16)
        wo_end = AR.mark()
        for q4 in range(4):
            S.dma("pool", wo_sb[:, q4 * 4:(q4 + 1) * 4, :], wsrc(w_o, q4 * 512, 512, 0, D), writes=[B("wo", q4)])
        S.barrier(exclude=[B("wo", q4) for q4 in range(4)])
        AR.release(m_ess)
        x1 = AR.alloc([128, 8, D], F32)
        m_res = AR.mark()
        assert AR.mark() <= wo_start, (AR.mark(), wo_start)
        AR.off = wo_end
        S.dma("sp", x1, x_own.rearrange("(a p) d -> p a d", p=128), writes=[B("x1")])
        for tt in range(8):
            for dc in range(4):
                bk = next_bank(0, 4)
                for kc in range(16):
                    S.op("pe", lambda e, bk=bk, kc=kc, tt=tt, dc=dc: e.matmul(out=ps[bk][:, :], lhsT=mT[:, kc, tt * 128:(tt + 1) * 128], rhs=wo_sb[:, kc, dc * 512:(dc + 1) * 512],
                                                                         start=(kc == 0), stop=(kc == 15)),
                         reads=[B("mT"), B("wo", kc // 4)], writes=[PB[bk]], signal=(kc == 15))
                S.op("dve", lambda e, bk=bk, tt=tt, dc=dc: e.tensor_tensor(out=x1[:, tt, dc * 512:(dc + 1) * 512], in0=x1[:, tt, dc * 512:(dc + 1) * 512], in1=ps[bk][:, :], op=ALU.add),
                     reads=[PB[bk], B("x1")], writes=[B("x1", tt)])
        tap("x1", x1, [B("x1", tt) for tt in range(8)])
        if stop_after <= 5:
            S.emit(); return nc
        S.barrier()
        AR.release(m_res)
        AR.release_top()

        hT3 = AR.alloc([128, 16, NOWN], BF16); bhT3 = B("hT3")
        m_I = AR.mark()
        QcT = AR.alloc([128, 4, NOWN], BF16)
        KcT = AR.alloc([128, 4, 256], BF16)
        Vcx = AR.alloc([128, 2, 4, 130], BF16)
        oc_bf = AR.alloc([128, 8, 512], BF16)
        ocT = AR.alloc([128, 4, NOWN], BF16)
        wco = AR.alloc([128, 4, D], BF16)
        PTc = [AR.alloc([128, 2, 512], BF16) for _ in range(2)]
        coefx = AR.alloc([128, 256], F32)
        memT = AR.alloc([128, 16, 256], BF16)
        m_I1 = AR.mark()
        n_xt = [AR.alloc([128, D], F32) for _ in range(2)]
        n_junk = [AR.alloc([128, D], BF16) for _ in range(2)]
        n_hb = [AR.alloc([128, D], BF16) for _ in range(2)]

        def x1_tiles(tt):
            return x1[:, tt, :], B("x1", tt)
        cnt_t = [0]

        def sinkT(dstT, dbuf):
            def f(tt, hb, bh):
                pb = (cnt_t[0] % 2) * 2
                cnt_t[0] += 1
                transpose_to(hb, bh, dstT, [dbuf], tt, pb)
            return f
        norm_tiles(None, 8, g_cross, tsink(hT3, bhT3), xin=x1_tiles)
        norm_tiles(mem, 2, g_mem, tsink(memT, B("memT")))
        S.barrier()
        AR.release(m_I1)
        wc = [AR.alloc([128, 16, 512], BF16) for _ in range(2)]
        S.dma("pool", wc[0], wsrc(w_ck, 0, D, 0, 512), writes=[B("wc", 0)])
        S.dma("pool", wc[1], wsrc(w_cv, 0, D, 0, 512), writes=[B("wc", 1)])
        S.dma("pool", wco, wsrc(w_co, 0, 512, 0, D), writes=[B("wco")])
        for hh in range(4):
            bk = next_bank(4, 4)
            for kc in range(16):
                S.op("pe", lambda e, bk=bk, kc=kc, hh=hh: e.matmul(out=ps[bk][:, 0:256], lhsT=wc[0][:, kc, hh * 128:(hh + 1) * 128], rhs=memT[:, kc, :],
                                                              start=(kc == 0), stop=(kc == 15)),
                     reads=[B("wc", 0), B("memT")], writes=[PB[bk]], signal=(kc == 15))
            S.op("act", lambda e, bk=bk, hh=hh: e.activation(out=KcT[:, hh, :], in_=ps[bk][:, 0:256], func=AF.Copy), reads=[PB[bk]], writes=[B("KcT")])
        S.op("pool", lambda e: e.memset(Vcx.rearrange("p a b c -> p (a b c)"), 1.0), writes=[B("Vcx")])
        for mt in range(2):
            bk = next_bank(4, 4)
            for kc in range(16):
                S.op("pe", lambda e, bk=bk, kc=kc, mt=mt: e.matmul(out=ps[bk][:, :], lhsT=memT[:, kc, mt * 128:(mt + 1) * 128], rhs=wc[1][:, kc, :],
                                                              start=(kc == 0), stop=(kc == 15)),
                     reads=[B("wc", 1), B("memT")], writes=[PB[bk]], signal=(kc == 15))
            S.op("dve", lambda e, bk=bk, mt=mt: e.tensor_copy(out=Vcx[:, mt, :, 0:128], in_=ps[bk][:, :].rearrange("p (h d) -> p h d", h=4)),
                 reads=[PB[bk]], writes=[B("Vcx")])
        S.dma("pool", wc[0], wsrc(w_cq, 0, D, 0, 512), writes=[B("wc", 0)])
        for hh in range(4):
            for tc in range(2):
                bk = next_bank(4, 4)
                for kc in range(16):
                    S.op("pe", lambda e, bk=bk, kc=kc, hh=hh, tc=tc: e.matmul(out=ps[bk][:, :], lhsT=wc[0][:, kc, hh * 128:(hh + 1) * 128], rhs=hT3[:, kc, tc * 512:(tc + 1) * 512],
                                                                         start=(kc == 0), stop=(kc == 15)),
                         reads=[B("wc", 0), bhT3], writes=[PB[bk]], signal=(kc == 15))
                S.op("act", lambda e, bk=bk, hh=hh, tc=tc: e.activation(out=QcT[:, hh, tc * 512:(tc + 1) * 512], in_=ps[bk][:, :], func=AF.Copy),
                     reads=[PB[bk]], writes=[B("QcT", hh)])
        ci = [0]
        for hh in range(4):
            for tc in range(2):
                sl = ci[0] % 2
                ci[0] += 1
                for mt in range(2):
                    bk = next_bank(4, 4)
                    S.op("pe", lambda e, bk=bk, hh=hh, tc=tc, mt=mt: e.matmul(out=ps[bk][:, :], lhsT=KcT[:, hh, mt * 128:(mt + 1) * 128], rhs=QcT[:, hh, tc * 512:(tc + 1) * 512],
                                                                         start=True, stop=True),
                         reads=[B("KcT"), B("QcT", hh)], writes=[PB[bk]])
                    S.op("act", lambda e, bk=bk, sl=sl, mt=mt: e.activation(out=PTc[sl][:, mt, :], in_=ps[bk][:, :], func=AF.Exp, scale=SCALE),
                         reads=[PB[bk]], writes=[B("PTc", sl)])
                for j in range(4):
                    ti = tc * 4 + j
                    bo = next_bank(0, 4)
                    for mt in range(2):
                        S.op("pe", lambda e, bo=bo, sl=sl, mt=mt, j=j, hh=hh: e.matmul(out=ps[bo][:, 0:129], lhsT=PTc[sl][:, mt, j * 128:(j + 1) * 128], rhs=Vcx[:, mt, hh, 0:129],
                                                                                  start=(mt == 0), stop=(mt == 1)),
                             reads=[B("PTc", sl), B("Vcx")], writes=[PB[bo]], signal=(mt == 1))
                    c0, bc = coef_slot_x = ((ci[0] * 8 + j) % 64) * 4, B("coefx", (ci[0] * 8 + j) % 64)
                    S.op("dve", lambda e, bo=bo, c0=c0: e.reciprocal(out=coefx[:, c0 + 1:c0 + 2], in_=ps[bo][:, 128:129]), reads=[PB[bo]], writes=[bc])
                    S.op("act", lambda e, bo=bo, c0=c0, ti=ti, hh=hh: e.activation(out=oc_bf[:, ti, hh * 128:(hh + 1) * 128], in_=ps[bo][:, 0:128], func=AF.Copy,
                                                                              scale=coefx[:, c0 + 1:c0 + 2]),
                         reads=[PB[bo], bc], writes=[B("oc_bf", ti)])
        for hh in range(4):
            for half in range(2):
                bk = next_bank(4, 4)
                pst = ps[bk].bitcast(BF16)
                for j in range(4):
                    ti = half * 4 + j
                    S.op("pe", lambda e, pst=pst, j=j, ti=ti, hh=hh: e.transpose(out=pst[:, j * 128:(j + 1) * 128], in_=oc_bf[:, ti, hh * 128:(hh + 1) * 128], identity=ident),
                         reads=[B("oc_bf", ti), bC], writes=[PB[bk]], signal=(j == 3))
                S.op("act", lambda e, pst=pst, hh=hh, half=half: e.activation(out=ocT[:, hh, half * 512:(half + 1) * 512], in_=pst[:, 0:512], func=AF.Copy),
                     reads=[PB[bk]], writes=[B("ocT")])
        for tt in range(8):
            for dc in range(4):
                bk = next_bank(0, 4)
                for kc in range(4):
                    S.op("pe", lambda e, bk=bk, kc=kc, tt=tt, dc=dc: e.matmul(out=ps[bk][:, :], lhsT=ocT[:, kc, tt * 128:(tt + 1) * 128], rhs=wco[:, kc, dc * 512:(dc + 1) * 512],
                                                                         start=(kc == 0), stop=(kc == 3)),
                         reads=[B("ocT"), B("wco")], writes=[PB[bk]], signal=(kc == 3))
                S.op("dve", lambda e, bk=bk, tt=tt, dc=dc: e.tensor_tensor(out=x1[:, tt, dc * 512:(dc + 1) * 512], in0=x1[:, tt, dc * 512:(dc + 1) * 512], in1=ps[bk][:, :], op=ALU.add),
                     reads=[PB[bk], B("x1", tt)], writes=[B("x1", tt)])
        tap("x2", x1, [B("x1", tt) for tt in range(8)])
        if stop_after <= 6:
            S.emit(); return nc
        S.barrier()
        AR.release(m_I)

        n_xt = None
        comb = AR.alloc([128, 8, 32], F32)
        mJ0 = AR.mark()
        ering = Ring("ering", 4, [128, 16, 512], top=True)

        def load_expert(ex):
            wgt, bwg = ering.load([(lambda t: t[:, 0:8, :], wsrc(w_eg[ex], 0, 1024, 0, 512)), (lambda t: t[:, 8:16, :], wsrc(w_eg[ex], 1024, 1024, 0, 512))])
            wut, bwu = ering.load([(lambda t: t[:, 0:8, :], wsrc(w_eu[ex], 0, 1024, 0, 512)), (lambda t: t[:, 8:16, :], wsrc(w_eu[ex], 1024, 1024, 0, 512))])
            wdt_, bwd = ering.load([(lambda t: t.rearrange("p a b -> p (a b)")[:, 0:4 * D].rearrange("p (a b) -> p a b", a=4), wsrc(w_ed[ex], 0, 512, 0, D))])
            return wgt, bwg, wut, bwu, wdt_, bwd
        pre_ex0 = load_expert(0)
        n_junk = [AR.alloc([128, D], BF16) for _ in range(1)] * 2
        n_hb = [AR.alloc([128, D], BF16) for _ in range(2)]
        hn32 = AR.alloc([128, D], F32)
        hnT32 = AR.alloc([128, 16, 128], F32)
        wr_sb = AR.alloc([128, 16, 36], F32)
        br_sb = AR.alloc([128, 36], F32)
        rt = AR.alloc([128, 8, 96], F32)
        S.dma("sp", wr_sb, w_r.rearrange("(k p) c -> p k c", p=128), writes=[B("wr")])
        S.dma("sp", br_sb, b_r[0:1, :].to_broadcast([128, 36]), writes=[B("wr")])

        def sinkJ(tt, hb, bh):
            pb = (cnt_t[0] % 2) * 2
            cnt_t[0] += 1
            transpose_to(hb, bh, hT3, [bhT3], tt, pb)
            c4 = norm_cur["c4"]
            S.op("dve", lambda e, tt=tt, c4=c4: e.scalar_tensor_tensor(out=hn32, in0=x1[:, tt, :], scalar=stats[:, c4 + 3:c4 + 4], in1=gb, op0=ALU.mult, op1=ALU.mult),
                 reads=[B("x1", tt), B("stats", norm_cur["col"]), B("gb")], writes=[B("hn32")])
            for q in range(4):
                bk = 4 + q % 2
                for k4 in range(4):
                    kc = q * 4 + k4
                    S.op("pe", lambda e, bk=bk, k4=k4, kc=kc: e.transpose(out=ps[bk][:, k4 * 128:(k4 + 1) * 128], in_=hn32[:, kc * 128:(kc + 1) * 128], identity=identf),
                         reads=[B("hn32"), bC], writes=[PB[bk]], signal=(k4 == 3))
                S.op("dve", lambda e, bk=bk, q=q: e.tensor_copy(out=hnT32[:, q * 4:(q + 1) * 4, :], in_=ps[bk][:, :].rearrange("p (k t) -> p k t", k=4)),
                     reads=[PB[bk]], writes=[B("hnT32")])
            bk = 6 + tt % 2
            for kc in range(16):
                S.op("pe", lambda e, bk=bk, kc=kc: e.matmul(out=ps[bk][:, 0:36], lhsT=hnT32[:, kc, :], rhs=wr_sb[:, kc, :], start=(kc == 0), stop=(kc == 15)),
                     reads=[B("hnT32"), B("wr")], writes=[PB[bk]], signal=(kc == 15))
            if router_pending:
                router_math(*router_pending.pop())
            router_pending.append((tt, bk))

        router_pending = []

        def router_math(tt, bk):
            R = rt[:, tt, :]
            br_ = B("rt", tt)
            S.op("dve", lambda e, bk=bk: e.tensor_tensor(out=R[:, 0:36], in0=ps[bk][:, 0:36], in1=br_sb, op=ALU.add), reads=[PB[bk], B("wr")], writes=[br_])
            S.op("dve", lambda e: e.tensor_reduce(out=R[:, 36:37], in_=R[:, 0:4], axis=mybir.AxisListType.X, op=ALU.max), reads=[br_], writes=[br_])
            S.op("dve", lambda e: e.tensor_scalar(out=R[:, 40:44], in0=R[:, 0:4], scalar1=R[:, 36:37], scalar2=None, op0=ALU.is_ge), reads=[br_], writes=[br_])
            S.op("dve", lambda e: e.tensor_scalar(out=R[:, 44:48], in0=R[:, 0:4], scalar1=R[:, 36:37], scalar2=None, op0=ALU.subtract), reads=[br_], writes=[br_])
            S.op("act", lambda e: e.activation(out=R[:, 44:48], in_=R[:, 44:48], func=AF.Exp, accum_out=R[:, 37:38]), reads=[br_], writes=[br_])
            S.op("dve", lambda e: e.reciprocal(out=R[:, 38:39], in_=R[:, 37:38]), reads=[br_], writes=[br_])
            S.op("dve", lambda e: e.tensor_scalar(out=R[:, 48:56], in0=R[:, 4:12], scalar1=R[:, 40:41], scalar2=None, op0=ALU.mult), reads=[br_], writes=[br_])
            for gq in range(1, 4):
                S.op("dve", lambda e, gq=gq: e.scalar_tensor_tensor(out=R[:, 48:56], in0=R[:, 4 + 8 * gq:12 + 8 * gq], scalar=R[:, 40 + gq:41 + gq], in1=R[:, 48:56],
                                                                   op0=ALU.mult, op1=ALU.add), reads=[br_], writes=[br_])
            S.op("dve", lambda e: e.max(out=R[:, 56:64], in_=R[:, 48:56]), reads=[br_], writes=[br_])
            S.op("dve", lambda e: e.tensor_tensor(out=R[:, 64:65], in0=R[:, 57:58], in1=R[:, 56:57], op=ALU.subtract), reads=[br_], writes=[br_])
            S.op("act", lambda e: e.activation(out=R[:, 65:66], in_=R[:, 64:65], func=AF.Exp), reads=[br_], writes=[br_])
            S.op("dve", lambda e: e.tensor_scalar(out=R[:, 65:66], in0=R[:, 65:66], scalar1=1.0, scalar2=None, op0=ALU.add), reads=[br_], writes=[br_])
            S.op("dve", lambda e: e.reciprocal(out=R[:, 66:67], in_=R[:, 65:66]), reads=[br_], writes=[br_])
            S.op("dve", lambda e: e.tensor_scalar(out=R[:, 67:68], in0=R[:, 66:67], scalar1=-1.0, scalar2=1.0, op0=ALU.mult, op1=ALU.add), reads=[br_], writes=[br_])
            S.op("dve", lambda e: e.tensor_scalar(out=R[:, 66:68], in0=R[:, 66:68], scalar1=R[:, 38:39], scalar2=None, op0=ALU.mult), reads=[br_], writes=[br_])
            S.op("dve", lambda e: e.tensor_scalar(out=R[:, 72:80], in0=R[:, 48:56], scalar1=R[:, 56:57], scalar2=R[:, 66:67], op0=ALU.is_equal, op1=ALU.mult),
                 reads=[br_], writes=[br_])
            S.op("dve", lambda e: e.tensor_scalar(out=R[:, 80:88], in0=R[:, 48:56], scalar1=R[:, 57:58], scalar2=R[:, 67:68], op0=ALU.is_equal, op1=ALU.mult),
                 reads=[br_], writes=[br_])
            S.op("dve", lambda e: e.tensor_tensor(out=R[:, 72:80], in0=R[:, 72:80], in1=R[:, 80:88], op=ALU.add), reads=[br_], writes=[br_])
            for gq in range(4):
                S.op("dve", lambda e, gq=gq: e.tensor_scalar(out=comb[:, tt, gq * 8:(gq + 1) * 8], in0=R[:, 72:80], scalar1=R[:, 40 + gq:41 + gq], scalar2=None, op0=ALU.mult),
                     reads=[br_], writes=[B("comb", tt)])
        norm_tiles(None, 8, g_moe, sinkJ, xin=x1_tiles)
        router_math(*router_pending.pop())
        tap("comb", comb, [B("comb", tt) for tt in range(8)])
        S.barrier(exclude=list(ering.b))
        AR.release(mJ0)
        mJ = AR.mark()
        hid = [AR.alloc([128, 4, NOWN], BF16) for _ in range(2)]
        sil = [AR.alloc([128, 512], F32) for _ in range(2)]
        si = [0]
        for ex in range(32):
            wgt, bwg, wut, bwu, wdt_, bwd = pre_ex0 if ex == 0 else load_expert(ex)
            wdt = wdt_.rearrange("p a b -> p (a b)")[:, 0:4 * D].rearrange("p (a b) -> p a b", a=4)
            hsl = ex % 2
            for fc in range(4):
                for tc in range(2):
                    ba = next_bank(0, 2)
                    bu = 2 + (pbi[0] % 2)
                    for kc in range(16):
                        S.op("pe", lambda e, ba=ba, kc=kc, fc=fc, tc=tc, wgt=wgt: e.matmul(out=ps[ba][:, :], lhsT=wgt[:, kc, fc * 128:(fc + 1) * 128], rhs=hT3[:, kc, tc * 512:(tc + 1) * 512],
                                                                                      start=(kc == 0), stop=(kc == 15)),
                             reads=[bwg, bhT3], writes=[PB[ba]], signal=(kc == 15))
                    for kc in range(16):
                        S.op("pe", lambda e, bu=bu, kc=kc, fc=fc, tc=tc, wut=wut: e.matmul(out=ps[bu][:, :], lhsT=wut[:, kc, fc * 128:(fc + 1) * 128], rhs=hT3[:, kc, tc * 512:(tc + 1) * 512],
                                                                                      start=(kc == 0), stop=(kc == 15)),
                             reads=[bwu, bhT3], writes=[PB[bu]], signal=(kc == 15))
                    ss_ = si[0] % 2
                    si[0] += 1
                    S.op("act", lambda e, ba=ba, ss_=ss_: e.activation(out=sil[ss_], in_=ps[ba][:, :], func=AF.Silu), reads=[PB[ba]], writes=[B("sil", ss_)])
                    S.op("dve", lambda e, bu=bu, ss_=ss_, hsl=hsl, fc=fc, tc=tc: e.tensor_tensor(out=hid[hsl][:, fc, tc * 512:(tc + 1) * 512], in0=sil[ss_], in1=ps[bu][:, :], op=ALU.mult),
                         reads=[B("sil", ss_), PB[bu]], writes=[B("hid", hsl)])
            for tt in range(8):
                for dc in range(4):
                    bk = 4 + (pbi[0] % 4)
                    pbi[0] += 1
                    for fc in range(4):
                        S.op("pe", lambda e, bk=bk, fc=fc, tt=tt, dc=dc, hsl=hsl, wdt=wdt: e.matmul(out=ps[bk][:, :], lhsT=hid[hsl][:, fc, tt * 128:(tt + 1) * 128],
                                                                                               rhs=wdt[:, fc, dc * 512:(dc + 1) * 512], start=(fc == 0), stop=(fc == 3)),
                             reads=[B("hid", hsl), bwd], writes=[PB[bk]], signal=(fc == 3))
                    S.op("dve", lambda e, bk=bk, tt=tt, dc=dc, ex=ex: e.scalar_tensor_tensor(out=x1[:, tt, dc * 512:(dc + 1) * 512], in0=ps[bk][:, :], scalar=comb[:, tt, ex:ex + 1],
                                                                                         in1=x1[:, tt, dc * 512:(dc + 1) * 512], op0=ALU.mult, op1=ALU.add),
                         reads=[PB[bk], B("comb", tt), B("x1", tt)], writes=[B("x1", tt)])
        tap("x3", x1, [B("x1", tt) for tt in range(8)])
        S.barrier()
        AR.release(mJ)
        yout = [AR.alloc([128, D], F32) for _ in range(2)]
        n_hb = yout
        n_junk = [AR.alloc([128, D], BF16)] * 2

        def sinkK(tt, hb, bh):
            S.tail.append(S.dma("sp", out_d[tt * 128:(tt + 1) * 128, :], hb, reads=[bh]))
        norm_tiles(None, 8, g_final, sinkK, xin=x1_tiles)
        S.emit()
    return nc


def _consts(h):
    c = {}
    c["c_ident"] = np.eye(128, dtype=np.float32)
    j = np.arange(128)
    c["c_negtri"] = -(j[:, None] > j[None, :]).astype(np.float32)
    c["c_negones"] = -np.ones((128, 128), np.float32)
    r = np.zeros((32, 32), np.float32)
    for m in range(16):
        r[m + 16, m] = -1.0
        r[m, m + 16] = 1.0
    c["c_r32"] = r
    inv = (500000.0 ** (-np.arange(16, dtype=np.float32) * (2.0 / 32))).astype(np.float32)
    invf = np.zeros((32, 2), np.float32)
    invf[:, 0] = np.concatenate([inv, inv]) / np.float32(2 * np.pi)
    c["c_invf"] = invf
    rel = [h, 3 - h]
    k = np.arange(128)[:, None]
    q = np.arange(256)[None, :]
    tq = np.array(rel)[q // 128] * 128 + q % 128
    cbi = np.zeros((128, 4, 256), np.float32); cbs = np.zeros((128, 4, 256), np.float32)
    for jj in range(4):
        s = jj * 128 + k
        cbi[:, jj, :] = np.where(s <= tq, 0.0, NEG)
        cbs[:, jj, :] = np.where(s < tq, 0.0, NEG)
    c["c_cbi"] = cbi.reshape(128, -1); c["c_cbs"] = cbs.reshape(128, -1)
    wb = np.zeros((128, 8, 256), np.float32)
    for jj in range(8):
        s = (jj - 4) * 128 + k
        dd = tq - s
        wb[:, jj, :] = np.where((dd >= 0) & (dd < 512), 0.0, NEG)
    c["c_wb"] = wb.reshape(128, -1)
    own_tiles = [2 * i + ((i % 2) ^ h) for i in range(8)]
    own_tok = np.concatenate([np.arange(g * 128, (g + 1) * 128) for g in own_tiles])
    cc = np.arange(128)[:, None]
    vis = (cc <= 126) & (cc * 16 + 31 <= own_tok[None, :])
    c["c_cmpb"] = np.where(vis, 0.0, NEG).astype(np.float32)
    jb = np.arange(32)[None, :]
    cur = (own_tok // 64)[:, None]
    forced = (jb == 0) | (jb == cur) | (jb == cur - 1)
    valid = jb <= cur
    fbv = np.where(valid, np.where(forced, 1000.0, 0.0), -1e9).astype(np.float32)
    c["c_fb"] = fbv.reshape(8, 128, 32).transpose(1, 0, 2).reshape(128, -1).copy()
    c["c_vm"] = valid.astype(np.float32).reshape(8, 128, 32).transpose(1, 0, 2).reshape(128, -1).copy()
    E = np.zeros((32, 16, 128), np.float32)
    for G in range(16):
        for kk in range(128):
            E[2 * G + kk // 64, G, kk] = 1.0
    c["c_E"] = E.reshape(32, -1)
    cs = np.arange(127) * 16
    ss = np.arange(32) * 64
    ov = ((cs[:, None] < ss[None, :] + 64) & (cs[:, None] + 32 > ss[None, :])).astype(np.float32)
    ovl = np.zeros((128, 33), np.float32)
    ovl[:127, :32] = ov
    ovl[:127, 32] = 1.0
    c["c_ovl"] = ovl
    return c, own_tok


_NC_CACHE = {}


def kernel(**inputs):
    x = np.asarray(inputs["x"], np.float32)
    f = lambda k: np.ascontiguousarray(np.asarray(inputs[k]))
    shared = {
        "g_mix": f("g_mix").reshape(1, D), "g_cross": f("g_cross").reshape(1, D), "g_mem": f("g_mem").reshape(1, D),
        "g_moe": f("g_moe").reshape(1, D), "g_final": f("g_final").reshape(1, D),
        "w_in": f("w_in").reshape(D, IN_W),
        "cmp_pe_k": f("cmp_pe_k").reshape(32, 128), "cmp_w1_k": f("cmp_w1_k").reshape(4096, 128), "cmp_w2_k": f("cmp_w2_k").reshape(128, 128),
        "cmp_pe_v": f("cmp_pe_v").reshape(32, 128), "cmp_w1_v": f("cmp_w1_v").reshape(4096, 128), "cmp_w2_v": f("cmp_w2_v").reshape(128, 128),
        "w_br_nsa": f("w_br_nsa").reshape(1024, D), "w_br_sb": f("w_br_sb").reshape(1024, D), "w_o": f("w_o").reshape(D, D),
        "w_cq": f("w_cq").reshape(D, 512), "w_ck": f("w_ck").reshape(D, 512), "w_cv": f("w_cv").reshape(D, 512), "w_co": f("w_co").reshape(512, D),
        "w_r": np.ascontiguousarray(np.concatenate([f("w_rg").reshape(D, 4), f("w_re").reshape(D, 32)], axis=1)),
        "b_r": np.ascontiguousarray(np.concatenate([f("b_rg").reshape(1, 4), f("b_re").reshape(1, 32)], axis=1)),
        "w_eg": f("w_eg").reshape(32, D, 512), "w_eu": f("w_eu").reshape(32, D, 512), "w_ed": f("w_ed").reshape(32, 512, D),
    }
    pos = np.asarray(inputs["positions"]).astype(np.int32)
    memv = np.asarray(inputs["mem"], np.float32)
    in_maps = []
    owns = []
    for c in range(8):
        b, h = c // 2, c % 2
        cst, own_tok = _consts(h)
        owns.append(own_tok)
        m = dict(shared)
        m.update(cst)
        m["x_nat"] = np.ascontiguousarray(x[b])
        m["x_own"] = np.ascontiguousarray(x[b][own_tok])
        m["pos_nat"] = np.ascontiguousarray(pos[b].reshape(1, T))
        m["pos_own"] = np.ascontiguousarray(pos[b][own_tok].reshape(1, NOWN))
        m["mem"] = np.ascontiguousarray(memv[b])
        in_maps.append(m)
    if "nc" not in _NC_CACHE:
        _NC_CACHE["nc"] = build()
    nc = _NC_CACHE["nc"]
    res = run_bass_kernel_spmd(nc, in_maps, core_ids=list(range(8)))
    out = np.zeros((4, T, D), np.float32)
    for c in range(8):
        out[c // 2][owns[c]] = res.results[c]["out"]
    return out
```
